# Optimizing a Trainium2 kernel written in Bass

```python
import jax, jax.numpy as jnp
from jax import lax
import numpy as np

D_MODEL = 1024
BATCH = 8
SEQ = 2048
DEPTH = 1

D_MIX = D_MODEL
D_LRU = D_MIX // 2
LRU_BLOCKS = 8
LRU_BLOCK_DIM = D_LRU // LRU_BLOCKS
CONV_WIDTH = 4
LRU_C = 8.0
D_ATTN = D_MIX - D_LRU
ATTN_HEADS = 8
HEAD_DIM = D_ATTN // ATTN_HEADS
Q_BLOCK = 128
D_IN_PROJ = 2 * D_LRU + 3 * D_ATTN + ATTN_HEADS
IN_SPLITS = (D_LRU, 2 * D_LRU, 2 * D_LRU + D_ATTN, 2 * D_LRU + 2 * D_ATTN, 2 * D_LRU + 3 * D_ATTN)
N_EXPERTS = 32
TOP_K = 4
D_EXPERT = D_MODEL
SWIGLU_ALPHA = 1.702
SWIGLU_LIMIT = 7.0
EXPERT_BLOCK = 256
N_MOD = 6
RMS_EPS = 1e-6

kernel_name = "hybrid_rglru_fox_moe_block"


def rmsnorm(x, g):
    xf = x.astype(jnp.float32)
    y = xf * lax.rsqrt(jnp.mean(xf * xf, axis=-1, keepdims=True) + RMS_EPS)
    return (y * g.astype(jnp.float32)).astype(x.dtype)


def causal_depthwise_conv(x, w, b):
    y = lax.conv_general_dilated(
        x, w[:, None, :].astype(x.dtype), window_strides=(1,),
        padding=[(CONV_WIDTH - 1, 0)], dimension_numbers=("NWC", "WIO", "NWC"),
        feature_group_count=x.shape[-1])
    return y + b


def _lin_rec_combine(left, right):
    a_l, b_l = left
    a_r, b_r = right
    return a_l * a_r, a_r * b_l + b_r


def rg_lru(x, wa, ba, wx, bx, lam):
    b, s, _ = x.shape
    xg = x.reshape(b, s, LRU_BLOCKS, LRU_BLOCK_DIM)
    r = jax.nn.sigmoid(jnp.einsum("bsgi,gij->bsgj", xg, wa) + ba).reshape(b, s, D_LRU)
    i = jax.nn.sigmoid(jnp.einsum("bsgi,gij->bsgj", xg, wx) + bx).reshape(b, s, D_LRU)
    log_a = -LRU_C * r.astype(jnp.float32) * jax.nn.softplus(-lam.astype(jnp.float32))
    a = jnp.exp(log_a)
    gated_x = jnp.sqrt(1.0 - jnp.exp(2.0 * log_a)) * (i * x).astype(jnp.float32)
    _, h = lax.associative_scan(_lin_rec_combine, (a, gated_x), axis=1)
    return h.astype(x.dtype)


def forgetting_attention(q, k, v, f_logit):
    b, s, h, dh = q.shape
    nb = s // Q_BLOCK
    cum = jnp.cumsum(jax.nn.log_sigmoid(f_logit.astype(jnp.float32)), axis=1).transpose(0, 2, 1)
    qb = q.reshape(b, nb, Q_BLOCK, h, dh).transpose(1, 0, 2, 3, 4)
    cq = cum.reshape(b, h, nb, Q_BLOCK).transpose(2, 0, 1, 3)
    kpos = jnp.arange(s, dtype=jnp.int32)
    qpos = kpos.reshape(nb, Q_BLOCK)
    scale = dh ** -0.5

    def block(args):
        q_blk, c_blk, p_blk = args
        logits = jnp.einsum("bqhd,bkhd->bhqk", q_blk, k).astype(jnp.float32) * scale
        logits = logits + c_blk[..., :, None] - cum[..., None, :]
        logits = jnp.where(p_blk[:, None] >= kpos[None, :], logits, -jnp.inf)
        p = jax.nn.softmax(logits, axis=-1).astype(v.dtype)
        return jnp.einsum("bhqk,bkhd->bqhd", p, v)

    out = lax.map(block, (qb, cq, qpos))
    return out.transpose(1, 0, 2, 3, 4).reshape(b, s, h * dh)


def hybrid_mixer(h, w_in, conv_w, conv_b, lru_wa, lru_ba, lru_wx, lru_bx, lru_lambda,
                 attn_fb, gn_lru, gn_attn, w_out):
    b, s, _ = h.shape
    proj = h @ w_in
    x_lru, y_lru, q, k, v, f_logit = jnp.split(proj, IN_SPLITS, axis=-1)
    x_conv = causal_depthwise_conv(x_lru, conv_w, conv_b)
    lru_out = rg_lru(x_conv, lru_wa, lru_ba, lru_wx, lru_bx, lru_lambda) * jax.nn.gelu(y_lru)
    shp = (b, s, ATTN_HEADS, HEAD_DIM)
    attn_out = forgetting_attention(q.reshape(shp), k.reshape(shp), v.reshape(shp), f_logit + attn_fb)
    merged = jnp.concatenate([rmsnorm(lru_out, gn_lru), rmsnorm(attn_out, gn_attn)], axis=-1)
    return merged @ w_out


def moe_ffn(h, w_router, b_router, w_up, b_up, w_down, b_down):
    n = h.shape[0]
    logits = (h @ w_router + b_router).astype(jnp.float32)
    top_logit, top_idx = lax.top_k(logits, TOP_K)
    top_w = jax.nn.softmax(top_logit, axis=-1).astype(h.dtype)
    e_flat = top_idx.reshape(-1).astype(jnp.int32)
    w_flat = top_w.reshape(-1)
    tok_flat = jnp.arange(n * TOP_K, dtype=jnp.int32) // TOP_K
    order = jnp.argsort(e_flat)
    e_sorted = e_flat[order]
    counts = jnp.bincount(e_flat, length=N_EXPERTS).astype(jnp.int32)
    starts = jnp.cumsum(counts) - counts
    padded = (counts + EXPERT_BLOCK - 1) // EXPERT_BLOCK * EXPERT_BLOCK
    pends = jnp.cumsum(padded)
    pstarts = pends - padded
    rank = jnp.arange(n * TOP_K, dtype=jnp.int32) - starts[e_sorted]
    dest = pstarts[e_sorted] + rank
    n_blocks = (n * TOP_K + EXPERT_BLOCK - 1) // EXPERT_BLOCK + N_EXPERTS
    n_rows = n_blocks * EXPERT_BLOCK
    row_tok = jnp.zeros((n_rows,), jnp.int32).at[dest].set(tok_flat[order])
    row_w = jnp.zeros((n_rows,), h.dtype).at[dest].set(w_flat[order])
    blk_start = jnp.arange(n_blocks, dtype=jnp.int32) * EXPERT_BLOCK
    blk_exp = jnp.minimum(jnp.searchsorted(pends, blk_start, side="right"), N_EXPERTS - 1).astype(jnp.int32)

    def expert_block(args):
        tok, e = args
        xb = h[tok]
        gu = xb @ w_up[e] + b_up[e]
        glu = jnp.minimum(gu[:, :D_EXPERT], SWIGLU_LIMIT)
        lin = jnp.clip(gu[:, D_EXPERT:], -SWIGLU_LIMIT, SWIGLU_LIMIT)
        act = glu * jax.nn.sigmoid(SWIGLU_ALPHA * glu) * (lin + 1.0)
        return act @ w_down[e] + b_down[e]

    rows = lax.map(expert_block, (row_tok.reshape(n_blocks, EXPERT_BLOCK), blk_exp))
    return jax.ops.segment_sum(rows.reshape(n_rows, -1) * row_w[:, None], row_tok, num_segments=n)


def setup_inputs(seed: int = 0) -> dict:
    key = jax.random.key(seed)
    ks = jax.random.split(key, 26)

    def nrm(k, shape, scale):
        return jax.random.normal(k, shape, jnp.float32) * scale

    u = jax.random.uniform(ks[13], (DEPTH, D_LRU), jnp.float32, 0.9, 0.999)
    p = u ** (1.0 / LRU_C)
    return {
        "x": nrm(ks[0], (BATCH, SEQ, D_MODEL), 1.0),
        "c": nrm(ks[1], (BATCH, D_MODEL), 1.0),
        "w_ada": nrm(ks[2], (DEPTH, D_MODEL, N_MOD * D_MODEL), 0.5 * D_MODEL ** -0.5),
        "b_ada": nrm(ks[3], (DEPTH, N_MOD * D_MODEL), 0.02),
        "norm_mix_pre": 1.0 + nrm(ks[4], (DEPTH, D_MODEL), 0.05),
        "norm_mix_post": 1.0 + nrm(ks[5], (DEPTH, D_MODEL), 0.05),
        "w_in": nrm(ks[6], (DEPTH, D_MODEL, D_IN_PROJ), D_MODEL ** -0.5),
        "conv_w": nrm(ks[7], (DEPTH, CONV_WIDTH, D_LRU), CONV_WIDTH ** -0.5),
        "conv_b": nrm(ks[8], (DEPTH, D_LRU), 0.02),
        "lru_wa": nrm(ks[9], (DEPTH, LRU_BLOCKS, LRU_BLOCK_DIM, LRU_BLOCK_DIM), LRU_BLOCK_DIM ** -0.5),
        "lru_ba": nrm(ks[10], (DEPTH, LRU_BLOCKS, LRU_BLOCK_DIM), 0.02),
        "lru_wx": nrm(ks[11], (DEPTH, LRU_BLOCKS, LRU_BLOCK_DIM, LRU_BLOCK_DIM), LRU_BLOCK_DIM ** -0.5),
        "lru_bx": nrm(ks[12], (DEPTH, LRU_BLOCKS, LRU_BLOCK_DIM), 0.02),
        "lru_lambda": jnp.log(p) - jnp.log1p(-p),
        "attn_fb": jnp.linspace(1.0, 5.0, ATTN_HEADS, dtype=jnp.float32)[None, :] + nrm(ks[14], (DEPTH, ATTN_HEADS), 0.1),
        "gn_lru": 1.0 + nrm(ks[15], (DEPTH, D_LRU), 0.05),
        "gn_attn": 1.0 + nrm(ks[16], (DEPTH, D_ATTN), 0.05),
        "w_out": nrm(ks[17], (DEPTH, D_MIX, D_MODEL), D_MIX ** -0.5),
        "norm_ffn_pre": 1.0 + nrm(ks[18], (DEPTH, D_MODEL), 0.05),
        "norm_ffn_post": 1.0 + nrm(ks[19], (DEPTH, D_MODEL), 0.05),
        "w_router": nrm(ks[20], (DEPTH, D_MODEL, N_EXPERTS), D_MODEL ** -0.5),
        "b_router": nrm(ks[21], (DEPTH, N_EXPERTS), 0.01),
        "w_up": nrm(ks[22], (DEPTH, N_EXPERTS, D_MODEL, 2 * D_EXPERT), D_MODEL ** -0.5),
        "b_up": nrm(ks[23], (DEPTH, N_EXPERTS, 2 * D_EXPERT), 0.01),
        "w_down": nrm(ks[24], (DEPTH, N_EXPERTS, D_EXPERT, D_MODEL), D_EXPERT ** -0.5),
        "b_down": nrm(ks[25], (DEPTH, N_EXPERTS, D_MODEL), 0.01),
    }


def reference(x, c, w_ada, b_ada, norm_mix_pre, norm_mix_post, w_in, conv_w, conv_b,
              lru_wa, lru_ba, lru_wx, lru_bx, lru_lambda, attn_fb, gn_lru, gn_attn, w_out,
              norm_ffn_pre, norm_ffn_post, w_router, b_router, w_up, b_up, w_down, b_down):
    b, s, d = x.shape
    cond = jax.nn.silu(c)
    for l in range(DEPTH):
        mod = cond @ w_ada[l] + b_ada[l]
        sh_m, sc_m, g_m, sh_f, sc_f, g_f = [m[:, None, :] for m in jnp.split(mod, N_MOD, axis=-1)]
        h = rmsnorm(x, norm_mix_pre[l]) * (1.0 + sc_m) + sh_m
        y = hybrid_mixer(h, w_in[l], conv_w[l], conv_b[l], lru_wa[l], lru_ba[l], lru_wx[l],
                         lru_bx[l], lru_lambda[l], attn_fb[l], gn_lru[l], gn_attn[l], w_out[l])
        x = x + g_m * rmsnorm(y, norm_mix_post[l])
        h = rmsnorm(x, norm_ffn_pre[l]) * (1.0 + sc_f) + sh_f
        y = moe_ffn(h.reshape(b * s, d), w_router[l], b_router[l], w_up[l], b_up[l],
                    w_down[l], b_down[l]).reshape(b, s, d)
        x = x + g_f * rmsnorm(y, norm_ffn_post[l])
    return x
```

```python
import os
import numpy as np
import concourse.bass as bass
import concourse.mybir as mybir
from concourse.bass_utils import run_bass_kernel_spmd

F32 = mybir.dt.float32
BF16 = mybir.dt.bfloat16
AF = mybir.ActivationFunctionType
ALU = mybir.AluOpType

D = 1024
S = 2048
NT = 16
NE = 32
EPS = 1e-6
SB_BASE = 16512
SB_TOP = 229344
SAME_SYNC = True
KSKIP = os.environ.get('KSKIP', '').split(',')
NCOL = 629
DT_SIZE = {F32: 4, BF16: 2}


class Buf:
    __slots__ = ("w", "r")

    def __init__(self):
        self.w = None
        self.r = {}


class Tile:
    def __init__(self, h):
        self.h = h
        self.bufs = {}

    def ap(self):
        return self.h.ap()

    def __getitem__(self, key):
        return self.h.ap()[key]

    def b(self, key=None):
        if key not in self.bufs:
            self.bufs[key] = Buf()
        return self.bufs[key]


class K:
    def __init__(self, nc, stack):
        self.nc = nc
        self.eng = {"pe": nc.tensor, "act": nc.scalar, "dve": nc.vector, "pool": nc.gpsimd, "sp": nc.sync}
        self.sems = {}
        self.cnt = {}
        self.waited = {e: {} for e in self.eng}
        for e in self.eng:
            self.sems[e] = stack.enter_context(nc.semaphore("s_" + e))
            self.cnt[e] = 0
        self.R = 8
        self.dma_n = {}
        for q in ("sp", "pool", "act"):
            self.dma_n[q] = 0
            for i in range(self.R):
                self.sems[("ring", q, i)] = stack.enter_context(nc.semaphore("r_%s_%d" % (q, i)))
        self.off = SB_BASE
        self.names = 0

    def alloc(self, shape, dtype, off=None, name=None):
        size = int(np.prod(shape[1:])) * DT_SIZE[dtype]
        size = (size + 63) // 64 * 64
        if off is None:
            off = self.off
            self.off += size
        assert off + size <= SB_TOP, ("SBUF overflow", name, off, size)
        self.names += 1
        h = self.nc.alloc_sbuf_tensor_at("%s_%d" % (name or "t", self.names), list(shape), dtype, offset=off)
        return Tile(h)

    def _deps(self, reads, writes):
        deps = {}

        def add(tok):
            if tok is None:
                return
            k, v = tok
            if deps.get(k, 0) < v:
                deps[k] = v

        for b in reads:
            add(b.w)
        for b in writes:
            add(b.w)
            for k, v in b.r.items():
                add((k, v))
        return deps

    def _wait(self, e, deps):
        w = self.waited[e]
        for k, v in deps.items():
            if k == e and (not SAME_SYNC or e == "pe" or e == "sp"):
                continue
            if w.get(k, 0) < v:
                self.eng[e].wait_ge(self.sems[k], v)
                w[k] = v

    def _commit(self, tok, reads, writes):
        k, v = tok
        for b in writes:
            b.w = tok
            b.r = {}
        for b in reads:
            if b.r.get(k, 0) < v:
                b.r[k] = v

    def op(self, e, fn, reads=(), writes=()):
        self._wait(e, self._deps(reads, writes))
        ins = fn(self.eng[e])
        self.cnt[e] += 1
        ins.then_inc(self.sems[e], 1)
        self._commit((e, self.cnt[e]), reads, writes)

    def dma(self, q, out, in_, reads=(), writes=(), **kw):
        n = self.dma_n[q]
        slot = n % self.R
        val = 16 * (n // self.R + 1)
        key = ("ring", q, slot)
        deps = self._deps(reads, writes)
        if val > 16:
            deps[key] = max(deps.get(key, 0), val - 16)
        self._wait(q, deps)
        ins = self.eng[q].dma_start(out=out, in_=in_, **kw)
        ins.then_inc(self.sems[key], 16)
        self.dma_n[q] = n + 1
        self._commit((key, val), reads, writes)

    def all_tokens(self):
        toks = {}
        for e in self.eng:
            if self.cnt[e] > 0:
                toks[e] = self.cnt[e]
        for q, n in self.dma_n.items():
            for i in range(min(n, self.R)):
                last = n - 1 - ((n - 1 - i) % self.R)
                toks[("ring", q, i)] = 16 * (last // self.R + 1)
        return toks

    def barrier(self, engines=("pe", "act", "dve", "pool", "sp")):
        toks = self.all_tokens()
        for e in engines:
            w = self.waited[e]
            for k, v in toks.items():
                if k == e:
                    continue
                if w.get(k, 0) < v:
                    self.eng[e].wait_ge(self.sems[k], v)
                    w[k] = v


def build_program(debug=(), n_experts_dbg=None, stop_after=None):
    from contextlib import ExitStack
    nc = bass.Bass("TRN2", target_bir_lowering=False)
    stack = ExitStack()

    def din(name, shape, dt=F32):
        return nc.dram_tensor(name, list(shape), dt, kind="ExternalInput").ap()

    x_d = din("x", [S, D])
    cols_d = din("cols", [128, NCOL])
    rows_d = din("rows", [4, D])
    brout_d = din("b_router", [1, NE])
    wada_d = din("w_ada", [D, 6 * D])
    win_d = din("w_in", [D, 2568])
    bd_d = din("bdiag", [128, 2 * 4 * 128])
    sel_d = din("sel", [97, 2 * 8 * 70])
    wout_d = din("w_out", [D, D])
    wr_d = din("w_router", [D, NE])
    wup_d = din("w_up", [NE, D, 2 * D])
    wdn_d = din("w_down", [NE, D, D])
    bdn_d = din("b_down", [NE, D])
    out_d = nc.dram_tensor("out", [S, D], F32, kind="ExternalOutput").ap()
    x2_d = nc.dram_tensor("x2_scratch", [S, D], F32, kind="ExternalOutput").ap()
    x2sc = Tile(None)
    dbg_out = {}

    k = K(nc, stack)
    KB = 1024

    PS = [Tile(stack.enter_context(nc.psum_tensor("ps%d" % i, [128, 1024], F32))) for i in range(4)]

    def bank(i):
        return PS[i // 2], (i % 2) * 512

    identf = k.alloc([128, 128], F32, name="identf")
    identb = k.alloc([128, 128], BF16, name="identb")
    onesf = k.alloc([128, 128], F32, name="onesf")
    maskT = k.alloc([128, 128], BF16, name="maskT")
    cols = k.alloc([128, NCOL], F32, name="cols")
    modT = k.alloc([128, 48], F32, name="modT")
    AS = k.alloc([128, 32], F32, name="AS")
    lam = k.alloc([128, 16], F32, name="lam")
    b1c = k.alloc([128, 512], F32, name="b1c")
    Grow_m = k.alloc([128, D], F32, name="Grow_m")
    Grow_f = k.alloc([128, D], F32, name="Grow_f")
    brout = k.alloc([128, NE], F32, name="brout")
    P_END = k.off
    B0 = P_END

    def dbg(name, tile_ap, shape, reads):
        if name not in debug:
            return
        d = nc.dram_tensor("dbg_" + name, list(shape), tile_ap.dtype, kind="ExternalOutput").ap()
        dbg_out[name] = d
        k.dma("sp", d, tile_ap, reads=reads)

    k.op("pool", lambda e: e.memset(identf[:], 1.0), writes=[identf.b()])
    k.op("pool", lambda e: e.affine_select(out=identf[:], in_=identf[:], pattern=[[1, 128]],
                                           compare_op=ALU.is_equal, fill=0.0, base=0, channel_multiplier=-1),
         reads=[identf.b()], writes=[identf.b()])
    k.op("pool", lambda e: e.tensor_copy(out=identb[:], in_=identf[:]), reads=[identf.b()], writes=[identb.b()])
    k.op("pool", lambda e: e.memset(onesf[:], 1.0), writes=[onesf.b()])
    k.op("pool", lambda e: e.memset(maskT[:], 0.0), writes=[maskT.b()])
    k.op("pool", lambda e: e.affine_select(out=maskT[:], in_=maskT[:], pattern=[[1, 128]],
                                           compare_op=ALU.is_ge, fill=-30000.0, base=0, channel_multiplier=-1),
         reads=[maskT.b()], writes=[maskT.b()])
    k.dma("sp", cols[:], cols_d[:, :], writes=[cols.b()])
    k.dma("sp", brout[:], brout_d[0, :].partition_broadcast(128), writes=[brout.b()])
    k.dma("sp", Grow_m[:], rows_d[2, :].partition_broadcast(128), writes=[Grow_m.b()])
    k.dma("sp", Grow_f[:], rows_d[3, :].partition_broadcast(128), writes=[Grow_f.b()])

    o = B0
    hT = k.alloc([128, 8, S], BF16, off=o, name="hT"); o += 32 * KB
    W_A = k.alloc([128, 8, 1024], BF16, off=o, name="W_A"); o += 16 * KB
    merged = k.alloc([128, 8, S], BF16, off=o, name="merged"); o += 32 * KB
    O_ATT = o
    W_QK = k.alloc([128, 8, 16, 70], BF16, off=o, name="W_QK"); o += 17920
    W_V = k.alloc([128, 8, 512], BF16, off=o, name="W_V"); o += 8 * KB
    W_F = k.alloc([128, 8, 97], BF16, off=o, name="W_F"); o += 1600
    vp = k.alloc([128, NT, 8, 65], BF16, off=o, name="vp"); o += 16640
    CP_OFF = o
    CP = k.alloc([97, S], BF16, off=o, name="CP"); o += 4 * KB
    SEL = k.alloc([97, 2, 8, 70], BF16, off=o, name="SEL"); o += 2240
    R0 = o

    win_v = win_d.rearrange("(k p) n -> p k n", p=128)
    k.dma("pool", SEL[:], sel_d.rearrange("p (a h c) -> p a h c", a=2, h=8), writes=[SEL.b()])

    MRG = B0 + 48 * KB
    xin4 = [k.alloc([128, D], F32, off=MRG + i * 4 * KB, name="xin4_%d" % i) for i in range(4)]
    xn = [k.alloc([128, D], F32, off=MRG + 16 * KB + i * 4 * KB, name="xn%d" % i) for i in range(4)]
    junk = k.alloc([128, D], BF16, off=CP_OFF, name="junk")
    st = k.alloc([128, 64], F32, off=CP_OFF + 2 * KB, name="st")

    def pre_B(g):
        for tt in range(4):
            i = g * 4 + tt
            xt = xin4[tt]
            k.dma("sp", xt[:], x_d[i * 128:(i + 1) * 128, :], writes=[xt.b()])
        for tt in range(4):
            xt = xin4[tt]
            sc = st[:, tt * 4:tt * 4 + 4]
            k.op("act", lambda e, xt=xt, sc=sc: e.activation(out=junk[:], in_=xt[:], func=AF.Square, accum_out=sc[:, 0:1]),
                 reads=[xt.b()], writes=[junk.b(), st.b(tt)])
            k.op("act", lambda e, sc=sc: e.activation(out=sc[:, 1:2], in_=sc[:, 0:1], func=AF.Sqrt, scale=1.0 / D, bias=EPS),
                 reads=[st.b(tt)], writes=[st.b(tt)])
            k.op("dve", lambda e, sc=sc: e.reciprocal(out=sc[:, 2:3], in_=sc[:, 1:2]), reads=[st.b(tt)], writes=[st.b(tt)])
            k.op("dve", lambda e, xt=xt, sc=sc, tt=tt: e.tensor_scalar(out=xn[tt][:], in0=xt[:], scalar1=sc[:, 2:3],
                                                                       scalar2=None, op0=ALU.mult),
                 reads=[xt.b(), st.b(tt)], writes=[xn[tt].b()])

    def post_B(g):
        a0 = 0
        for kk in range(8):
            bt, c0 = bank(kk % 2)
            for tt in range(4):
                k.op("pe", lambda e, kk=kk, tt=tt, bt=bt, c0=c0: e.transpose(
                    out=bt[:, c0 + tt * 128:c0 + (tt + 1) * 128], in_=xn[tt][:, kk * 128:(kk + 1) * 128], identity=identf[:]),
                    reads=[xn[tt].b(), identf.b()], writes=[bt.b(kk % 2)])
            k.op("dve", lambda e, kk=kk, bt=bt, c0=c0: e.tensor_scalar(
                out=hT[:, kk, g * 512:(g + 1) * 512], in0=bt[:, c0:c0 + 512],
                scalar1=AS[:, a0 + kk:a0 + kk + 1], scalar2=AS[:, a0 + 8 + kk:a0 + 9 + kk], op0=ALU.mult, op1=ALU.add),
                reads=[bt.b(kk % 2), AS.b()], writes=[hT.b(g)])

    pre_B(0)
    o = R0
    wada = [k.alloc([128, 8, 1024], BF16, off=o + i * 16 * KB, name="wada%d" % i) for i in range(3)]
    o += 48 * KB
    csil = k.alloc([128, 8], F32, off=o, name="csil"); o += 64
    cb = k.alloc([128, 8, 128], BF16, off=o, name="cb"); o += 2 * KB
    brow = k.alloc([128, 2, D], F32, off=o, name="brow"); o += 8 * KB

    k.op("act", lambda e: e.activation(out=csil[:], in_=cols[:, 0:8], func=AF.Silu), reads=[cols.b()], writes=[csil.b()])
    for kk in range(8):
        k.op("dve", lambda e, kk=kk: e.tensor_copy(out=cb[:, kk, :], in_=csil[:, kk:kk + 1].to_broadcast([128, 128])),
             reads=[csil.b()], writes=[cb.b()])
    wada_v = wada_d.rearrange("(k p) n -> p k n", p=128)
    pst, _ = bank(0)

    def adaln_cols(g, wb, cbt, pt, pc0, pkey):
        for j in range(8):
            for kk in range(8):
                k.op("pe", lambda e, j=j, kk=kk: e.matmul(
                    pt[:, pc0 + j:pc0 + j + 1], lhsT=wb[:, kk, j * 128:(j + 1) * 128], rhs=cbt[:, kk, 0:1],
                    start=(kk == 0), stop=(kk == 7)), reads=[wb.b(), cbt.b()], writes=[pkey])

    def adaln_gate(g, wb, cbt, browt):
        gt = PS[1]
        for hh in range(2):
            for kk in range(8):
                k.op("pe", lambda e, hh=hh, kk=kk: e.matmul(
                    gt[:, hh * 512:(hh + 1) * 512], lhsT=cbt[:, kk, :], rhs=wb[:, kk, hh * 512:(hh + 1) * 512],
                    start=(kk == 0), stop=(kk == 7)), reads=[wb.b(), cbt.b()], writes=[gt.b(hh * 512)])
        G = Grow_m if g == 2 else Grow_f
        bi = 0 if g == 2 else 1
        k.op("dve", lambda e: e.tensor_tensor(out=browt[:, bi, :], in0=gt[:, :], in1=browt[:, bi, :], op=ALU.add),
             reads=[gt.b(0), gt.b(512), browt.b()], writes=[browt.b()])
        k.op("dve", lambda e: e.tensor_tensor(out=G[:], in0=G[:], in1=browt[:, bi, :], op=ALU.mult),
             reads=[browt.b(), G.b()], writes=[G.b()])

    def adaln_AS(a0, g0, sc0, sh0):
        k.op("dve", lambda e: e.scalar_tensor_tensor(
            out=AS[:, a0:a0 + 8], in0=modT[:, sc0:sc0 + 8], scalar=1.0, in1=cols[:, g0:g0 + 8],
            op0=ALU.add, op1=ALU.mult), reads=[modT.b(), cols.b()], writes=[AS.b()])
        k.op("dve", lambda e: e.tensor_copy(out=AS[:, a0 + 8:a0 + 16], in_=modT[:, sh0:sh0 + 8]),
             reads=[modT.b()], writes=[AS.b()])

    for g in range(2):
        wb = wada[g]
        k.dma("pool", wb[:], wada_v[:, :, g * 1024:(g + 1) * 1024], writes=[wb.b()])
    k.dma("pool", W_A[:], win_v[:, :, 0:1024], writes=[W_A.b()])
    k.dma("pool", W_V[:], win_v[:, :, 2048:2560], writes=[W_V.b()])
    for g in range(2):
        adaln_cols(g, wada[g], cb, pst, 8 * g, pst.b(0))
    k.op("dve", lambda e: e.tensor_tensor(out=modT[:, 0:16], in0=pst[:, 0:16], in1=cols[:, 8:24], op=ALU.add),
         reads=[pst.b(0), cols.b()], writes=[modT.b()])
    adaln_AS(0, 56, 8, 0)
    L = cols[:, 100:104]
    k.op("act", lambda e: e.activation(out=lam[:, 8:12], in_=L, func=AF.Abs),
         reads=[cols.b()], writes=[lam.b()])
    k.op("act", lambda e: e.activation(out=lam[:, 8:12], in_=lam[:, 8:12], func=AF.Exp, scale=-1.0),
         reads=[lam.b()], writes=[lam.b()])
    k.op("act", lambda e: e.activation(out=lam[:, 8:12], in_=lam[:, 8:12], func=AF.Ln, bias=1.0),
         reads=[lam.b()], writes=[lam.b()])
    k.op("dve", lambda e: e.tensor_scalar(out=lam[:, 12:16], in0=L, scalar1=-1.0, scalar2=0.0, op0=ALU.mult, op1=ALU.max),
         reads=[cols.b()], writes=[lam.b()])
    k.op("dve", lambda e: e.tensor_tensor(out=lam[:, 8:12], in0=lam[:, 8:12], in1=lam[:, 12:16], op=ALU.add),
         reads=[lam.b()], writes=[lam.b()])
    k.op("dve", lambda e: e.tensor_scalar(out=lam[:, 0:4], in0=lam[:, 8:12], scalar1=-8.0, scalar2=None, op0=ALU.mult),
         reads=[lam.b()], writes=[lam.b()])
    k.op("dve", lambda e: e.tensor_scalar(out=lam[:, 4:8], in0=lam[:, 8:12], scalar1=-16.0, scalar2=None, op0=ALU.mult),
         reads=[lam.b()], writes=[lam.b()])
    k.op("dve", lambda e: e.tensor_scalar(out=b1c[:], in0=cols[:, 117:629], scalar1=1.0, scalar2=None, op0=ALU.add),
         reads=[cols.b()], writes=[b1c.b()])
    k.barrier()

    if stop_after == 'A':
        k.barrier()
        return nc, stack, dbg_out
    o = R0
    qkst = k.alloc([128, 8, 1024], BF16, off=o, name="qkst"); o += 16 * KB
    fst = k.alloc([128, 8, 8], BF16, off=o, name="fst"); o += 128
    O_F = o

    k.dma("pool", fst[:], win_v[:, :, 2560:2568], writes=[fst.b()])
    k.dma("pool", qkst[:], win_v[:, :, 1024:2048], writes=[qkst.b()])
    k.op("pool", lambda e: e.memset(W_F[:], 0.0), writes=[W_F.b()])
    for r0 in (0, 32, 64):
        k.op("pool", lambda e, r0=r0: e.tensor_copy(out=W_F[:, :, r0:r0 + 8], in_=fst[:]),
             reads=[fst.b()], writes=[W_F.b()])
    k.op("pool", lambda e: e.memset(vp[:, :, :, 64:65], 1.0), writes=[vp.b("ones")])

    def prenorm_group(g, src_tiles, a0, dstT, dstF32=None, xn=None, junk=None, st=None):
        for tt in range(4):
            xt, xb_ = src_tiles[tt]
            i = g * 4 + tt
            sc = st[:, tt * 4:tt * 4 + 4]
            k.op("act", lambda e, xt=xt, sc=sc: e.activation(out=junk[:], in_=xt[:], func=AF.Square, accum_out=sc[:, 0:1]),
                 reads=[xb_], writes=[junk.b(), st.b(tt)])
            k.op("act", lambda e, sc=sc: e.activation(out=sc[:, 1:2], in_=sc[:, 0:1], func=AF.Sqrt, scale=1.0 / D, bias=EPS),
                 reads=[st.b(tt)], writes=[st.b(tt)])
            k.op("dve", lambda e, sc=sc: e.reciprocal(out=sc[:, 2:3], in_=sc[:, 1:2]), reads=[st.b(tt)], writes=[st.b(tt)])
            k.op("dve", lambda e, xt=xt, sc=sc, tt=tt: e.tensor_scalar(out=xn[tt][:], in0=xt[:], scalar1=sc[:, 2:3],
                                                                       scalar2=None, op0=ALU.mult),
                 reads=[xb_, st.b(tt)], writes=[xn[tt].b()])
        for kk in range(8):
            bt, c0 = bank(kk % 2)
            for tt in range(4):
                k.op("pe", lambda e, kk=kk, tt=tt, bt=bt, c0=c0: e.transpose(
                    out=bt[:, c0 + tt * 128:c0 + (tt + 1) * 128], in_=xn[tt][:, kk * 128:(kk + 1) * 128], identity=identf[:]),
                    reads=[xn[tt].b(), identf.b()], writes=[bt.b(kk % 2)])
            k.op("dve", lambda e, kk=kk, bt=bt, c0=c0: e.tensor_scalar(
                out=dstT[:, kk, g * 512:(g + 1) * 512], in0=bt[:, c0:c0 + 512],
                scalar1=AS[:, a0 + kk:a0 + kk + 1], scalar2=AS[:, a0 + 8 + kk:a0 + 9 + kk], op0=ALU.mult, op1=ALU.add),
                reads=[bt.b(kk % 2), AS.b()], writes=[dstT.b(g)])
            if dstF32 is not None:
                k.op("dve", lambda e, kk=kk, bt=bt, c0=c0: e.tensor_scalar(
                    out=dstF32[:, kk, :], in0=bt[:, c0:c0 + 512],
                    scalar1=AS[:, a0 + kk:a0 + kk + 1], scalar2=AS[:, a0 + 8 + kk:a0 + 9 + kk], op0=ALU.mult, op1=ALU.add),
                    reads=[bt.b(kk % 2), AS.b()], writes=[dstF32.b()])

    o = O_F
    F1 = k.alloc([97, S], F32, off=o, name="F1"); o += 8 * KB
    F2 = k.alloc([97, S], F32, off=o, name="F2"); o += 8 * KB
    F3 = k.alloc([97, S], F32, off=o, name="F3"); o += 8 * KB
    CPh = k.alloc([97, S], BF16, off=o, name="CPh"); o += 4 * KB
    CPm = k.alloc([97, S], BF16, off=o, name="CPm"); o += 4 * KB
    CPl = k.alloc([97, S], BF16, off=o, name="CPl"); o += 4 * KB
    assert o <= SB_TOP
    fbcol = cols[0:97, 112:113]

    def vproj(i):
        bt, c0 = bank(2 + i % 2)
        for kk in range(8):
            k.op("pe", lambda e, i=i, kk=kk, bt=bt, c0=c0: e.matmul(
                bt[:, c0:c0 + 512], lhsT=hT[:, kk, i * 128:(i + 1) * 128], rhs=W_V[:, kk, :],
                start=(kk == 0), stop=(kk == 7)), reads=[hT.b(i // 4), W_V.b()], writes=[bt.b(i % 2)])
        k.op("act", lambda e, i=i, bt=bt, c0=c0: e.activation(
            out=vp[:, i, :, 0:64], in_=bt[:, c0:c0 + 512].rearrange("p (h c) -> p h c", c=64), func=AF.Copy),
            reads=[bt.b(i % 2)], writes=[vp.b(i)])

    def fproj(g):
        bt, c0 = bank(4 + g % 2)
        for kk in range(8):
            k.op("pe", lambda e, g=g, kk=kk, bt=bt, c0=c0: e.matmul(
                bt[0:97, c0:c0 + 512], lhsT=W_F[:, kk, :], rhs=hT[:, kk, g * 512:(g + 1) * 512],
                start=(kk == 0), stop=(kk == 7)), reads=[hT.b(g), W_F.b()], writes=[bt.b(g % 2)])
        k.op("dve", lambda e, g=g, bt=bt, c0=c0: e.tensor_scalar(
            out=F1[:, g * 512:(g + 1) * 512], in0=bt[0:97, c0:c0 + 512], scalar1=fbcol, scalar2=None, op0=ALU.add),
            reads=[bt.b(g % 2), cols.b()], writes=[F1.b()])

    for g in range(4):
        if g >= 1:
            pre_B(g)
        post_B(g)
        if g >= 1:
            for tt in range(4):
                vproj((g - 1) * 4 + tt)
            fproj(g - 1)
    for tt in range(4):
        vproj(12 + tt)
    fproj(3)
    dbg("hT", hT[:], [128, 8, S], [hT.b(g) for g in range(4)])
    k.op("act", lambda e: e.activation(out=F2[:], in_=F1[:], func=AF.Abs),
         reads=[F1.b()], writes=[F2.b()])
    k.op("act", lambda e: e.activation(out=F2[:], in_=F2[:], func=AF.Exp, scale=-1.0), reads=[F2.b()], writes=[F2.b()])
    k.op("act", lambda e: e.activation(out=F2[:], in_=F2[:], func=AF.Ln, bias=1.0), reads=[F2.b()], writes=[F2.b()])
    k.op("dve", lambda e: e.tensor_scalar(out=F1[:], in0=F1[:], scalar1=0.0, scalar2=None, op0=ALU.min),
         reads=[F1.b()], writes=[F1.b()])
    k.op("dve", lambda e: e.tensor_tensor(out=F1[:], in0=F1[:], in1=F2[:], op=ALU.subtract),
         reads=[F1.b(), F2.b()], writes=[F1.b()])
    k.op("pool", lambda e: e.memset(F3[:], 1.0), writes=[F3.b()])
    k.op("pool", lambda e: e.memset(W_QK[:], 0.0), writes=[W_QK.b()])
    k.op("pool", lambda e: e.tensor_copy(out=W_QK[:, :, :, 0:64],
                                         in_=qkst[:].rearrange("p k (h c) -> p k h c", c=64)),
         reads=[qkst.b()], writes=[W_QK.b()])
    k.op("dve", lambda e: e.tensor_tensor_scan(out=F2[:], data0=F3[:], data1=F1[:], initial=0.0, op0=ALU.mult, op1=ALU.add),
         reads=[F1.b(), F3.b()], writes=[F2.b()])
    dbg("C", F2[:], [97, S], [F2.b()])
    k.op("dve", lambda e: e.tensor_copy(out=CPh[:], in_=F2[:]), reads=[F2.b()], writes=[CPh.b()])
    k.op("dve", lambda e: e.tensor_tensor(out=F1[:], in0=F2[:], in1=CPh[:], op=ALU.subtract),
         reads=[F2.b(), CPh.b()], writes=[F1.b()])
    k.op("dve", lambda e: e.tensor_copy(out=CPm[:], in_=F1[:]), reads=[F1.b()], writes=[CPm.b()])
    k.op("dve", lambda e: e.tensor_tensor(out=F3[:], in0=F1[:], in1=CPm[:], op=ALU.subtract),
         reads=[F1.b(), CPm.b()], writes=[F3.b()])
    k.op("dve", lambda e: e.tensor_copy(out=CPl[:], in_=F3[:]), reads=[F3.b()], writes=[CPl.b()])
    mc = lambda j: cols[0:97, 113 + j:114 + j]
    k.op("dve", lambda e: e.tensor_scalar(out=CP[:], in0=CPh[:], scalar1=mc(0), scalar2=None, op0=ALU.mult),
         reads=[CPh.b(), cols.b()], writes=[CP.b()])
    k.op("dve", lambda e: e.scalar_tensor_tensor(out=CP[:], in0=CPm[:], scalar=mc(1), in1=CP[:], op0=ALU.mult, op1=ALU.add),
         reads=[CPm.b(), CP.b()], writes=[CP.b()])
    k.op("dve", lambda e: e.scalar_tensor_tensor(out=CP[:], in0=CPl[:], scalar=mc(2), in1=CP[:], op0=ALU.mult, op1=ALU.add),
         reads=[CPl.b(), CP.b()], writes=[CP.b()])
    k.op("dve", lambda e: e.tensor_scalar(out=CP[:], in0=CP[:], scalar1=mc(3), scalar2=None, op0=ALU.add),
         reads=[CP.b()], writes=[CP.b()])
    k.barrier()

    if stop_after == 'B':
        k.barrier()
        return nc, stack, dbg_out
    o = R0
    qk_t = [[k.alloc([70, S], BF16, off=o + (2 * s + j) * 4 * KB, name="qk%d%d" % (s, j)) for j in range(2)] for s in range(2)]
    o += 16 * KB
    PT = [k.alloc([128, 512], BF16, off=o + i * KB, name="PT%d" % i) for i in range(4)]; o += 4 * KB
    attn = k.alloc([128, NT, 512], F32, off=o, name="attn"); o += 32 * KB
    rden = k.alloc([128, 8], F32, off=o, name="rden"); o += 64
    ant = [k.alloc([128, 512], F32, off=o + i * 2 * KB, name="ant%d" % i) for i in range(4)]; o += 8 * KB
    st2 = k.alloc([128, 64], F32, off=o, name="st2"); o += 256
    junk2 = k.alloc([128, 512], BF16, off=o, name="junk2"); o += KB
    assert o <= SB_TOP

    def qk_proj(h):
        s = h % 2
        for j in range(2):
            dst = qk_t[s][j]
            for g in range(4):
                bt, c0 = bank(6 + g % 2)
                for kk in range(8):
                    k.op("pe", lambda e, j=j, g=g, kk=kk, bt=bt, c0=c0: e.matmul(
                        bt[0:70, c0:c0 + 512], lhsT=W_QK[:, kk, j * 8 + h, :], rhs=hT[:, kk, g * 512:(g + 1) * 512],
                        start=(kk == 0), stop=False), reads=[hT.b(g), W_QK.b()], writes=[bt.b(g % 2)])
                k.op("pe", lambda e, j=j, g=g, bt=bt, c0=c0: e.matmul(
                    bt[0:70, c0:c0 + 512], lhsT=SEL[:, j, h, :], rhs=CP[:, g * 512:(g + 1) * 512],
                    start=False, stop=True), reads=[CP.b(), SEL.b()], writes=[bt.b(g % 2)])
                if j == 0:
                    k.op("dve", lambda e, g=g, bt=bt, c0=c0, dst=dst: e.tensor_scalar(
                        out=dst[:, g * 512:(g + 1) * 512], in0=bt[0:70, c0:c0 + 512], scalar1=0.125, scalar2=None,
                        op0=ALU.mult), reads=[bt.b(g % 2)], writes=[dst.b()])
                else:
                    k.op("dve", lambda e, g=g, bt=bt, c0=c0, dst=dst: e.tensor_copy(
                        out=dst[:, g * 512:(g + 1) * 512], in_=bt[0:70, c0:c0 + 512]),
                        reads=[bt.b(g % 2)], writes=[dst.b()])

    def attn_head(h):
        s = h % 2
        qp, kp = qk_t[s]
        groups = []
        for i in range(NT):
            nb = i + 1
            for g0 in range(0, nb, 4):
                groups.append((i, list(range(g0, min(nb, g0 + 4)))))
        n = len(groups)

        def qk_mm(gi):
            i, blks = groups[gi]
            bt, c0 = bank(gi % 3)
            for jj, j in enumerate(blks):
                diag = (j == i)
                k.op("pe", lambda e, jj=jj, j=j, i=i, bt=bt, c0=c0, diag=diag: e.matmul(
                    bt[:, c0 + jj * 128:c0 + (jj + 1) * 128], lhsT=kp[:, j * 128:(j + 1) * 128],
                    rhs=qp[:, i * 128:(i + 1) * 128], start=True, stop=not diag),
                    reads=[kp.b(), qp.b()], writes=[bt.b(c0)])
                if diag:
                    k.op("pe", lambda e, jj=jj, bt=bt, c0=c0: e.matmul(
                        bt[:, c0 + jj * 128:c0 + (jj + 1) * 128], lhsT=identb[:], rhs=maskT[:],
                        start=False, stop=True), reads=[identb.b(), maskT.b()], writes=[bt.b(c0)])

        def exp_pv(gi):
            i, blks = groups[gi]
            bt, c0 = bank(gi % 3)
            nbk = len(blks)
            pt = PT[gi % 4]
            k.op("act", lambda e, bt=bt, c0=c0, nbk=nbk, pt=pt: e.activation(
                out=pt[:, 0:nbk * 128], in_=bt[:, c0:c0 + nbk * 128], func=AF.Exp),
                reads=[bt.b(c0)], writes=[pt.b()])
            ot, oc0 = bank(3 + (i % 2))
            for jj, j in enumerate(blks):
                k.op("pe", lambda e, jj=jj, j=j, i=i, ot=ot, oc0=oc0, pt=pt: e.matmul(
                    ot[:, oc0:oc0 + 65], lhsT=pt[:, jj * 128:(jj + 1) * 128], rhs=vp[:, j, h, :],
                    start=(j == 0), stop=(j == i)), reads=[pt.b(), vp.b(j), vp.b("ones")], writes=[ot.b(oc0)])
            if blks[-1] == i:
                k.op("dve", lambda e, ot=ot, oc0=oc0: e.reciprocal(out=rden[:, h:h + 1], in_=ot[:, oc0 + 64:oc0 + 65]),
                     reads=[ot.b(oc0)], writes=[rden.b(h)])
                k.op("dve", lambda e, ot=ot, oc0=oc0, i=i: e.tensor_scalar(
                    out=attn[:, i, h * 64:(h + 1) * 64], in0=ot[:, oc0:oc0 + 64], scalar1=rden[:, h:h + 1], scalar2=None,
                    op0=ALU.mult), reads=[ot.b(oc0), rden.b(h)], writes=[attn.b(i)])

        qk_mm(0)
        for gi in range(n):
            if gi + 1 < n:
                qk_mm(gi + 1)
            exp_pv(gi)

    qk_proj(0)
    for h in range(8):
        if h + 1 < 8:
            qk_proj(h + 1)
        attn_head(h)
    dbg("attn", attn[:], [128, NT, 512], [attn.b(i) for i in range(NT)])

    for g in range(4):
        for tt in range(4):
            i = g * 4 + tt
            sc = st2[:, tt * 4:tt * 4 + 4]
            k.op("act", lambda e, i=i, sc=sc: e.activation(out=junk2[:], in_=attn[:, i, :], func=AF.Square, accum_out=sc[:, 0:1]),
                 reads=[attn.b(i)], writes=[junk2.b(), st2.b(tt)])
            k.op("act", lambda e, sc=sc: e.activation(out=sc[:, 1:2], in_=sc[:, 0:1], func=AF.Sqrt, scale=1.0 / 512, bias=EPS),
                 reads=[st2.b(tt)], writes=[st2.b(tt)])
            k.op("dve", lambda e, sc=sc: e.reciprocal(out=sc[:, 2:3], in_=sc[:, 1:2]), reads=[st2.b(tt)], writes=[st2.b(tt)])
            k.op("dve", lambda e, i=i, sc=sc, tt=tt: e.tensor_scalar(out=ant[tt][:], in0=attn[:, i, :], scalar1=sc[:, 2:3],
                                                                     scalar2=None, op0=ALU.mult),
                 reads=[attn.b(i), st2.b(tt)], writes=[ant[tt].b()])
        for cc in range(4):
            bt, c0 = bank(cc % 2)
            for tt in range(4):
                k.op("pe", lambda e, cc=cc, tt=tt, bt=bt, c0=c0: e.transpose(
                    out=bt[:, c0 + tt * 128:c0 + (tt + 1) * 128], in_=ant[tt][:, cc * 128:(cc + 1) * 128], identity=identf[:]),
                    reads=[ant[tt].b(), identf.b()], writes=[bt.b(c0)])
            k.op("dve", lambda e, cc=cc, bt=bt, c0=c0, g=g: e.tensor_scalar(
                out=merged[:, 4 + cc, g * 512:(g + 1) * 512], in0=bt[:, c0:c0 + 512],
                scalar1=cols[:, 108 + cc:109 + cc], scalar2=None, op0=ALU.mult),
                reads=[bt.b(c0), cols.b()], writes=[merged.b(("a", g))])
    k.barrier()

    if stop_after == 'C':
        k.barrier()
        return nc, stack, dbg_out
    o = O_ATT
    xl = k.alloc([128, 4, S + 4], F32, off=o, name="xl"); o += 4 * (S + 4) * 4
    gl = k.alloc([128, 4, S], BF16, off=o, name="gl"); o += 16 * KB
    BD = k.alloc([128, 2, 4, 128], BF16, off=o, name="BD"); o += 2 * KB
    xc2 = [k.alloc([128, S], F32, off=o + i * 8 * KB, name="xc%d" % i) for i in range(2)]; o += 16 * KB
    xcb2 = [k.alloc([128, S], BF16, off=o + i * 4 * KB, name="xcb%d" % i) for i in range(2)]; o += 8 * KB
    T1 = k.alloc([128, S], F32, off=o, name="T1"); o += 8 * KB
    T2 = k.alloc([128, S], F32, off=o, name="T2"); o += 8 * KB
    T3 = k.alloc([128, S], F32, off=o, name="T3"); o += 8 * KB
    sq = [k.alloc([128, 512], F32, off=o + i * 2 * KB, name="sq%d" % i) for i in range(2)]; o += 4 * KB
    rs = [k.alloc([128, 512], F32, off=o + i * 2 * KB, name="rs%d" % i) for i in range(2)]; o += 4 * KB
    assert o <= SB_TOP, o
    k.dma("pool", BD[:], bd_d.rearrange("p (a c j) -> p a c j", a=2, c=4), writes=[BD.b()])
    k.op("pool", lambda e: e.memset(xl[:, :, 0:4], 0.0), writes=[xl.b("pad")])

    def lru_inproj(c):
        for g in range(4):
            bt, c0 = bank(g % 2)
            for kk in range(8):
                k.op("pe", lambda e, c=c, g=g, kk=kk, bt=bt, c0=c0: e.matmul(
                    bt[:, c0:c0 + 512], lhsT=W_A[:, kk, c * 128:(c + 1) * 128], rhs=hT[:, kk, g * 512:(g + 1) * 512],
                    start=(kk == 0), stop=(kk == 7)), reads=[hT.b(g), W_A.b()], writes=[bt.b(c0)])
            k.op("act", lambda e, c=c, g=g, bt=bt, c0=c0: e.activation(
                out=xl[:, c, 4 + g * 512:4 + (g + 1) * 512], in_=bt[:, c0:c0 + 512], func=AF.Copy),
                reads=[bt.b(c0)], writes=[xl.b(c)])
        for g in range(4):
            bt, c0 = bank(2 + g % 2)
            for kk in range(8):
                k.op("pe", lambda e, c=c, g=g, kk=kk, bt=bt, c0=c0: e.matmul(
                    bt[:, c0:c0 + 512], lhsT=W_A[:, kk, 512 + c * 128:512 + (c + 1) * 128], rhs=hT[:, kk, g * 512:(g + 1) * 512],
                    start=(kk == 0), stop=(kk == 7)), reads=[hT.b(g), W_A.b()], writes=[bt.b(c0)])
            k.op("act", lambda e, c=c, g=g, bt=bt, c0=c0: e.activation(
                out=gl[:, c, g * 512:(g + 1) * 512], in_=bt[:, c0:c0 + 512], func=AF.Gelu_apprx_tanh),
                reads=[bt.b(c0)], writes=[gl.b(c)])

    def lru_conv(c):
        xc = xc2[c % 2]
        xcb = xcb2[c % 2]
        cw = lambda j: cols[:, 72 + c * 4 + j:73 + c * 4 + j]
        k.op("dve", lambda e: e.tensor_scalar(out=xc[:], in0=xl[:, c, 4:4 + S], scalar1=cw(3), scalar2=cols[:, 88 + c:89 + c],
                                              op0=ALU.mult, op1=ALU.add),
             reads=[xl.b(c), xl.b("pad"), cols.b()], writes=[xc.b()])
        for j in range(3):
            sh = 3 - j
            k.op("dve", lambda e, j=j, sh=sh: e.scalar_tensor_tensor(
                out=xc[:], in0=xl[:, c, 4 - sh:4 - sh + S], scalar=cw(j), in1=xc[:], op0=ALU.mult, op1=ALU.add),
                reads=[xl.b(c), xl.b("pad"), xc.b()], writes=[xc.b()])
        k.op("pool", lambda e: e.tensor_copy(out=xcb[:], in_=xc[:]), reads=[xc.b()], writes=[xcb.b()])

    def lru_gates(c):
        xcb = xcb2[c % 2]
        for gate, dstT, bcol in ((0, T1, 92), (1, T3, 96)):
            for g in range(4):
                bt, c0 = bank(4 + (gate * 4 + g) % 4)
                k.op("pe", lambda e, gate=gate, g=g, bt=bt, c0=c0: e.matmul(
                    bt[:, c0:c0 + 512], lhsT=BD[:, gate, c, :], rhs=xcb[:, g * 512:(g + 1) * 512], start=True, stop=True),
                    reads=[BD.b(), xcb.b()], writes=[bt.b(c0)])
                k.op("act", lambda e, g=g, bt=bt, c0=c0, dstT=dstT, bcol=bcol: e.activation(
                    out=dstT[:, g * 512:(g + 1) * 512], in_=bt[:, c0:c0 + 512], func=AF.Sigmoid,
                    bias=cols[:, bcol + c:bcol + c + 1]), reads=[bt.b(c0), cols.b()], writes=[dstT.b()])
        k.op("act", lambda e: e.activation(out=T2[:], in_=T1[:], func=AF.Exp, scale=lam[:, c:c + 1]),
             reads=[T1.b(), lam.b()], writes=[T2.b()])
        k.op("act", lambda e: e.activation(out=T1[:], in_=T1[:], func=AF.Exp, scale=lam[:, 4 + c:5 + c]),
             reads=[T1.b(), lam.b()], writes=[T1.b()])
        k.op("act", lambda e: e.activation(out=T1[:], in_=T1[:], func=AF.Sqrt, scale=-1.0, bias=1.0),
             reads=[T1.b()], writes=[T1.b()])

    def lru_rest(c):
        xc = xc2[c % 2]
        k.op("dve", lambda e: e.tensor_tensor(out=T3[:], in0=T3[:], in1=xc[:], op=ALU.mult),
             reads=[T3.b(), xc.b()], writes=[T3.b()])
        k.op("dve", lambda e: e.tensor_tensor(out=T3[:], in0=T3[:], in1=T1[:], op=ALU.mult),
             reads=[T3.b(), T1.b()], writes=[T3.b()])
        k.op("dve", lambda e: e.tensor_tensor_scan(out=T1[:], data0=T2[:], data1=T3[:], initial=0.0, op0=ALU.mult, op1=ALU.add),
             reads=[T2.b(), T3.b(), T1.b()], writes=[T1.b()])
        k.op("dve", lambda e: e.tensor_tensor(out=xl[:, c, 4:4 + S], in0=T1[:], in1=gl[:, c, :], op=ALU.mult),
             reads=[T1.b(), gl.b(c), xl.b(c)], writes=[xl.b(c)])

    wada2 = [k.alloc([128, 8, 1024], BF16, off=B0 + i * 16 * KB, name="wadaL%d" % i) for i in range(2)]
    cb2 = k.alloc([128, 8, 128], BF16, off=B0 + 32 * KB, name="cb2")
    csil2 = k.alloc([128, 8], F32, off=B0 + 34 * KB, name="csil2")
    brow2 = k.alloc([128, 2, D], F32, off=B0 + 36 * KB, name="brow2")

    def late_load(g):
        wb = wada2[g % 2]
        k.dma("pool", wb[:], wada_v[:, :, g * 1024:(g + 1) * 1024], writes=[wb.b()])

    def late_setup():
        for en in ("sp", "act", "dve", "pool"):
            k._wait(en, {"pe": k.cnt["pe"]})
        k.op("act", lambda e: e.activation(out=csil2[:], in_=cols[:, 0:8], func=AF.Silu), reads=[cols.b()], writes=[csil2.b()])
        for kk in range(8):
            k.op("dve", lambda e, kk=kk: e.tensor_copy(out=cb2[:, kk, :], in_=csil2[:, kk:kk + 1].to_broadcast([128, 128])),
                 reads=[csil2.b()], writes=[cb2.b()])
        k.dma("sp", brow2[:, 0, :], rows_d[0, :].partition_broadcast(128), writes=[brow2.b()])
        k.dma("sp", brow2[:, 1, :], rows_d[1, :].partition_broadcast(128), writes=[brow2.b()])
        late_load(2)
        late_load(3)

    def late_pe(g):
        wb = wada2[g % 2]
        pt = PS[1]
        adaln_cols(g, wb, cb2, pt, 0, pt.b(0))
        k.op("dve", lambda e: e.tensor_tensor(out=modT[:, 8 * g:8 * g + 8], in0=pt[:, 0:8], in1=cols[:, 8 + 8 * g:16 + 8 * g], op=ALU.add),
             reads=[pt.b(0), cols.b()], writes=[modT.b()])
        if g in (2, 5):
            adaln_gate(g, wb, cb2, brow2)

    lru_inproj(0)
    lru_conv(0)
    lru_inproj(1)
    for c in range(4):
        lru_gates(c)
        if c >= 2:
            late_pe(c)
            late_load(c + 2)
        if c + 1 < 4:
            lru_conv(c + 1)
        if c + 2 < 4:
            lru_inproj(c + 2)
            if c + 2 == 3:
                late_setup()
        lru_rest(c)
    late_pe(4)
    late_pe(5)
    adaln_AS(16, 64, 32, 24)
    dbg("modT", modT[:], [128, 48], [modT.b()])
    dbg("Grow_m", Grow_m[:], [128, D], [Grow_m.b()])
    dbg("lru", xl[:], [128, 4, S + 4], [xl.b(c) for c in range(4)])
    for g in range(4):
        bt, c0 = bank(g % 2)
        for c in range(4):
            sqt = sq[c % 2]
            k.op("act", lambda e, c=c, g=g, sqt=sqt: e.activation(out=sqt[:], in_=xl[:, c, 4 + g * 512:4 + (g + 1) * 512], func=AF.Square),
                 reads=[xl.b(c)], writes=[sqt.b()])
            k.op("pe", lambda e, c=c, bt=bt, c0=c0, sqt=sqt: e.matmul(bt[:, c0:c0 + 512], lhsT=onesf[:], rhs=sqt[:],
                                                                      start=(c == 0), stop=(c == 3)),
                 reads=[sqt.b(), onesf.b()], writes=[bt.b(c0)])
        rt = rs[g % 2]
        k.op("act", lambda e, bt=bt, c0=c0, rt=rt: e.activation(out=rt[:], in_=bt[:, c0:c0 + 512], func=AF.Sqrt, scale=1.0 / 512, bias=EPS),
             reads=[bt.b(c0)], writes=[rt.b()])
        k.op("dve", lambda e, rt=rt: e.reciprocal(out=rt[:], in_=rt[:]), reads=[rt.b()], writes=[rt.b()])
        for c in range(4):
            k.op("dve", lambda e, c=c, g=g, rt=rt: e.scalar_tensor_tensor(
                out=merged[:, c, g * 512:(g + 1) * 512], in0=xl[:, c, 4 + g * 512:4 + (g + 1) * 512],
                scalar=cols[:, 104 + c:105 + c], in1=rt[:], op0=ALU.mult, op1=ALU.mult),
                reads=[xl.b(c), rt.b(), cols.b()], writes=[merged.b(("l", g))])
    dbg("merged", merged[:], [128, 8, S], [merged.b(("l", g)) for g in range(4)] + [merged.b(("a", g)) for g in range(4)])
    k.barrier()

    if stop_after == 'D':
        k.barrier()
        return nc, stack, dbg_out
    h2T = k.alloc([128, 8, S], BF16, off=B0, name="h2T")
    wts = k.alloc([128, NT, NE], F32, off=B0 + 32 * KB, name="wts")
    yacc = k.alloc([128, NT, D], F32, off=B0 + 34 * KB, name="yacc")
    M0 = B0 + 98 * KB
    o = M0
    w_out = k.alloc([128, 8, D], BF16, off=o, name="w_out"); o += 16 * KB
    xr = [k.alloc([128, D], F32, off=o + i * 4 * KB, name="xr%d" % i) for i in range(3)]; o += 12 * KB
    tt_ = [k.alloc([128, D], F32, off=o + i * 4 * KB, name="tt%d" % i) for i in range(2)]; o += 8 * KB
    x2t = [k.alloc([128, D], F32, off=o + i * 4 * KB, name="x2t%d" % i) for i in range(4)]; o += 16 * KB
    hf = k.alloc([128, 8, 512], F32, off=o, name="hf"); o += 16 * KB
    wrt = k.alloc([128, 8, NE], F32, off=o, name="wrt"); o += KB
    lg = k.alloc([128, 4, 128], F32, off=o, name="lg"); o += 2 * KB
    xn = [k.alloc([128, D], F32, off=o + i * 4 * KB, name="xnb%d" % i) for i in range(4)]; o += 16 * KB
    junk2b = [k.alloc([128, D], BF16, off=o + i * 2 * KB, name="junkb%d" % i) for i in range(2)]; o += 4 * KB
    st3 = k.alloc([128, NT * 8], F32, off=o, name="st3"); o += 512
    assert o <= SB_TOP, o

    k.dma("pool", w_out[:], wout_d.rearrange("(k p) n -> p k n", p=128), writes=[w_out.b()])
    k.dma("sp", wrt[:], wr_d.rearrange("(k p) n -> p k n", p=128), writes=[wrt.b()])

    def ef_load(i):
        xt = xr[i % 3]
        k.dma("sp", xt[:], x_d[i * 128:(i + 1) * 128, :], writes=[xt.b()])

    def ef_s0(i):
        g = i // 4
        yt = PS[1 + i % 2]
        for hh in range(2):
            for kk in range(8):
                k.op("pe", lambda e, i=i, hh=hh, kk=kk, yt=yt: e.matmul(
                    yt[:, hh * 512:(hh + 1) * 512], lhsT=merged[:, kk, i * 128:(i + 1) * 128],
                    rhs=w_out[:, kk, hh * 512:(hh + 1) * 512], start=(kk == 0), stop=(kk == 7)),
                    reads=[merged.b(("l", g)), merged.b(("a", g)), w_out.b()], writes=[yt.b("y")])

    def ef_s1(i):
        xt = xr[i % 3]
        yt = PS[1 + i % 2]
        sc = st3[:, i * 8:i * 8 + 4]
        sb = st3.b(("y", i))
        jk = junk2b[i % 2]
        k.op("act", lambda e, yt=yt, sc=sc, jk=jk: e.activation(out=jk[:], in_=yt[:, :], func=AF.Square, accum_out=sc[:, 0:1]),
             reads=[yt.b("y")], writes=[jk.b(), sb])
        k.op("act", lambda e, sc=sc: e.activation(out=sc[:, 1:2], in_=sc[:, 0:1], func=AF.Sqrt, scale=1.0 / D, bias=EPS),
             reads=[sb], writes=[sb])
        k.op("dve", lambda e, sc=sc: e.reciprocal(out=sc[:, 2:3], in_=sc[:, 1:2]), reads=[sb], writes=[sb])
        tq = tt_[i % 2]
        k.op("dve", lambda e, yt=yt, sc=sc, tq=tq: e.scalar_tensor_tensor(
            out=tq[:], in0=yt[:, :], scalar=sc[:, 2:3], in1=Grow_m[:], op0=ALU.mult, op1=ALU.mult),
            reads=[yt.b("y"), sb, Grow_m.b()], writes=[tq.b()])
        x2 = x2t[i % 4]
        k.op("pool", lambda e, tq=tq, xt=xt, x2=x2: e.tensor_tensor(out=x2[:], in0=tq[:], in1=xt[:], op=ALU.add),
             reads=[tq.b(), xt.b()], writes=[x2.b()])
        k.dma("sp", x2_d[i * 128:(i + 1) * 128, :], x2[:], reads=[x2.b()], writes=[x2sc.b(i)])

    def ef_s2(i):
        x2 = x2t[i % 4]
        sc = st3[:, i * 8 + 4:i * 8 + 8]
        sb = st3.b(("x", i))
        jk = junk2b[i % 2]
        k.op("act", lambda e, x2=x2, sc=sc, jk=jk: e.activation(out=jk[:], in_=x2[:], func=AF.Square, accum_out=sc[:, 0:1]),
             reads=[x2.b()], writes=[jk.b(), sb])
        k.op("act", lambda e, sc=sc: e.activation(out=sc[:, 1:2], in_=sc[:, 0:1], func=AF.Sqrt, scale=1.0 / D, bias=EPS),
             reads=[sb], writes=[sb])
        k.op("dve", lambda e, sc=sc: e.reciprocal(out=sc[:, 2:3], in_=sc[:, 1:2]), reads=[sb], writes=[sb])
        k.op("dve", lambda e, x2=x2, sc=sc, i=i: e.tensor_scalar(out=xn[i % 4][:], in0=x2[:], scalar1=sc[:, 2:3],
                                                                  scalar2=None, op0=ALU.mult),
             reads=[x2.b(), sb], writes=[xn[i % 4].b()])

    def ef_s3(g):
        a0 = 16
        for kk in range(8):
            bt, c0 = bank(kk % 2)
            for tt in range(4):
                k.op("pe", lambda e, kk=kk, tt=tt, bt=bt, c0=c0: e.transpose(
                    out=bt[:, c0 + tt * 128:c0 + (tt + 1) * 128], in_=xn[tt][:, kk * 128:(kk + 1) * 128], identity=identf[:]),
                    reads=[xn[tt].b(), identf.b()], writes=[bt.b(kk % 2)])
            k.op("dve", lambda e, kk=kk, bt=bt, c0=c0: e.tensor_scalar(
                out=hf[:, kk, :], in0=bt[:, c0:c0 + 512],
                scalar1=AS[:, a0 + kk:a0 + kk + 1], scalar2=AS[:, a0 + 8 + kk:a0 + 9 + kk], op0=ALU.mult, op1=ALU.add),
                reads=[bt.b(kk % 2), AS.b()], writes=[hf.b()])
            k.op("act", lambda e, kk=kk: e.activation(out=h2T[:, kk, g * 512:(g + 1) * 512], in_=hf[:, kk, :], func=AF.Copy),
                 reads=[hf.b()], writes=[h2T.b(g)])

    def ef_s4(g):
        for tt in range(4):
            i = g * 4 + tt
            lt, lc0 = bank(6 + tt % 2)
            for kk in range(8):
                k.op("pe", lambda e, tt=tt, kk=kk, lt=lt, lc0=lc0: e.matmul(
                    lt[:, lc0:lc0 + NE], lhsT=hf[:, kk, tt * 128:(tt + 1) * 128], rhs=wrt[:, kk, :],
                    start=(kk == 0), stop=(kk == 7)), reads=[hf.b(), wrt.b()], writes=[lt.b(lc0)])
            L_ = lg[:, tt, 0:32]
            X_ = lg[:, tt, 32:48]
            M_ = lg[:, tt, 48:80]
            E_ = lg[:, tt, 80:112]
            lb = lg.b(tt)
            k.op("dve", lambda e, lt=lt, lc0=lc0, L_=L_: e.tensor_tensor(out=L_, in0=lt[:, lc0:lc0 + NE], in1=brout[:], op=ALU.add),
                 reads=[lt.b(lc0), brout.b()], writes=[lb])
            k.op("dve", lambda e, L_=L_, X_=X_: e.max(out=X_[:, 0:8], in_=L_), reads=[lb], writes=[lb])
            k.op("dve", lambda e, X_=X_: e.tensor_scalar(out=X_[:, 8:9], in0=X_[:, 0:1], scalar1=-1.0, scalar2=None, op0=ALU.mult),
                 reads=[lb], writes=[lb])
            k.op("dve", lambda e, L_=L_, X_=X_, M_=M_: e.tensor_scalar(out=M_, in0=L_, scalar1=X_[:, 3:4], scalar2=None, op0=ALU.is_ge),
                 reads=[lb], writes=[lb])
            k.op("act", lambda e, L_=L_, X_=X_, E_=E_: e.activation(out=E_, in_=L_, func=AF.Exp, bias=X_[:, 8:9]),
                 reads=[lb], writes=[lb])
            k.op("dve", lambda e, E_=E_, M_=M_, X_=X_: e.scalar_tensor_tensor(
                out=E_, in0=E_, scalar=1.0, in1=M_, op0=ALU.mult, op1=ALU.mult, accum_out=X_[:, 9:10]),
                reads=[lb], writes=[lb])
            k.op("dve", lambda e, X_=X_: e.reciprocal(out=X_[:, 10:11], in_=X_[:, 9:10]), reads=[lb], writes=[lb])
            k.op("dve", lambda e, E_=E_, X_=X_, i=i: e.tensor_scalar(out=wts[:, i, :], in0=E_, scalar1=X_[:, 10:11], scalar2=None, op0=ALU.mult),
                 reads=[lb], writes=[wts.b(i)])

    ef_load(0)
    ef_load(1)
    for step in range(NT + 5):
        if step < NT:
            ef_s0(step)
        if 0 <= step - 1 < NT:
            ef_s1(step - 1)
        if step + 2 < NT:
            ef_load(step + 2)
        t3 = step - 3
        if 0 <= t3 < NT and t3 % 4 == 3:
            ef_s3(t3 // 4)
        t4 = step - 4
        if 0 <= t4 < NT and t4 % 4 == 3:
            ef_s4(t4 // 4)
        if 0 <= step - 2 < NT:
            ef_s2(step - 2)
    dbg("h2T", h2T[:], [128, 8, S], [h2T.b(g) for g in range(4)])
    dbg("wts", wts[:], [128, NT, NE], [wts.b(i) for i in range(NT)])
    k.barrier()

    if stop_after == 'EF':
        k.barrier()
        return nc, stack, dbg_out
    o = M0
    actb = [k.alloc([128, 4, S], BF16, off=o + i * 16 * KB, name="act%d" % i) for i in range(2)]; o += 32 * KB
    wup = [k.alloc([128, 8, 2, 512], BF16, off=o + i * 16 * KB, name="wup%d" % i) for i in range(2)]; o += 32 * KB
    wdn = [k.alloc([128, 4, D], BF16, off=o + i * 8 * KB, name="wdn%d" % i) for i in range(2)]; o += 16 * KB
    TG_OFF = o
    tG = [k.alloc([128, 512], F32, off=o + i * 2 * KB, name="tG%d" % i) for i in range(2)]; o += 4 * KB
    tS = [k.alloc([128, 512], BF16, off=o + i * KB, name="tS%d" % i) for i in range(2)]; o += 2 * KB
    TL_OFF = o
    tL = [k.alloc([128, 512], F32, off=o + i * 2 * KB, name="tL%d" % i) for i in range(2)]; o += 4 * KB
    wT = k.alloc([32, 128], F32, off=o, name="wT"); o += 512
    bdn = k.alloc([32, D], BF16, off=o, name="bdn"); o += 4 * KB
    assert o <= SB_TOP, (o, SB_TOP)
    MOE_END = o

    NP = NE * 2 if n_experts_dbg is None else n_experts_dbg * 2

    def load_up(n):
        e_, p_ = n // 2, n % 2
        src = wup_d[e_].rearrange("(k p) (t f) -> p k t f", p=128, t=2)
        for t in range(2):
            k.dma("pool", wup[n % 2][:, :, t, :], src[:, :, t, p_ * 512:(p_ + 1) * 512], writes=[wup[n % 2].b()])

    def load_dn(n):
        e_, p_ = n // 2, n % 2
        k.dma("pool", wdn[n % 2][:], wdn_d[e_, p_ * 512:(p_ + 1) * 512, :].rearrange("(kk p) d -> p kk d", p=128),
              writes=[wdn[n % 2].b()])

    load_up(0)
    load_dn(0)
    k.dma("pool", bdn[:], bdn_d[:, :], writes=[bdn.b()])
    wTall = [k.alloc([32, 1024], BF16, off=TG_OFF, name="wTa0"),
             k.alloc([32, 1024], BF16, off=TL_OFF, name="wTa1")]
    for q4 in range(4):
        bt, c0 = bank(q4 % 2)
        for t4 in range(4):
            tt = q4 * 4 + t4
            k.op("pe", lambda e, tt=tt, t4=t4, bt=bt, c0=c0: e.transpose(
                out=bt[0:32, c0 + t4 * 128:c0 + (t4 + 1) * 128], in_=wts[:, tt, :], identity=identf[:]),
                reads=[wts.b(tt), identf.b()], writes=[bt.b(c0)])
        wt_ = wTall[q4 // 2]
        k.op("act", lambda e, bt=bt, c0=c0, wt_=wt_, q4=q4: e.activation(
            out=wt_[:, (q4 % 2) * 512:(q4 % 2 + 1) * 512], in_=bt[0:32, c0:c0 + 512], func=AF.Copy),
            reads=[bt.b(c0)], writes=[wt_.b(q4 % 2)])
    for tt in range(NT):
        wt_ = wTall[tt // 8]
        cc = (tt % 8) * 128
        yt = PS[1 + tt % 3]
        for hh in range(2):
            k.op("pe", lambda e, hh=hh, yt=yt, wt_=wt_, cc=cc: e.matmul(
                yt[:, hh * 512:(hh + 1) * 512], lhsT=wt_[:, cc:cc + 128], rhs=bdn[:, hh * 512:(hh + 1) * 512],
                start=True, stop=True),
                reads=[wt_.b((tt % 8) // 4), bdn.b()], writes=[yt.b("y")])
        k.op("act", lambda e, tt=tt, yt=yt: e.activation(out=yacc[:, tt, :], in_=yt[:, :], func=AF.Copy),
             reads=[yt.b("y")], writes=[yacc.b(tt)])
    k.barrier(engines=("pe", "dve", "act"))

    unit = [0]
    vcnt = [0]

    def up(n):
        e_, p_ = n // 2, n % 2
        wb = wup[n % 2]
        ab = actb[n % 2]
        for j in range(4):
            cg = 117 + e_ * 16 + 4 * p_ + j
            cl = e_ * 16 + 8 + 4 * p_ + j
            for tg in range(4):
                u = unit[0]; unit[0] += 1
                gt, gc0 = bank((u % 2) * 2)
                lt, lc0 = bank((u % 2) * 2 + 1)
                for kk in range(8):
                    k.op("pe", lambda e, kk=kk, j=j, tg=tg, gt=gt, gc0=gc0, wb=wb: e.matmul(
                        gt[:, gc0:gc0 + 512], lhsT=wb[:, kk, 0, j * 128:(j + 1) * 128], rhs=h2T[:, kk, tg * 512:(tg + 1) * 512],
                        start=(kk == 0), stop=(kk == 7)), reads=[wb.b(), h2T.b(tg)], writes=[gt.b(("u", gc0))])
                for kk in range(8):
                    k.op("pe", lambda e, kk=kk, j=j, tg=tg, lt=lt, lc0=lc0, wb=wb: e.matmul(
                        lt[:, lc0:lc0 + 512], lhsT=wb[:, kk, 1, j * 128:(j + 1) * 128], rhs=h2T[:, kk, tg * 512:(tg + 1) * 512],
                        start=(kk == 0), stop=(kk == 7)), reads=[wb.b(), h2T.b(tg)], writes=[lt.b(("u", lc0))])
                g_, s_, l_ = tG[u % 2], tS[u % 2], tL[u % 2]
                k.op("dve", lambda e, gt=gt, gc0=gc0, g_=g_, cg=cg: e.tensor_scalar(
                    out=g_[:], in0=gt[:, gc0:gc0 + 512], scalar1=cols[:, cg:cg + 1], scalar2=7.0, op0=ALU.add, op1=ALU.min),
                    reads=[gt.b(("u", gc0)), cols.b()], writes=[g_.b()])
                k.op("act", lambda e, g_=g_, s_=s_: e.activation(out=s_[:], in_=g_[:], func=AF.Gelu_apprx_sigmoid),
                     reads=[g_.b()], writes=[s_.b()])
                k.op("dve", lambda e, lt=lt, lc0=lc0, l_=l_, cl=cl: e.tensor_scalar(
                    out=l_[:], in0=lt[:, lc0:lc0 + 512], scalar1=b1c[:, cl:cl + 1], scalar2=8.0, op0=ALU.add, op1=ALU.min),
                    reads=[lt.b(("u", lc0)), b1c.b()], writes=[l_.b()])
                k.op("dve", lambda e, l_=l_, s_=s_, ab=ab, j=j, tg=tg: e.scalar_tensor_tensor(
                    out=ab[:, j, tg * 512:(tg + 1) * 512], in0=l_[:], scalar=-6.0, in1=s_[:], op0=ALU.max, op1=ALU.mult),
                    reads=[l_.b(), s_.b()], writes=[ab.b(tg)])

    FO = M0 + 32 * KB
    xf = [k.alloc([128, D], F32, off=FO + i * 4 * KB, name="xf%d" % i) for i in range(3)]
    tqf = [k.alloc([128, D], F32, off=FO + 12 * KB + i * 4 * KB, name="tf%d" % i) for i in range(2)]
    otf = [k.alloc([128, D], F32, off=FO + 20 * KB + i * 4 * KB, name="of%d" % i) for i in range(2)]
    junkf = [k.alloc([128, D], BF16, off=FO + 28 * KB + i * 2 * KB, name="junkf%d" % i) for i in range(2)]
    st3f = k.alloc([128, NT * 4], F32, off=MOE_END, name="st3f")
    assert MOE_END + 256 <= SB_TOP

    def final_load(i):
        k.dma("sp", xf[i % 3][:], x2_d[i * 128:(i + 1) * 128, :], reads=[x2sc.b(i)], writes=[xf[i % 3].b()])

    def final_tile(i):
        xt = xf[i % 3]
        sc = st3f[:, i * 4:i * 4 + 4]
        sb = st3f.b(i)
        jk = junkf[i % 2]
        k.op("act", lambda e, i=i, sc=sc, jk=jk: e.activation(out=jk[:], in_=yacc[:, i, :], func=AF.Square, accum_out=sc[:, 0:1]),
             reads=[yacc.b(i)], writes=[jk.b(), sb])
        k.op("act", lambda e, sc=sc: e.activation(out=sc[:, 1:2], in_=sc[:, 0:1], func=AF.Sqrt, scale=1.0 / D, bias=EPS),
             reads=[sb], writes=[sb])
        k.op("dve", lambda e, sc=sc: e.reciprocal(out=sc[:, 2:3], in_=sc[:, 1:2]), reads=[sb], writes=[sb])
        tq = tqf[i % 2]
        k.op("dve", lambda e, i=i, sc=sc, tq=tq: e.scalar_tensor_tensor(
            out=tq[:], in0=yacc[:, i, :], scalar=sc[:, 2:3], in1=Grow_f[:], op0=ALU.mult, op1=ALU.mult),
            reads=[yacc.b(i), sb, Grow_f.b()], writes=[tq.b()])
        ot = otf[i % 2]
        k.op("pool", lambda e, tq=tq, xt=xt, ot=ot: e.tensor_tensor(out=ot[:], in0=tq[:], in1=xt[:], op=ALU.add),
             reads=[tq.b(), xt.b()], writes=[ot.b()])
        k.dma("sp", out_d[i * 128:(i + 1) * 128, :], ot[:], reads=[ot.b()])
        if i + 2 < NT:
            final_load(i + 2)

    def down(n):
        e_, p_ = n // 2, n % 2
        wb = wdn[n % 2]
        ab = actb[n % 2]
        last = (n == NP - 1)
        if last:
            for en in ("sp", "act", "dve", "pool"):
                k._wait(en, {"pe": k.cnt["pe"]})
            final_load(0)
            final_load(1)
        for tt in range(NT):
            v = vcnt[0]; vcnt[0] += 1
            yt = PS[2 + v % 2]
            for hh in range(2):
                for kk in range(4):
                    k.op("pe", lambda e, tt=tt, hh=hh, kk=kk, yt=yt, wb=wb, ab=ab: e.matmul(
                        yt[:, hh * 512:(hh + 1) * 512], lhsT=ab[:, kk, tt * 128:(tt + 1) * 128], rhs=wb[:, kk, hh * 512:(hh + 1) * 512],
                        start=(kk == 0), stop=(kk == 3)), reads=[ab.b(tt // 4), wb.b()], writes=[yt.b("d")])
            k.op("dve", lambda e, tt=tt, yt=yt, e_=e_: e.scalar_tensor_tensor(
                out=yacc[:, tt, :], in0=yt[:, :], scalar=wts[:, tt, e_:e_ + 1], in1=yacc[:, tt, :], op0=ALU.mult, op1=ALU.add),
                reads=[yt.b("d"), wts.b(tt), yacc.b(tt)], writes=[yacc.b(tt)])
            if last:
                final_tile(tt)

    for n in range(NP + 1):
        if n < NP:
            if n + 1 < NP:
                load_up(n + 1)
            up(n)
        if n >= 1:
            down(n - 1)
        if n + 1 < NP:
            load_dn(n + 1)
    dbg("yacc", yacc[:], [128, NT, D], [yacc.b(i) for i in range(NT)])
    k.barrier()
    return nc, stack, dbg_out


def _prep_shared(inp):
    f = np.float32
    sh = {}
    sh["w_ada"] = np.ascontiguousarray(inp["w_ada"][0], f)
    sh["w_in"] = np.ascontiguousarray(inp["w_in"][0], f)
    sh["w_out"] = np.ascontiguousarray(inp["w_out"][0], f)
    sh["w_router"] = np.ascontiguousarray(inp["w_router"][0], f)
    sh["w_up"] = np.ascontiguousarray(inp["w_up"][0], f)
    sh["w_down"] = np.ascontiguousarray(inp["w_down"][0], f)
    sh["b_down"] = np.ascontiguousarray(inp["b_down"][0], f)
    sh["b_router"] = np.ascontiguousarray(inp["b_router"][0].reshape(1, NE), f)
    b_ada = inp["b_ada"][0]
    rows = np.zeros((4, D), f)
    rows[0] = b_ada[2 * D:3 * D]
    rows[1] = b_ada[5 * D:6 * D]
    rows[2] = inp["norm_mix_post"][0]
    rows[3] = inp["norm_ffn_post"][0]
    sh["rows"] = rows
    bd = np.zeros((128, 2, 4, 128), f)
    for a, w in enumerate((inp["lru_wa"][0], inp["lru_wx"][0])):
        for c in range(4):
            for hlf in range(2):
                bd[hlf * 64:(hlf + 1) * 64, a, c, hlf * 64:(hlf + 1) * 64] = w[2 * c + hlf]
    sh["bdiag"] = bd.reshape(128, -1)
    sel = np.zeros((97, 2, 8, 70), f)
    for h in range(8):
        sel[h, 0, h, 64] = 8.0
        sel[32 + h, 0, h, 65] = 8.0
        sel[64 + h, 0, h, 66] = 8.0
        sel[96, 0, h, 67:70] = 8.0
        sel[96, 1, h, 64:67] = 1.0
        sel[h, 1, h, 67] = -1.0
        sel[32 + h, 1, h, 68] = -1.0
        sel[64 + h, 1, h, 69] = -1.0
    sh["sel"] = sel.reshape(97, -1)
    cols = np.zeros((128, NCOL), f)
    colT = lambda v: np.asarray(v, f).reshape(-1, 128).T
    cols[:, 8:56] = colT(b_ada)
    cols[:, 56:64] = colT(inp["norm_mix_pre"][0])
    cols[:, 64:72] = colT(inp["norm_ffn_pre"][0])
    cw = inp["conv_w"][0]
    for c in range(4):
        for j in range(4):
            cols[:, 72 + c * 4 + j] = cw[j, c * 128:(c + 1) * 128]
    cols[:, 88:92] = colT(inp["conv_b"][0])
    cols[:, 92:96] = colT(inp["lru_ba"][0].reshape(-1))
    cols[:, 96:100] = colT(inp["lru_bx"][0].reshape(-1))
    cols[:, 100:104] = colT(inp["lru_lambda"][0])
    cols[:, 104:108] = colT(inp["gn_lru"][0])
    cols[:, 108:112] = colT(inp["gn_attn"][0])
    fb = inp["attn_fb"][0]
    for r0 in (0, 32, 64):
        cols[r0:r0 + 8, 112] = fb
    cols[0:8, 113] = 1.0
    cols[32:40, 114] = 1.0
    cols[64:72, 115] = 1.0
    cols[96, 116] = 1.0
    bu = inp["b_up"][0]
    cols[:, 117:629] = bu.reshape(NE, 16, 128).transpose(2, 0, 1).reshape(128, NE * 16)
    return sh, cols


_CACHE = {}


def kernel(**inputs):
    inp = {k_: np.asarray(v) for k_, v in inputs.items()}
    n = 8
    sh, cols0 = _prep_shared(inp)
    if "prog" not in _CACHE:
        _CACHE["prog"] = build_program()
    nc, stack, _ = _CACHE["prog"]
    in_maps = []
    for b in range(n):
        m = dict(sh)
        m["x"] = np.ascontiguousarray(inp["x"][b], np.float32)
        cols = cols0.copy()
        cols[:, 0:8] = np.asarray(inp["c"][b], np.float32).reshape(8, 128).T
        m["cols"] = cols
        in_maps.append(m)
    res = run_bass_kernel_spmd(nc, in_maps, core_ids=list(range(n)))
    out = np.stack([np.asarray(r["out"], np.float32) for r in res.results], axis=0)
    return out
```

```python
import os
import numpy as np
import concourse.bass as bass
import concourse.mybir as mybir
from concourse.bass_utils import run_bass_kernel_spmd

F32 = mybir.dt.float32
BF16 = mybir.dt.bfloat16
AF = mybir.ActivationFunctionType
ALU = mybir.AluOpType

D = 1024
S = 2048
NT = 16
NE = 32
EPS = 1e-6
SB_BASE = 16512
SB_TOP = 229344
SAME_SYNC = True
KSKIP = os.environ.get('KSKIP', '').split(',')
NCOL = 629
DT_SIZE = {F32: 4, BF16: 2}


class Buf:
    __slots__ = ("w", "r")

    def __init__(self):
        self.w = None
        self.r = {}


class Tile:
    def __init__(self, h):
        self.h = h
        self.bufs = {}

    def ap(self):
        return self.h.ap()

    def __getitem__(self, key):
        return self.h.ap()[key]

    def b(self, key=None):
        if key not in self.bufs:
            self.bufs[key] = Buf()
        return self.bufs[key]


class K:
    def __init__(self, nc, stack):
        self.nc = nc
        self.eng = {"pe": nc.tensor, "act": nc.scalar, "dve": nc.vector, "pool": nc.gpsimd, "sp": nc.sync}
        self.sems = {}
        self.cnt = {}
        self.waited = {e: {} for e in self.eng}
        for e in self.eng:
            self.sems[e] = stack.enter_context(nc.semaphore("s_" + e))
            self.cnt[e] = 0
        self.R = 8
        self.dma_n = {}
        for q in ("sp", "pool", "act"):
            self.dma_n[q] = 0
            for i in range(self.R):
                self.sems[("ring", q, i)] = stack.enter_context(nc.semaphore("r_%s_%d" % (q, i)))
        self.off = SB_BASE
        self.names = 0

    def alloc(self, shape, dtype, off=None, name=None):
        size = int(np.prod(shape[1:])) * DT_SIZE[dtype]
        size = (size + 63) // 64 * 64
        if off is None:
            off = self.off
            self.off += size
        assert off + size <= SB_TOP, ("SBUF overflow", name, off, size)
        self.names += 1
        h = self.nc.alloc_sbuf_tensor_at("%s_%d" % (name or "t", self.names), list(shape), dtype, offset=off)
        return Tile(h)

    def _deps(self, reads, writes):
        deps = {}

        def add(tok):
            if tok is None:
                return
            k, v = tok
            if deps.get(k, 0) < v:
                deps[k] = v

        for b in reads:
            add(b.w)
        for b in writes:
            add(b.w)
            for k, v in b.r.items():
                add((k, v))
        return deps

    def _wait(self, e, deps):
        w = self.waited[e]
        for k, v in deps.items():
            if k == e and (not SAME_SYNC or e == "pe" or e == "sp"):
                continue
            if w.get(k, 0) < v:
                self.eng[e].wait_ge(self.sems[k], v)
                w[k] = v

    def _commit(self, tok, reads, writes):
        k, v = tok
        for b in writes:
            b.w = tok
            b.r = {}
        for b in reads:
            if b.r.get(k, 0) < v:
                b.r[k] = v

    def op(self, e, fn, reads=(), writes=()):
        self._wait(e, self._deps(reads, writes))
        ins = fn(self.eng[e])
        self.cnt[e] += 1
        ins.then_inc(self.sems[e], 1)
        self._commit((e, self.cnt[e]), reads, writes)

    def dma(self, q, out, in_, reads=(), writes=(), **kw):
        n = self.dma_n[q]
        slot = n % self.R
        val = 16 * (n // self.R + 1)
        key = ("ring", q, slot)
        deps = self._deps(reads, writes)
        if val > 16:
            deps[key] = max(deps.get(key, 0), val - 16)
        self._wait(q, deps)
        ins = self.eng[q].dma_start(out=out, in_=in_, **kw)
        ins.then_inc(self.sems[key], 16)
        self.dma_n[q] = n + 1
        self._commit((key, val), reads, writes)

    def all_tokens(self):
        toks = {}
        for e in self.eng:
            if self.cnt[e] > 0:
                toks[e] = self.cnt[e]
        for q, n in self.dma_n.items():
            for i in range(min(n, self.R)):
                last = n - 1 - ((n - 1 - i) % self.R)
                toks[("ring", q, i)] = 16 * (last // self.R + 1)
        return toks

    def barrier(self, engines=("pe", "act", "dve", "pool", "sp")):
        toks = self.all_tokens()
        for e in engines:
            w = self.waited[e]
            for k, v in toks.items():
                if k == e:
                    continue
                if w.get(k, 0) < v:
                    self.eng[e].wait_ge(self.sems[k], v)
                    w[k] = v


def build_program(debug=(), n_experts_dbg=None, stop_after=None):
    from contextlib import ExitStack
    nc = bass.Bass("TRN2", target_bir_lowering=False)
    stack = ExitStack()

    def din(name, shape, dt=F32):
        return nc.dram_tensor(name, list(shape), dt, kind="ExternalInput").ap()

    x_d = din("x", [S, D])
    cols_d = din("cols", [128, NCOL])
    rows_d = din("rows", [4, D])
    brout_d = din("b_router", [1, NE])
    wada_d = din("w_ada", [D, 6 * D])
    win_d = din("w_in", [D, 2568])
    bd_d = din("bdiag", [128, 2 * 4 * 128])
    sel_d = din("sel", [97, 2 * 8 * 70])
    wout_d = din("w_out", [D, D])
    wr_d = din("w_router", [D, NE])
    wup_d = din("w_up", [NE, D, 2 * D])
    wdn_d = din("w_down", [NE, D, D])
    bdn_d = din("b_down", [NE, D])
    out_d = nc.dram_tensor("out", [S, D], F32, kind="ExternalOutput").ap()
    x2_d = nc.dram_tensor("x2_scratch", [S, D], F32, kind="ExternalOutput").ap()
    x2sc = Tile(None)
    dbg_out = {}

    k = K(nc, stack)
    KB = 1024

    PS = [Tile(stack.enter_context(nc.psum_tensor("ps%d" % i, [128, 1024], F32))) for i in range(4)]

    def bank(i):
        return PS[i // 2], (i % 2) * 512

    identf = k.alloc([128, 128], F32, name="identf")
    identb = k.alloc([128, 128], BF16, name="identb")
    onesf = k.alloc([128, 128], F32, name="onesf")
    maskT = k.alloc([128, 128], BF16, name="maskT")
    cols = k.alloc([128, NCOL], F32, name="cols")
    modT = k.alloc([128, 48], F32, name="modT")
    AS = k.alloc([128, 32], F32, name="AS")
    lam = k.alloc([128, 16], F32, name="lam")
    b1c = k.alloc([128, 512], F32, name="b1c")
    Grow_m = k.alloc([128, D], F32, name="Grow_m")
    Grow_f = k.alloc([128, D], F32, name="Grow_f")
    brout = k.alloc([128, NE], F32, name="brout")
    P_END = k.off
    B0 = P_END

    def dbg(name, tile_ap, shape, reads):
        if name not in debug:
            return
        d = nc.dram_tensor("dbg_" + name, list(shape), tile_ap.dtype, kind="ExternalOutput").ap()
        dbg_out[name] = d
        k.dma("sp", d, tile_ap, reads=reads)

    k.op("pool", lambda e: e.memset(identf[:], 1.0), writes=[identf.b()])
    k.op("pool", lambda e: e.affine_select(out=identf[:], in_=identf[:], pattern=[[1, 128]],
                                           compare_op=ALU.is_equal, fill=0.0, base=0, channel_multiplier=-1),
         reads=[identf.b()], writes=[identf.b()])
    k.op("pool", lambda e: e.tensor_copy(out=identb[:], in_=identf[:]), reads=[identf.b()], writes=[identb.b()])
    k.op("pool", lambda e: e.memset(onesf[:], 1.0), writes=[onesf.b()])
    k.op("pool", lambda e: e.memset(maskT[:], 0.0), writes=[maskT.b()])
    k.op("pool", lambda e: e.affine_select(out=maskT[:], in_=maskT[:], pattern=[[1, 128]],
                                           compare_op=ALU.is_ge, fill=-30000.0, base=0, channel_multiplier=-1),
         reads=[maskT.b()], writes=[maskT.b()])
    k.dma("sp", cols[:], cols_d[:, :], writes=[cols.b()])
    k.dma("sp", brout[:], brout_d[0, :].partition_broadcast(128), writes=[brout.b()])
    k.dma("sp", Grow_m[:], rows_d[2, :].partition_broadcast(128), writes=[Grow_m.b()])
    k.dma("sp", Grow_f[:], rows_d[3, :].partition_broadcast(128), writes=[Grow_f.b()])

    o = B0
    hT = k.alloc([128, 8, S], BF16, off=o, name="hT"); o += 32 * KB
    W_A = k.alloc([128, 8, 1024], BF16, off=o, name="W_A"); o += 16 * KB
    merged = k.alloc([128, 8, S], BF16, off=o, name="merged"); o += 32 * KB
    O_ATT = o
    W_QK = k.alloc([128, 8, 16, 70], BF16, off=o, name="W_QK"); o += 17920
    W_V = k.alloc([128, 8, 512], BF16, off=o, name="W_V"); o += 8 * KB
    W_F = k.alloc([128, 8, 97], BF16, off=o, name="W_F"); o += 1600
    vp = k.alloc([128, NT, 8, 65], BF16, off=o, name="vp"); o += 16640
    CP_OFF = o
    CP = k.alloc([97, S], BF16, off=o, name="CP"); o += 4 * KB
    SEL = k.alloc([97, 2, 8, 70], BF16, off=o, name="SEL"); o += 2240
    R0 = o

    win_v = win_d.rearrange("(k p) n -> p k n", p=128)
    k.dma("pool", SEL[:], sel_d.rearrange("p (a h c) -> p a h c", a=2, h=8), writes=[SEL.b()])

    MRG = B0 + 48 * KB
    xin4 = [k.alloc([128, D], F32, off=MRG + i * 4 * KB, name="xin4_%d" % i) for i in range(4)]
    xn = [k.alloc([128, D], F32, off=MRG + 16 * KB + i * 4 * KB, name="xn%d" % i) for i in range(4)]
    junk = k.alloc([128, D], BF16, off=CP_OFF, name="junk")
    st = k.alloc([128, 64], F32, off=CP_OFF + 2 * KB, name="st")

    def pre_B(g):
        for tt in range(4):
            i = g * 4 + tt
            xt = xin4[tt]
            k.dma("sp", xt[:], x_d[i * 128:(i + 1) * 128, :], writes=[xt.b()])
        for tt in range(4):
            xt = xin4[tt]
            sc = st[:, tt * 4:tt * 4 + 4]
            k.op("act", lambda e, xt=xt, sc=sc: e.activation(out=junk[:], in_=xt[:], func=AF.Square, accum_out=sc[:, 0:1]),
                 reads=[xt.b()], writes=[junk.b(), st.b(tt)])
            k.op("act", lambda e, sc=sc: e.activation(out=sc[:, 1:2], in_=sc[:, 0:1], func=AF.Sqrt, scale=1.0 / D, bias=EPS),
                 reads=[st.b(tt)], writes=[st.b(tt)])
            k.op("dve", lambda e, sc=sc: e.reciprocal(out=sc[:, 2:3], in_=sc[:, 1:2]), reads=[st.b(tt)], writes=[st.b(tt)])
            k.op("dve", lambda e, xt=xt, sc=sc, tt=tt: e.tensor_scalar(out=xn[tt][:], in0=xt[:], scalar1=sc[:, 2:3],
                                                                       scalar2=None, op0=ALU.mult),
                 reads=[xt.b(), st.b(tt)], writes=[xn[tt].b()])

    def post_B(g):
        a0 = 0
        for kk in range(8):
            bt, c0 = bank(kk % 2)
            for tt in range(4):
                k.op("pe", lambda e, kk=kk, tt=tt, bt=bt, c0=c0: e.transpose(
                    out=bt[:, c0 + tt * 128:c0 + (tt + 1) * 128], in_=xn[tt][:, kk * 128:(kk + 1) * 128], identity=identf[:]),
                    reads=[xn[tt].b(), identf.b()], writes=[bt.b(kk % 2)])
            k.op("dve", lambda e, kk=kk, bt=bt, c0=c0: e.tensor_scalar(
                out=hT[:, kk, g * 512:(g + 1) * 512], in0=bt[:, c0:c0 + 512],
                scalar1=AS[:, a0 + kk:a0 + kk + 1], scalar2=AS[:, a0 + 8 + kk:a0 + 9 + kk], op0=ALU.mult, op1=ALU.add),
                reads=[bt.b(kk % 2), AS.b()], writes=[hT.b(g)])

    pre_B(0)
    o = R0
    wada = [k.alloc([128, 8, 1024], BF16, off=o + i * 16 * KB, name="wada%d" % i) for i in range(3)]
    o += 48 * KB
    csil = k.alloc([128, 8], F32, off=o, name="csil"); o += 64
    cb = k.alloc([128, 8, 128], BF16, off=o, name="cb"); o += 2 * KB
    brow = k.alloc([128, 2, D], F32, off=o, name="brow"); o += 8 * KB

    k.op("act", lambda e: e.activation(out=csil[:], in_=cols[:, 0:8], func=AF.Silu), reads=[cols.b()], writes=[csil.b()])
    for kk in range(8):
        k.op("dve", lambda e, kk=kk: e.tensor_copy(out=cb[:, kk, :], in_=csil[:, kk:kk + 1].to_broadcast([128, 128])),
             reads=[csil.b()], writes=[cb.b()])
    wada_v = wada_d.rearrange("(k p) n -> p k n", p=128)
    pst, _ = bank(0)

    def adaln_cols(g, wb, cbt, pt, pc0, pkey):
        for j in range(8):
            for kk in range(8):
                k.op("pe", lambda e, j=j, kk=kk: e.matmul(
                    pt[:, pc0 + j:pc0 + j + 1], lhsT=wb[:, kk, j * 128:(j + 1) * 128], rhs=cbt[:, kk, 0:1],
                    start=(kk == 0), stop=(kk == 7)), reads=[wb.b(), cbt.b()], writes=[pkey])

    def adaln_gate(g, wb, cbt, browt):
        gt = PS[1]
        for hh in range(2):
            for kk in range(8):
                k.op("pe", lambda e, hh=hh, kk=kk: e.matmul(
                    gt[:, hh * 512:(hh + 1) * 512], lhsT=cbt[:, kk, :], rhs=wb[:, kk, hh * 512:(hh + 1) * 512],
                    start=(kk == 0), stop=(kk == 7)), reads=[wb.b(), cbt.b()], writes=[gt.b(hh * 512)])
        G = Grow_m if g == 2 else Grow_f
        bi = 0 if g == 2 else 1
        k.op("dve", lambda e: e.tensor_tensor(out=browt[:, bi, :], in0=gt[:, :], in1=browt[:, bi, :], op=ALU.add),
             reads=[gt.b(0), gt.b(512), browt.b()], writes=[browt.b()])
        k.op("dve", lambda e: e.tensor_tensor(out=G[:], in0=G[:], in1=browt[:, bi, :], op=ALU.mult),
             reads=[browt.b(), G.b()], writes=[G.b()])

    def adaln_AS(a0, g0, sc0, sh0):
        k.op("dve", lambda e: e.scalar_tensor_tensor(
            out=AS[:, a0:a0 + 8], in0=modT[:, sc0:sc0 + 8], scalar=1.0, in1=cols[:, g0:g0 + 8],
            op0=ALU.add, op1=ALU.mult), reads=[modT.b(), cols.b()], writes=[AS.b()])
        k.op("dve", lambda e: e.tensor_copy(out=AS[:, a0 + 8:a0 + 16], in_=modT[:, sh0:sh0 + 8]),
             reads=[modT.b()], writes=[AS.b()])

    for g in range(2):
        wb = wada[g]
        k.dma("pool", wb[:], wada_v[:, :, g * 1024:(g + 1) * 1024], writes=[wb.b()])
    k.dma("pool", W_A[:], win_v[:, :, 0:1024], writes=[W_A.b()])
    k.dma("pool", W_V[:], win_v[:, :, 2048:2560], writes=[W_V.b()])
    for g in range(2):
        adaln_cols(g, wada[g], cb, pst, 8 * g, pst.b(0))
    k.op("dve", lambda e: e.tensor_tensor(out=modT[:, 0:16], in0=pst[:, 0:16], in1=cols[:, 8:24], op=ALU.add),
         reads=[pst.b(0), cols.b()], writes=[modT.b()])
    adaln_AS(0, 56, 8, 0)
    L = cols[:, 100:104]
    k.op("act", lambda e: e.activation(out=lam[:, 8:12], in_=L, func=AF.Abs),
         reads=[cols.b()], writes=[lam.b()])
    k.op("act", lambda e: e.activation(out=lam[:, 8:12], in_=lam[:, 8:12], func=AF.Exp, scale=-1.0),
         reads=[lam.b()], writes=[lam.b()])
    k.op("act", lambda e: e.activation(out=lam[:, 8:12], in_=lam[:, 8:12], func=AF.Ln, bias=1.0),
         reads=[lam.b()], writes=[lam.b()])
    k.op("dve", lambda e: e.tensor_scalar(out=lam[:, 12:16], in0=L, scalar1=-1.0, scalar2=0.0, op0=ALU.mult, op1=ALU.max),
         reads=[cols.b()], writes=[lam.b()])
    k.op("dve", lambda e: e.tensor_tensor(out=lam[:, 8:12], in0=lam[:, 8:12], in1=lam[:, 12:16], op=ALU.add),
         reads=[lam.b()], writes=[lam.b()])
    k.op("dve", lambda e: e.tensor_scalar(out=lam[:, 0:4], in0=lam[:, 8:12], scalar1=-8.0, scalar2=None, op0=ALU.mult),
         reads=[lam.b()], writes=[lam.b()])
    k.op("dve", lambda e: e.tensor_scalar(out=lam[:, 4:8], in0=lam[:, 8:12], scalar1=-16.0, scalar2=None, op0=ALU.mult),
         reads=[lam.b()], writes=[lam.b()])
    k.op("dve", lambda e: e.tensor_scalar(out=b1c[:], in0=cols[:, 117:629], scalar1=1.0, scalar2=None, op0=ALU.add),
         reads=[cols.b()], writes=[b1c.b()])
    k.barrier()

    if stop_after == 'A':
        k.barrier()
        return nc, stack, dbg_out
    o = R0
    qkst = k.alloc([128, 8, 1024], BF16, off=o, name="qkst"); o += 16 * KB
    fst = k.alloc([128, 8, 8], BF16, off=o, name="fst"); o += 128
    O_F = o

    k.dma("pool", fst[:], win_v[:, :, 2560:2568], writes=[fst.b()])
    k.dma("pool", qkst[:], win_v[:, :, 1024:2048], writes=[qkst.b()])
    k.op("pool", lambda e: e.memset(W_F[:], 0.0), writes=[W_F.b()])
    for r0 in (0, 32, 64):
        k.op("pool", lambda e, r0=r0: e.tensor_copy(out=W_F[:, :, r0:r0 + 8], in_=fst[:]),
             reads=[fst.b()], writes=[W_F.b()])
    k.op("pool", lambda e: e.memset(vp[:, :, :, 64:65], 1.0), writes=[vp.b("ones")])

    def prenorm_group(g, src_tiles, a0, dstT, dstF32=None, xn=None, junk=None, st=None):
        for tt in range(4):
            xt, xb_ = src_tiles[tt]
            i = g * 4 + tt
            sc = st[:, tt * 4:tt * 4 + 4]
            k.op("act", lambda e, xt=xt, sc=sc: e.activation(out=junk[:], in_=xt[:], func=AF.Square, accum_out=sc[:, 0:1]),
                 reads=[xb_], writes=[junk.b(), st.b(tt)])
            k.op("act", lambda e, sc=sc: e.activation(out=sc[:, 1:2], in_=sc[:, 0:1], func=AF.Sqrt, scale=1.0 / D, bias=EPS),
                 reads=[st.b(tt)], writes=[st.b(tt)])
            k.op("dve", lambda e, sc=sc: e.reciprocal(out=sc[:, 2:3], in_=sc[:, 1:2]), reads=[st.b(tt)], writes=[st.b(tt)])
            k.op("dve", lambda e, xt=xt, sc=sc, tt=tt: e.tensor_scalar(out=xn[tt][:], in0=xt[:], scalar1=sc[:, 2:3],
                                                                       scalar2=None, op0=ALU.mult),
                 reads=[xb_, st.b(tt)], writes=[xn[tt].b()])
        for kk in range(8):
            bt, c0 = bank(kk % 2)
            for tt in range(4):
                k.op("pe", lambda e, kk=kk, tt=tt, bt=bt, c0=c0: e.transpose(
                    out=bt[:, c0 + tt * 128:c0 + (tt + 1) * 128], in_=xn[tt][:, kk * 128:(kk + 1) * 128], identity=identf[:]),
                    reads=[xn[tt].b(), identf.b()], writes=[bt.b(kk % 2)])
            k.op("dve", lambda e, kk=kk, bt=bt, c0=c0: e.tensor_scalar(
                out=dstT[:, kk, g * 512:(g + 1) * 512], in0=bt[:, c0:c0 + 512],
                scalar1=AS[:, a0 + kk:a0 + kk + 1], scalar2=AS[:, a0 + 8 + kk:a0 + 9 + kk], op0=ALU.mult, op1=ALU.add),
                reads=[bt.b(kk % 2), AS.b()], writes=[dstT.b(g)])
            if dstF32 is not None:
                k.op("dve", lambda e, kk=kk, bt=bt, c0=c0: e.tensor_scalar(
                    out=dstF32[:, kk, :], in0=bt[:, c0:c0 + 512],
                    scalar1=AS[:, a0 + kk:a0 + kk + 1], scalar2=AS[:, a0 + 8 + kk:a0 + 9 + kk], op0=ALU.mult, op1=ALU.add),
                    reads=[bt.b(kk % 2), AS.b()], writes=[dstF32.b()])

    o = O_F
    F1 = k.alloc([97, S], F32, off=o, name="F1"); o += 8 * KB
    F2 = k.alloc([97, S], F32, off=o, name="F2"); o += 8 * KB
    F3 = k.alloc([97, S], F32, off=o, name="F3"); o += 8 * KB
    CPh = k.alloc([97, S], BF16, off=o, name="CPh"); o += 4 * KB
    CPm = k.alloc([97, S], BF16, off=o, name="CPm"); o += 4 * KB
    CPl = k.alloc([97, S], BF16, off=o, name="CPl"); o += 4 * KB
    assert o <= SB_TOP
    fbcol = cols[0:97, 112:113]

    def vproj(i):
        bt, c0 = bank(2 + i % 2)
        for kk in range(8):
            k.op("pe", lambda e, i=i, kk=kk, bt=bt, c0=c0: e.matmul(
                bt[:, c0:c0 + 512], lhsT=hT[:, kk, i * 128:(i + 1) * 128], rhs=W_V[:, kk, :],
                start=(kk == 0), stop=(kk == 7)), reads=[hT.b(i // 4), W_V.b()], writes=[bt.b(i % 2)])
        k.op("act", lambda e, i=i, bt=bt, c0=c0: e.activation(
            out=vp[:, i, :, 0:64], in_=bt[:, c0:c0 + 512].rearrange("p (h c) -> p h c", c=64), func=AF.Copy),
            reads=[bt.b(i % 2)], writes=[vp.b(i)])

    def fproj(g):
        bt, c0 = bank(4 + g % 2)
        for kk in range(8):
            k.op("pe", lambda e, g=g, kk=kk, bt=bt, c0=c0: e.matmul(
                bt[0:97, c0:c0 + 512], lhsT=W_F[:, kk, :], rhs=hT[:, kk, g * 512:(g + 1) * 512],
                start=(kk == 0), stop=(kk == 7)), reads=[hT.b(g), W_F.b()], writes=[bt.b(g % 2)])
        k.op("dve", lambda e, g=g, bt=bt, c0=c0: e.tensor_scalar(
            out=F1[:, g * 512:(g + 1) * 512], in0=bt[0:97, c0:c0 + 512], scalar1=fbcol, scalar2=None, op0=ALU.add),
            reads=[bt.b(g % 2), cols.b()], writes=[F1.b()])

    for g in range(4):
        if g >= 1:
            pre_B(g)
        post_B(g)
        if g >= 1:
            for tt in range(4):
                vproj((g - 1) * 4 + tt)
            fproj(g - 1)
    for tt in range(4):
        vproj(12 + tt)
    fproj(3)
    dbg("hT", hT[:], [128, 8, S], [hT.b(g) for g in range(4)])
    k.op("act", lambda e: e.activation(out=F2[:], in_=F1[:], func=AF.Abs),
         reads=[F1.b()], writes=[F2.b()])
    k.op("act", lambda e: e.activation(out=F2[:], in_=F2[:], func=AF.Exp, scale=-1.0), reads=[F2.b()], writes=[F2.b()])
    k.op("act", lambda e: e.activation(out=F2[:], in_=F2[:], func=AF.Ln, bias=1.0), reads=[F2.b()], writes=[F2.b()])
    k.op("dve", lambda e: e.tensor_scalar(out=F1[:], in0=F1[:], scalar1=0.0, scalar2=None, op0=ALU.min),
         reads=[F1.b()], writes=[F1.b()])
    k.op("dve", lambda e: e.tensor_tensor(out=F1[:], in0=F1[:], in1=F2[:], op=ALU.subtract),
         reads=[F1.b(), F2.b()], writes=[F1.b()])
    k.op("pool", lambda e: e.memset(F3[:], 1.0), writes=[F3.b()])
    k.op("pool", lambda e: e.memset(W_QK[:], 0.0), writes=[W_QK.b()])
    k.op("pool", lambda e: e.tensor_copy(out=W_QK[:, :, :, 0:64],
                                         in_=qkst[:].rearrange("p k (h c) -> p k h c", c=64)),
         reads=[qkst.b()], writes=[W_QK.b()])
    k.op("dve", lambda e: e.tensor_tensor_scan(out=F2[:], data0=F3[:], data1=F1[:], initial=0.0, op0=ALU.mult, op1=ALU.add),
         reads=[F1.b(), F3.b()], writes=[F2.b()])
    dbg("C", F2[:], [97, S], [F2.b()])
    k.op("dve", lambda e: e.tensor_copy(out=CPh[:], in_=F2[:]), reads=[F2.b()], writes=[CPh.b()])
    k.op("dve", lambda e: e.tensor_tensor(out=F1[:], in0=F2[:], in1=CPh[:], op=ALU.subtract),
         reads=[F2.b(), CPh.b()], writes=[F1.b()])
    k.op("dve", lambda e: e.tensor_copy(out=CPm[:], in_=F1[:]), reads=[F1.b()], writes=[CPm.b()])
    k.op("dve", lambda e: e.tensor_tensor(out=F3[:], in0=F1[:], in1=CPm[:], op=ALU.subtract),
         reads=[F1.b(), CPm.b()], writes=[F3.b()])
    k.op("dve", lambda e: e.tensor_copy(out=CPl[:], in_=F3[:]), reads=[F3.b()], writes=[CPl.b()])
    mc = lambda j: cols[0:97, 113 + j:114 + j]
    k.op("dve", lambda e: e.tensor_scalar(out=CP[:], in0=CPh[:], scalar1=mc(0), scalar2=None, op0=ALU.mult),
         reads=[CPh.b(), cols.b()], writes=[CP.b()])
    k.op("dve", lambda e: e.scalar_tensor_tensor(out=CP[:], in0=CPm[:], scalar=mc(1), in1=CP[:], op0=ALU.mult, op1=ALU.add),
         reads=[CPm.b(), CP.b()], writes=[CP.b()])
    k.op("dve", lambda e: e.scalar_tensor_tensor(out=CP[:], in0=CPl[:], scalar=mc(2), in1=CP[:], op0=ALU.mult, op1=ALU.add),
         reads=[CPl.b(), CP.b()], writes=[CP.b()])
    k.op("dve", lambda e: e.tensor_scalar(out=CP[:], in0=CP[:], scalar1=mc(3), scalar2=None, op0=ALU.add),
         reads=[CP.b()], writes=[CP.b()])
    k.barrier()

    if stop_after == 'B':
        k.barrier()
        return nc, stack, dbg_out
    o = R0
    qk_t = [[k.alloc([70, S], BF16, off=o + (2 * s + j) * 4 * KB, name="qk%d%d" % (s, j)) for j in range(2)] for s in range(2)]
    o += 16 * KB
    PT = [k.alloc([128, 512], BF16, off=o + i * KB, name="PT%d" % i) for i in range(4)]; o += 4 * KB
    attn = k.alloc([128, NT, 512], F32, off=o, name="attn"); o += 32 * KB
    rden = k.alloc([128, 8], F32, off=o, name="rden"); o += 64
    ant = [k.alloc([128, 512], F32, off=o + i * 2 * KB, name="ant%d" % i) for i in range(4)]; o += 8 * KB
    st2 = k.alloc([128, 64], F32, off=o, name="st2"); o += 256
    junk2 = k.alloc([128, 512], BF16, off=o, name="junk2"); o += KB
    assert o <= SB_TOP

    def qk_proj(h):
        s = h % 2
        for j in range(2):
            dst = qk_t[s][j]
            for g in range(4):
                bt, c0 = bank(6 + g % 2)
                for kk in range(8):
                    k.op("pe", lambda e, j=j, g=g, kk=kk, bt=bt, c0=c0: e.matmul(
                        bt[0:70, c0:c0 + 512], lhsT=W_QK[:, kk, j * 8 + h, :], rhs=hT[:, kk, g * 512:(g + 1) * 512],
                        start=(kk == 0), stop=False), reads=[hT.b(g), W_QK.b()], writes=[bt.b(g % 2)])
                k.op("pe", lambda e, j=j, g=g, bt=bt, c0=c0: e.matmul(
                    bt[0:70, c0:c0 + 512], lhsT=SEL[:, j, h, :], rhs=CP[:, g * 512:(g + 1) * 512],
                    start=False, stop=True), reads=[CP.b(), SEL.b()], writes=[bt.b(g % 2)])
                if j == 0:
                    k.op("dve", lambda e, g=g, bt=bt, c0=c0, dst=dst: e.tensor_scalar(
                        out=dst[:, g * 512:(g + 1) * 512], in0=bt[0:70, c0:c0 + 512], scalar1=0.125, scalar2=None,
                        op0=ALU.mult), reads=[bt.b(g % 2)], writes=[dst.b()])
                else:
                    k.op("dve", lambda e, g=g, bt=bt, c0=c0, dst=dst: e.tensor_copy(
                        out=dst[:, g * 512:(g + 1) * 512], in_=bt[0:70, c0:c0 + 512]),
                        reads=[bt.b(g % 2)], writes=[dst.b()])

    def attn_head(h):
        s = h % 2
        qp, kp = qk_t[s]
        groups = []
        for i in range(NT):
            nb = i + 1
            for g0 in range(0, nb, 4):
                groups.append((i, list(range(g0, min(nb, g0 + 4)))))
        n = len(groups)

        def qk_mm(gi):
            i, blks = groups[gi]
            bt, c0 = bank(gi % 3)
            for jj, j in enumerate(blks):
                diag = (j == i)
                k.op("pe", lambda e, jj=jj, j=j, i=i, bt=bt, c0=c0, diag=diag: e.matmul(
                    bt[:, c0 + jj * 128:c0 + (jj + 1) * 128], lhsT=kp[:, j * 128:(j + 1) * 128],
                    rhs=qp[:, i * 128:(i + 1) * 128], start=True, stop=not diag),
                    reads=[kp.b(), qp.b()], writes=[bt.b(c0)])
                if diag:
                    k.op("pe", lambda e, jj=jj, bt=bt, c0=c0: e.matmul(
                        bt[:, c0 + jj * 128:c0 + (jj + 1) * 128], lhsT=identb[:], rhs=maskT[:],
                        start=False, stop=True), reads=[identb.b(), maskT.b()], writes=[bt.b(c0)])

        def exp_pv(gi):
            i, blks = groups[gi]
            bt, c0 = bank(gi % 3)
            nbk = len(blks)
            pt = PT[gi % 4]
            k.op("act", lambda e, bt=bt, c0=c0, nbk=nbk, pt=pt: e.activation(
                out=pt[:, 0:nbk * 128], in_=bt[:, c0:c0 + nbk * 128], func=AF.Exp),
                reads=[bt.b(c0)], writes=[pt.b()])
            ot, oc0 = bank(3 + (i % 2))
            for jj, j in enumerate(blks):
                k.op("pe", lambda e, jj=jj, j=j, i=i, ot=ot, oc0=oc0, pt=pt: e.matmul(
                    ot[:, oc0:oc0 + 65], lhsT=pt[:, jj * 128:(jj + 1) * 128], rhs=vp[:, j, h, :],
                    start=(j == 0), stop=(j == i)), reads=[pt.b(), vp.b(j), vp.b("ones")], writes=[ot.b(oc0)])
            if blks[-1] == i:
                k.op("dve", lambda e, ot=ot, oc0=oc0: e.reciprocal(out=rden[:, h:h + 1], in_=ot[:, oc0 + 64:oc0 + 65]),
                     reads=[ot.b(oc0)], writes=[rden.b(h)])
                k.op("dve", lambda e, ot=ot, oc0=oc0, i=i: e.tensor_scalar(
                    out=attn[:, i, h * 64:(h + 1) * 64], in0=ot[:, oc0:oc0 + 64], scalar1=rden[:, h:h + 1], scalar2=None,
                    op0=ALU.mult), reads=[ot.b(oc0), rden.b(h)], writes=[attn.b(i)])

        qk_mm(0)
        for gi in range(n):
            if gi + 1 < n:
                qk_mm(gi + 1)
            exp_pv(gi)

    qk_proj(0)
    for h in range(8):
        if h + 1 < 8:
            qk_proj(h + 1)
        attn_head(h)
    dbg("attn", attn[:], [128, NT, 512], [attn.b(i) for i in range(NT)])

    for g in range(4):
        for tt in range(4):
            i = g * 4 + tt
            sc = st2[:, tt * 4:tt * 4 + 4]
            k.op("act", lambda e, i=i, sc=sc: e.activation(out=junk2[:], in_=attn[:, i, :], func=AF.Square, accum_out=sc[:, 0:1]),
                 reads=[attn.b(i)], writes=[junk2.b(), st2.b(tt)])
            k.op("act", lambda e, sc=sc: e.activation(out=sc[:, 1:2], in_=sc[:, 0:1], func=AF.Sqrt, scale=1.0 / 512, bias=EPS),
                 reads=[st2.b(tt)], writes=[st2.b(tt)])
            k.op("dve", lambda e, sc=sc: e.reciprocal(out=sc[:, 2:3], in_=sc[:, 1:2]), reads=[st2.b(tt)], writes=[st2.b(tt)])
            k.op("dve", lambda e, i=i, sc=sc, tt=tt: e.tensor_scalar(out=ant[tt][:], in0=attn[:, i, :], scalar1=sc[:, 2:3],
                                                                     scalar2=None, op0=ALU.mult),
                 reads=[attn.b(i), st2.b(tt)], writes=[ant[tt].b()])
        for cc in range(4):
            bt, c0 = bank(cc % 2)
            for tt in range(4):
                k.op("pe", lambda e, cc=cc, tt=tt, bt=bt, c0=c0: e.transpose(
                    out=bt[:, c0 + tt * 128:c0 + (tt + 1) * 128], in_=ant[tt][:, cc * 128:(cc + 1) * 128], identity=identf[:]),
                    reads=[ant[tt].b(), identf.b()], writes=[bt.b(c0)])
            k.op("dve", lambda e, cc=cc, bt=bt, c0=c0, g=g: e.tensor_scalar(
                out=merged[:, 4 + cc, g * 512:(g + 1) * 512], in0=bt[:, c0:c0 + 512],
                scalar1=cols[:, 108 + cc:109 + cc], scalar2=None, op0=ALU.mult),
                reads=[bt.b(c0), cols.b()], writes=[merged.b(("a", g))])
    k.barrier()

    if stop_after == 'C':
        k.barrier()
        return nc, stack, dbg_out
    o = O_ATT
    xl = k.alloc([128, 4, S + 4], F32, off=o, name="xl"); o += 4 * (S + 4) * 4
    gl = k.alloc([128, 4, S], BF16, off=o, name="gl"); o += 16 * KB
    BD = k.alloc([128, 2, 4, 128], BF16, off=o, name="BD"); o += 2 * KB
    xc2 = [k.alloc([128, S], F32, off=o + i * 8 * KB, name="xc%d" % i) for i in range(2)]; o += 16 * KB
    xcb2 = [k.alloc([128, S], BF16, off=o + i * 4 * KB, name="xcb%d" % i) for i in range(2)]; o += 8 * KB
    T1 = k.alloc([128, S], F32, off=o, name="T1"); o += 8 * KB
    T2 = k.alloc([128, S], F32, off=o, name="T2"); o += 8 * KB
    T3 = k.alloc([128, S], F32, off=o, name="T3"); o += 8 * KB
    sq = [k.alloc([128, 512], F32, off=o + i * 2 * KB, name="sq%d" % i) for i in range(2)]; o += 4 * KB
    rs = [k.alloc([128, 512], F32, off=o + i * 2 * KB, name="rs%d" % i) for i in range(2)]; o += 4 * KB
    assert o <= SB_TOP, o
    k.dma("pool", BD[:], bd_d.rearrange("p (a c j) -> p a c j", a=2, c=4), writes=[BD.b()])
    k.op("pool", lambda e: e.memset(xl[:, :, 0:4], 0.0), writes=[xl.b("pad")])

    def lru_inproj(c):
        for g in range(4):
            bt, c0 = bank(g % 2)
            for kk in range(8):
                k.op("pe", lambda e, c=c, g=g, kk=kk, bt=bt, c0=c0: e.matmul(
                    bt[:, c0:c0 + 512], lhsT=W_A[:, kk, c * 128:(c + 1) * 128], rhs=hT[:, kk, g * 512:(g + 1) * 512],
                    start=(kk == 0), stop=(kk == 7)), reads=[hT.b(g), W_A.b()], writes=[bt.b(c0)])
            k.op("act", lambda e, c=c, g=g, bt=bt, c0=c0: e.activation(
                out=xl[:, c, 4 + g * 512:4 + (g + 1) * 512], in_=bt[:, c0:c0 + 512], func=AF.Copy),
                reads=[bt.b(c0)], writes=[xl.b(c)])
        for g in range(4):
            bt, c0 = bank(2 + g % 2)
            for kk in range(8):
                k.op("pe", lambda e, c=c, g=g, kk=kk, bt=bt, c0=c0: e.matmul(
                    bt[:, c0:c0 + 512], lhsT=W_A[:, kk, 512 + c * 128:512 + (c + 1) * 128], rhs=hT[:, kk, g * 512:(g + 1) * 512],
                    start=(kk == 0), stop=(kk == 7)), reads=[hT.b(g), W_A.b()], writes=[bt.b(c0)])
            k.op("act", lambda e, c=c, g=g, bt=bt, c0=c0: e.activation(
                out=gl[:, c, g * 512:(g + 1) * 512], in_=bt[:, c0:c0 + 512], func=AF.Gelu_apprx_tanh),
                reads=[bt.b(c0)], writes=[gl.b(c)])

    def lru_conv(c):
        xc = xc2[c % 2]
        xcb = xcb2[c % 2]
        cw = lambda j: cols[:, 72 + c * 4 + j:73 + c * 4 + j]
        k.op("dve", lambda e: e.tensor_scalar(out=xc[:], in0=xl[:, c, 4:4 + S], scalar1=cw(3), scalar2=cols[:, 88 + c:89 + c],
                                              op0=ALU.mult, op1=ALU.add),
             reads=[xl.b(c), xl.b("pad"), cols.b()], writes=[xc.b()])
        for j in range(3):
            sh = 3 - j
            k.op("dve", lambda e, j=j, sh=sh: e.scalar_tensor_tensor(
                out=xc[:], in0=xl[:, c, 4 - sh:4 - sh + S], scalar=cw(j), in1=xc[:], op0=ALU.mult, op1=ALU.add),
                reads=[xl.b(c), xl.b("pad"), xc.b()], writes=[xc.b()])
        k.op("pool", lambda e: e.tensor_copy(out=xcb[:], in_=xc[:]), reads=[xc.b()], writes=[xcb.b()])

    def lru_gates(c):
        xcb = xcb2[c % 2]
        for gate, dstT, bcol in ((0, T1, 92), (1, T3, 96)):
            for g in range(4):
                bt, c0 = bank(4 + (gate * 4 + g) % 4)
                k.op("pe", lambda e, gate=gate, g=g, bt=bt, c0=c0: e.matmul(
                    bt[:, c0:c0 + 512], lhsT=BD[:, gate, c, :], rhs=xcb[:, g * 512:(g + 1) * 512], start=True, stop=True),
                    reads=[BD.b(), xcb.b()], writes=[bt.b(c0)])
                k.op("act", lambda e, g=g, bt=bt, c0=c0, dstT=dstT, bcol=bcol: e.activation(
                    out=dstT[:, g * 512:(g + 1) * 512], in_=bt[:, c0:c0 + 512], func=AF.Sigmoid,
                    bias=cols[:, bcol + c:bcol + c + 1]), reads=[bt.b(c0), cols.b()], writes=[dstT.b()])
        k.op("act", lambda e: e.activation(out=T2[:], in_=T1[:], func=AF.Exp, scale=lam[:, c:c + 1]),
             reads=[T1.b(), lam.b()], writes=[T2.b()])
        k.op("act", lambda e: e.activation(out=T1[:], in_=T1[:], func=AF.Exp, scale=lam[:, 4 + c:5 + c]),
             reads=[T1.b(), lam.b()], writes=[T1.b()])
        k.op("act", lambda e: e.activation(out=T1[:], in_=T1[:], func=AF.Sqrt, scale=-1.0, bias=1.0),
             reads=[T1.b()], writes=[T1.b()])

    def lru_rest(c):
        xc = xc2[c % 2]
        k.op("dve", lambda e: e.tensor_tensor(out=T3[:], in0=T3[:], in1=xc[:], op=ALU.mult),
             reads=[T3.b(), xc.b()], writes=[T3.b()])
        k.op("dve", lambda e: e.tensor_tensor(out=T3[:], in0=T3[:], in1=T1[:], op=ALU.mult),
             reads=[T3.b(), T1.b()], writes=[T3.b()])
        k.op("dve", lambda e: e.tensor_tensor_scan(out=T1[:], data0=T2[:], data1=T3[:], initial=0.0, op0=ALU.mult, op1=ALU.add),
             reads=[T2.b(), T3.b(), T1.b()], writes=[T1.b()])
        k.op("dve", lambda e: e.tensor_tensor(out=xl[:, c, 4:4 + S], in0=T1[:], in1=gl[:, c, :], op=ALU.mult),
             reads=[T1.b(), gl.b(c), xl.b(c)], writes=[xl.b(c)])

    wada2 = [k.alloc([128, 8, 1024], BF16, off=B0 + i * 16 * KB, name="wadaL%d" % i) for i in range(2)]
    cb2 = k.alloc([128, 8, 128], BF16, off=B0 + 32 * KB, name="cb2")
    csil2 = k.alloc([128, 8], F32, off=B0 + 34 * KB, name="csil2")
    brow2 = k.alloc([128, 2, D], F32, off=B0 + 36 * KB, name="brow2")

    def late_load(g):
        wb = wada2[g % 2]
        k.dma("pool", wb[:], wada_v[:, :, g * 1024:(g + 1) * 1024], writes=[wb.b()])

    def late_setup():
        for en in ("sp", "act", "dve", "pool"):
            k._wait(en, {"pe": k.cnt["pe"]})
        k.op("act", lambda e: e.activation(out=csil2[:], in_=cols[:, 0:8], func=AF.Silu), reads=[cols.b()], writes=[csil2.b()])
        for kk in range(8):
            k.op("dve", lambda e, kk=kk: e.tensor_copy(out=cb2[:, kk, :], in_=csil2[:, kk:kk + 1].to_broadcast([128, 128])),
                 reads=[csil2.b()], writes=[cb2.b()])
        k.dma("sp", brow2[:, 0, :], rows_d[0, :].partition_broadcast(128), writes=[brow2.b()])
        k.dma("sp", brow2[:, 1, :], rows_d[1, :].partition_broadcast(128), writes=[brow2.b()])
        late_load(2)
        late_load(3)

    def late_pe(g):
        wb = wada2[g % 2]
        pt = PS[1]
        adaln_cols(g, wb, cb2, pt, 0, pt.b(0))
        k.op("dve", lambda e: e.tensor_tensor(out=modT[:, 8 * g:8 * g + 8], in0=pt[:, 0:8], in1=cols[:, 8 + 8 * g:16 + 8 * g], op=ALU.add),
             reads=[pt.b(0), cols.b()], writes=[modT.b()])
        if g in (2, 5):
            adaln_gate(g, wb, cb2, brow2)

    lru_inproj(0)
    lru_conv(0)
    lru_inproj(1)
    for c in range(4):
        lru_gates(c)
        if c >= 2:
            late_pe(c)
            late_load(c + 2)
        if c + 1 < 4:
            lru_conv(c + 1)
        if c + 2 < 4:
            lru_inproj(c + 2)
            if c + 2 == 3:
                late_setup()
        lru_rest(c)
    late_pe(4)
    late_pe(5)
    adaln_AS(16, 64, 32, 24)
    dbg("modT", modT[:], [128, 48], [modT.b()])
    dbg("Grow_m", Grow_m[:], [128, D], [Grow_m.b()])
    dbg("lru", xl[:], [128, 4, S + 4], [xl.b(c) for c in range(4)])
    for g in range(4):
        bt, c0 = bank(g % 2)
        for c in range(4):
            sqt = sq[c % 2]
            k.op("act", lambda e, c=c, g=g, sqt=sqt: e.activation(out=sqt[:], in_=xl[:, c, 4 + g * 512:4 + (g + 1) * 512], func=AF.Square),
                 reads=[xl.b(c)], writes=[sqt.b()])
            k.op("pe", lambda e, c=c, bt=bt, c0=c0, sqt=sqt: e.matmul(bt[:, c0:c0 + 512], lhsT=onesf[:], rhs=sqt[:],
                                                                      start=(c == 0), stop=(c == 3)),
                 reads=[sqt.b(), onesf.b()], writes=[bt.b(c0)])
        rt = rs[g % 2]
        k.op("act", lambda e, bt=bt, c0=c0, rt=rt: e.activation(out=rt[:], in_=bt[:, c0:c0 + 512], func=AF.Sqrt, scale=1.0 / 512, bias=EPS),
             reads=[bt.b(c0)], writes=[rt.b()])
        k.op("dve", lambda e, rt=rt: e.reciprocal(out=rt[:], in_=rt[:]), reads=[rt.b()], writes=[rt.b()])
        for c in range(4):
            k.op("dve", lambda e, c=c, g=g, rt=rt: e.scalar_tensor_tensor(
                out=merged[:, c, g * 512:(g + 1) * 512], in0=xl[:, c, 4 + g * 512:4 + (g + 1) * 512],
                scalar=cols[:, 104 + c:105 + c], in1=rt[:], op0=ALU.mult, op1=ALU.mult),
                reads=[xl.b(c), rt.b(), cols.b()], writes=[merged.b(("l", g))])
    dbg("merged", merged[:], [128, 8, S], [merged.b(("l", g)) for g in range(4)] + [merged.b(("a", g)) for g in range(4)])
    k.barrier()

    if stop_after == 'D':
        k.barrier()
        return nc, stack, dbg_out
    h2T = k.alloc([128, 8, S], BF16, off=B0, name="h2T")
    wts = k.alloc([128, NT, NE], F32, off=B0 + 32 * KB, name="wts")
    yacc = k.alloc([128, NT, D], F32, off=B0 + 34 * KB, name="yacc")
    M0 = B0 + 98 * KB
    o = M0
    w_out = k.alloc([128, 8, D], BF16, off=o, name="w_out"); o += 16 * KB
    xr = [k.alloc([128, D], F32, off=o + i * 4 * KB, name="xr%d" % i) for i in range(3)]; o += 12 * KB
    tt_ = [k.alloc([128, D], F32, off=o + i * 4 * KB, name="tt%d" % i) for i in range(2)]; o += 8 * KB
    x2t = [k.alloc([128, D], F32, off=o + i * 4 * KB, name="x2t%d" % i) for i in range(4)]; o += 16 * KB
    hf = k.alloc([128, 8, 512], F32, off=o, name="hf"); o += 16 * KB
    wrt = k.alloc([128, 8, NE], F32, off=o, name="wrt"); o += KB
    lg = k.alloc([128, 4, 128], F32, off=o, name="lg"); o += 2 * KB
    xn = [k.alloc([128, D], F32, off=o + i * 4 * KB, name="xnb%d" % i) for i in range(4)]; o += 16 * KB
    junk2b = [k.alloc([128, D], BF16, off=o + i * 2 * KB, name="junkb%d" % i) for i in range(2)]; o += 4 * KB
    st3 = k.alloc([128, NT * 8], F32, off=o, name="st3"); o += 512
    assert o <= SB_TOP, o

    k.dma("pool", w_out[:], wout_d.rearrange("(k p) n -> p k n", p=128), writes=[w_out.b()])
    k.dma("sp", wrt[:], wr_d.rearrange("(k p) n -> p k n", p=128), writes=[wrt.b()])

    def ef_load(i):
        xt = xr[i % 3]
        k.dma("sp", xt[:], x_d[i * 128:(i + 1) * 128, :], writes=[xt.b()])

    def ef_s0(i):
        g = i // 4
        yt = PS[1 + i % 2]
        for hh in range(2):
            for kk in range(8):
                k.op("pe", lambda e, i=i, hh=hh, kk=kk, yt=yt: e.matmul(
                    yt[:, hh * 512:(hh + 1) * 512], lhsT=merged[:, kk, i * 128:(i + 1) * 128],
                    rhs=w_out[:, kk, hh * 512:(hh + 1) * 512], start=(kk == 0), stop=(kk == 7)),
                    reads=[merged.b(("l", g)), merged.b(("a", g)), w_out.b()], writes=[yt.b("y")])

    def ef_s1(i):
        xt = xr[i % 3]
        yt = PS[1 + i % 2]
        sc = st3[:, i * 8:i * 8 + 4]
        sb = st3.b(("y", i))
        jk = junk2b[i % 2]
        k.op("act", lambda e, yt=yt, sc=sc, jk=jk: e.activation(out=jk[:], in_=yt[:, :], func=AF.Square, accum_out=sc[:, 0:1]),
             reads=[yt.b("y")], writes=[jk.b(), sb])
        k.op("act", lambda e, sc=sc: e.activation(out=sc[:, 1:2], in_=sc[:, 0:1], func=AF.Sqrt, scale=1.0 / D, bias=EPS),
             reads=[sb], writes=[sb])
        k.op("dve", lambda e, sc=sc: e.reciprocal(out=sc[:, 2:3], in_=sc[:, 1:2]), reads=[sb], writes=[sb])
        tq = tt_[i % 2]
        k.op("dve", lambda e, yt=yt, sc=sc, tq=tq: e.scalar_tensor_tensor(
            out=tq[:], in0=yt[:, :], scalar=sc[:, 2:3], in1=Grow_m[:], op0=ALU.mult, op1=ALU.mult),
            reads=[yt.b("y"), sb, Grow_m.b()], writes=[tq.b()])
        x2 = x2t[i % 4]
        k.op("pool", lambda e, tq=tq, xt=xt, x2=x2: e.tensor_tensor(out=x2[:], in0=tq[:], in1=xt[:], op=ALU.add),
             reads=[tq.b(), xt.b()], writes=[x2.b()])
        k.dma("sp", x2_d[i * 128:(i + 1) * 128, :], x2[:], reads=[x2.b()], writes=[x2sc.b(i)])

    def ef_s2(i):
        x2 = x2t[i % 4]
        sc = st3[:, i * 8 + 4:i * 8 + 8]
        sb = st3.b(("x", i))
        jk = junk2b[i % 2]
        k.op("act", lambda e, x2=x2, sc=sc, jk=jk: e.activation(out=jk[:], in_=x2[:], func=AF.Square, accum_out=sc[:, 0:1]),
             reads=[x2.b()], writes=[jk.b(), sb])
        k.op("act", lambda e, sc=sc: e.activation(out=sc[:, 1:2], in_=sc[:, 0:1], func=AF.Sqrt, scale=1.0 / D, bias=EPS),
             reads=[sb], writes=[sb])
        k.op("dve", lambda e, sc=sc: e.reciprocal(out=sc[:, 2:3], in_=sc[:, 1:2]), reads=[sb], writes=[sb])
        k.op("dve", lambda e, x2=x2, sc=sc, i=i: e.tensor_scalar(out=xn[i % 4][:], in0=x2[:], scalar1=sc[:, 2:3],
                                                                  scalar2=None, op0=ALU.mult),
             reads=[x2.b(), sb], writes=[xn[i % 4].b()])

    def ef_s3(g):
        a0 = 16
        for kk in range(8):
            bt, c0 = bank(kk % 2)
            for tt in range(4):
                k.op("pe", lambda e, kk=kk, tt=tt, bt=bt, c0=c0: e.transpose(
                    out=bt[:, c0 + tt * 128:c0 + (tt + 1) * 128], in_=xn[tt][:, kk * 128:(kk + 1) * 128], identity=identf[:]),
                    reads=[xn[tt].b(), identf.b()], writes=[bt.b(kk % 2)])
            k.op("dve", lambda e, kk=kk, bt=bt, c0=c0: e.tensor_scalar(
                out=h2T[:, kk, g * 512:(g + 1) * 512], in0=bt[:, c0:c0 + 512],
                scalar1=AS[:, a0 + kk:a0 + kk + 1], scalar2=AS[:, a0 + 8 + kk:a0 + 9 + kk], op0=ALU.mult, op1=ALU.add),
                reads=[bt.b(kk % 2), AS.b()], writes=[h2T.b(g)])
            k.op("dve", lambda e, kk=kk, bt=bt, c0=c0: e.tensor_scalar(
                out=hf[:, kk, :], in0=bt[:, c0:c0 + 512],
                scalar1=AS[:, a0 + kk:a0 + kk + 1], scalar2=AS[:, a0 + 8 + kk:a0 + 9 + kk], op0=ALU.mult, op1=ALU.add),
                reads=[bt.b(kk % 2), AS.b()], writes=[hf.b()])

    def ef_s4(g):
        for tt in range(4):
            i = g * 4 + tt
            lt, lc0 = bank(6 + tt % 2)
            for kk in range(8):
                k.op("pe", lambda e, tt=tt, kk=kk, lt=lt, lc0=lc0: e.matmul(
                    lt[:, lc0:lc0 + NE], lhsT=hf[:, kk, tt * 128:(tt + 1) * 128], rhs=wrt[:, kk, :],
                    start=(kk == 0), stop=(kk == 7)), reads=[hf.b(), wrt.b()], writes=[lt.b(lc0)])
            L_ = lg[:, tt, 0:32]
            X_ = lg[:, tt, 32:48]
            M_ = lg[:, tt, 48:80]
            E_ = lg[:, tt, 80:112]
            lb = lg.b(tt)
            k.op("dve", lambda e, lt=lt, lc0=lc0, L_=L_: e.tensor_tensor(out=L_, in0=lt[:, lc0:lc0 + NE], in1=brout[:], op=ALU.add),
                 reads=[lt.b(lc0), brout.b()], writes=[lb])
            k.op("dve", lambda e, L_=L_, X_=X_: e.max(out=X_[:, 0:8], in_=L_), reads=[lb], writes=[lb])
            k.op("dve", lambda e, X_=X_: e.tensor_scalar(out=X_[:, 8:9], in0=X_[:, 0:1], scalar1=-1.0, scalar2=None, op0=ALU.mult),
                 reads=[lb], writes=[lb])
            k.op("dve", lambda e, L_=L_, X_=X_, M_=M_: e.tensor_scalar(out=M_, in0=L_, scalar1=X_[:, 3:4], scalar2=None, op0=ALU.is_ge),
                 reads=[lb], writes=[lb])
            k.op("act", lambda e, L_=L_, X_=X_, E_=E_: e.activation(out=E_, in_=L_, func=AF.Exp, bias=X_[:, 8:9]),
                 reads=[lb], writes=[lb])
            k.op("dve", lambda e, E_=E_, M_=M_, X_=X_: e.scalar_tensor_tensor(
                out=E_, in0=E_, scalar=1.0, in1=M_, op0=ALU.mult, op1=ALU.mult, accum_out=X_[:, 9:10]),
                reads=[lb], writes=[lb])
            k.op("dve", lambda e, X_=X_: e.reciprocal(out=X_[:, 10:11], in_=X_[:, 9:10]), reads=[lb], writes=[lb])
            k.op("dve", lambda e, E_=E_, X_=X_, i=i: e.tensor_scalar(out=wts[:, i, :], in0=E_, scalar1=X_[:, 10:11], scalar2=None, op0=ALU.mult),
                 reads=[lb], writes=[wts.b(i)])

    ef_load(0)
    ef_load(1)
    for step in range(NT + 5):
        if step < NT:
            ef_s0(step)
        if 0 <= step - 1 < NT:
            ef_s1(step - 1)
        if step + 2 < NT:
            ef_load(step + 2)
        t3 = step - 3
        if 0 <= t3 < NT and t3 % 4 == 3:
            ef_s3(t3 // 4)
        t4 = step - 4
        if 0 <= t4 < NT and t4 % 4 == 3:
            ef_s4(t4 // 4)
        if 0 <= step - 2 < NT:
            ef_s2(step - 2)
    dbg("h2T", h2T[:], [128, 8, S], [h2T.b(g) for g in range(4)])
    dbg("wts", wts[:], [128, NT, NE], [wts.b(i) for i in range(NT)])
    k.barrier()

    if stop_after == 'EF':
        k.barrier()
        return nc, stack, dbg_out
    o = M0
    actb = [k.alloc([128, 4, S], BF16, off=o + i * 16 * KB, name="act%d" % i) for i in range(2)]; o += 32 * KB
    wup = [k.alloc([128, 8, 2, 512], BF16, off=o + i * 16 * KB, name="wup%d" % i) for i in range(2)]; o += 32 * KB
    wdn = [k.alloc([128, 4, D], BF16, off=o + i * 8 * KB, name="wdn%d" % i) for i in range(2)]; o += 16 * KB
    TG_OFF = o
    tG = [k.alloc([128, 512], F32, off=o + i * 2 * KB, name="tG%d" % i) for i in range(2)]; o += 4 * KB
    tS = [k.alloc([128, 512], BF16, off=o + i * KB, name="tS%d" % i) for i in range(2)]; o += 2 * KB
    TL_OFF = o
    tL = [k.alloc([128, 512], F32, off=o + i * 2 * KB, name="tL%d" % i) for i in range(2)]; o += 4 * KB
    wT = k.alloc([32, 128], F32, off=o, name="wT"); o += 512
    BDN_OFF = o
    bdn = k.alloc([32, D], BF16, off=o, name="bdn"); o += 4 * KB
    assert o <= SB_TOP, (o, SB_TOP)
    MOE_END = o

    NP = NE * 2 if n_experts_dbg is None else n_experts_dbg * 2

    def load_up(n):
        e_, p_ = n // 2, n % 2
        src = wup_d[e_].rearrange("(k p) (t f) -> p k t f", p=128, t=2)
        for t in range(2):
            k.dma("pool", wup[n % 2][:, :, t, :], src[:, :, t, p_ * 512:(p_ + 1) * 512], writes=[wup[n % 2].b()])

    def load_dn(n):
        e_, p_ = n // 2, n % 2
        k.dma("pool", wdn[n % 2][:], wdn_d[e_, p_ * 512:(p_ + 1) * 512, :].rearrange("(kk p) d -> p kk d", p=128),
              writes=[wdn[n % 2].b()])

    load_up(0)
    load_dn(0)
    k.dma("pool", bdn[:], bdn_d[:, :], writes=[bdn.b()])
    wTh = k.alloc([32, 1024], BF16, off=BDN_OFF + 2 * KB, name="wTh")

    def yinit_T(half):
        pt = PS[2]
        for t8 in range(8):
            tt = half * 8 + t8
            k.op("pe", lambda e, tt=tt, t8=t8: e.transpose(
                out=pt[0:32, t8 * 128:(t8 + 1) * 128], in_=wts[:, tt, :], identity=identf[:]),
                reads=[wts.b(tt), identf.b()], writes=[pt.b("d")])
        k.op("act", lambda e: e.activation(out=wTh[:], in_=pt[0:32, :], func=AF.Copy),
             reads=[pt.b("d")], writes=[wTh.b()])

    def yinit_M(half):
        for t8 in range(8):
            tt = half * 8 + t8
            yt = PS[3 - t8 % 2]
            for hh in range(2):
                k.op("pe", lambda e, hh=hh, yt=yt, t8=t8: e.matmul(
                    yt[:, hh * 512:(hh + 1) * 512], lhsT=wTh[:, t8 * 128:(t8 + 1) * 128], rhs=bdn[:, hh * 512:(hh + 1) * 512],
                    start=True, stop=True),
                    reads=[wTh.b(), bdn.b()], writes=[yt.b("d")])
            k.op("act", lambda e, tt=tt, yt=yt: e.activation(out=yacc[:, tt, :], in_=yt[:, :], func=AF.Copy),
                 reads=[yt.b("d")], writes=[yacc.b(tt)])

    yinit_T(0)

    unit = [0]
    vcnt = [0]

    def up(n):
        e_, p_ = n // 2, n % 2
        wb = wup[n % 2]
        ab = actb[n % 2]
        for j in range(4):
            cg = 117 + e_ * 16 + 4 * p_ + j
            cl = e_ * 16 + 8 + 4 * p_ + j
            for tg in range(4):
                u = unit[0]; unit[0] += 1
                gt, gc0 = bank((u % 2) * 2)
                lt, lc0 = bank((u % 2) * 2 + 1)
                for kk in range(8):
                    k.op("pe", lambda e, kk=kk, j=j, tg=tg, gt=gt, gc0=gc0, wb=wb: e.matmul(
                        gt[:, gc0:gc0 + 512], lhsT=wb[:, kk, 0, j * 128:(j + 1) * 128], rhs=h2T[:, kk, tg * 512:(tg + 1) * 512],
                        start=(kk == 0), stop=(kk == 7)), reads=[wb.b(), h2T.b(tg)], writes=[gt.b(("u", gc0))])
                for kk in range(8):
                    k.op("pe", lambda e, kk=kk, j=j, tg=tg, lt=lt, lc0=lc0, wb=wb: e.matmul(
                        lt[:, lc0:lc0 + 512], lhsT=wb[:, kk, 1, j * 128:(j + 1) * 128], rhs=h2T[:, kk, tg * 512:(tg + 1) * 512],
                        start=(kk == 0), stop=(kk == 7)), reads=[wb.b(), h2T.b(tg)], writes=[lt.b(("u", lc0))])
                g_, s_, l_ = tG[u % 2], tS[u % 2], tL[u % 2]
                k.op("dve", lambda e, gt=gt, gc0=gc0, g_=g_, cg=cg: e.tensor_scalar(
                    out=g_[:], in0=gt[:, gc0:gc0 + 512], scalar1=cols[:, cg:cg + 1], scalar2=7.0, op0=ALU.add, op1=ALU.min),
                    reads=[gt.b(("u", gc0)), cols.b()], writes=[g_.b()])
                k.op("act", lambda e, g_=g_, s_=s_: e.activation(out=s_[:], in_=g_[:], func=AF.Gelu_apprx_sigmoid),
                     reads=[g_.b()], writes=[s_.b()])
                k.op("dve", lambda e, lt=lt, lc0=lc0, l_=l_, cl=cl: e.tensor_scalar(
                    out=l_[:], in0=lt[:, lc0:lc0 + 512], scalar1=b1c[:, cl:cl + 1], scalar2=8.0, op0=ALU.add, op1=ALU.min),
                    reads=[lt.b(("u", lc0)), b1c.b()], writes=[l_.b()])
                k.op("dve", lambda e, l_=l_, s_=s_, ab=ab, j=j, tg=tg: e.scalar_tensor_tensor(
                    out=ab[:, j, tg * 512:(tg + 1) * 512], in0=l_[:], scalar=-6.0, in1=s_[:], op0=ALU.max, op1=ALU.mult),
                    reads=[l_.b(), s_.b()], writes=[ab.b(tg)])

    FO = M0 + 32 * KB
    xf = [k.alloc([128, D], F32, off=FO + i * 4 * KB, name="xf%d" % i) for i in range(3)]
    tqf = [k.alloc([128, D], F32, off=FO + 12 * KB + i * 4 * KB, name="tf%d" % i) for i in range(2)]
    otf = [k.alloc([128, D], F32, off=FO + 20 * KB + i * 4 * KB, name="of%d" % i) for i in range(2)]
    junkf = [k.alloc([128, D], BF16, off=FO + 28 * KB + i * 2 * KB, name="junkf%d" % i) for i in range(2)]
    st3f = k.alloc([128, NT * 4], F32, off=MOE_END, name="st3f")
    assert MOE_END + 256 <= SB_TOP

    def final_load(i):
        k.dma("sp", xf[i % 3][:], x2_d[i * 128:(i + 1) * 128, :], reads=[x2sc.b(i)], writes=[xf[i % 3].b()])

    def final_tile(i):
        xt = xf[i % 3]
        sc = st3f[:, i * 4:i * 4 + 4]
        sb = st3f.b(i)
        jk = junkf[i % 2]
        k.op("act", lambda e, i=i, sc=sc, jk=jk: e.activation(out=jk[:], in_=yacc[:, i, :], func=AF.Square, accum_out=sc[:, 0:1]),
             reads=[yacc.b(i)], writes=[jk.b(), sb])
        k.op("act", lambda e, sc=sc: e.activation(out=sc[:, 1:2], in_=sc[:, 0:1], func=AF.Sqrt, scale=1.0 / D, bias=EPS),
             reads=[sb], writes=[sb])
        k.op("dve", lambda e, sc=sc: e.reciprocal(out=sc[:, 2:3], in_=sc[:, 1:2]), reads=[sb], writes=[sb])
        tq = tqf[i % 2]
        k.op("dve", lambda e, i=i, sc=sc, tq=tq: e.scalar_tensor_tensor(
            out=tq[:], in0=yacc[:, i, :], scalar=sc[:, 2:3], in1=Grow_f[:], op0=ALU.mult, op1=ALU.mult),
            reads=[yacc.b(i), sb, Grow_f.b()], writes=[tq.b()])
        ot = otf[i % 2]
        k.op("pool", lambda e, tq=tq, xt=xt, ot=ot: e.tensor_tensor(out=ot[:], in0=tq[:], in1=xt[:], op=ALU.add),
             reads=[tq.b(), xt.b()], writes=[ot.b()])
        k.dma("sp", out_d[i * 128:(i + 1) * 128, :], ot[:], reads=[ot.b()])
        if i + 2 < NT:
            final_load(i + 2)

    def down(n):
        e_, p_ = n // 2, n % 2
        wb = wdn[n % 2]
        ab = actb[n % 2]
        last = (n == NP - 1)
        if last:
            for en in ("sp", "act", "dve", "pool"):
                k._wait(en, {"pe": k.cnt["pe"]})
            final_load(0)
            final_load(1)
        for tt in range(NT):
            v = vcnt[0]; vcnt[0] += 1
            yt = PS[2 + v % 2]
            for hh in range(2):
                for kk in range(4):
                    k.op("pe", lambda e, tt=tt, hh=hh, kk=kk, yt=yt, wb=wb, ab=ab: e.matmul(
                        yt[:, hh * 512:(hh + 1) * 512], lhsT=ab[:, kk, tt * 128:(tt + 1) * 128], rhs=wb[:, kk, hh * 512:(hh + 1) * 512],
                        start=(kk == 0), stop=(kk == 3)), reads=[ab.b(tt // 4), wb.b()], writes=[yt.b("d")])
            k.op("dve", lambda e, tt=tt, yt=yt, e_=e_: e.scalar_tensor_tensor(
                out=yacc[:, tt, :], in0=yt[:, :], scalar=wts[:, tt, e_:e_ + 1], in1=yacc[:, tt, :], op0=ALU.mult, op1=ALU.add),
                reads=[yt.b("d"), wts.b(tt), yacc.b(tt)], writes=[yacc.b(tt)])
            if last:
                final_tile(tt)

    for n in range(NP + 1):
        if n < NP:
            if n + 1 < NP:
                load_up(n + 1)
            up(n)
            if n == 0:
                yinit_M(0)
                yinit_T(1)
                yinit_M(1)
        if n >= 1:
            down(n - 1)
        if n + 1 < NP:
            load_dn(n + 1)
    dbg("yacc", yacc[:], [128, NT, D], [yacc.b(i) for i in range(NT)])
    k.barrier()
    return nc, stack, dbg_out


def _prep_shared(inp):
    f = np.float32
    sh = {}
    sh["w_ada"] = np.ascontiguousarray(inp["w_ada"][0], f)
    sh["w_in"] = np.ascontiguousarray(inp["w_in"][0], f)
    sh["w_out"] = np.ascontiguousarray(inp["w_out"][0], f)
    sh["w_router"] = np.ascontiguousarray(inp["w_router"][0], f)
    sh["w_up"] = np.ascontiguousarray(inp["w_up"][0], f)
    sh["w_down"] = np.ascontiguousarray(inp["w_down"][0], f)
    sh["b_down"] = np.ascontiguousarray(inp["b_down"][0], f)
    sh["b_router"] = np.ascontiguousarray(inp["b_router"][0].reshape(1, NE), f)
    b_ada = inp["b_ada"][0]
    rows = np.zeros((4, D), f)
    rows[0] = b_ada[2 * D:3 * D]
    rows[1] = b_ada[5 * D:6 * D]
    rows[2] = inp["norm_mix_post"][0]
    rows[3] = inp["norm_ffn_post"][0]
    sh["rows"] = rows
    bd = np.zeros((128, 2, 4, 128), f)
    for a, w in enumerate((inp["lru_wa"][0], inp["lru_wx"][0])):
        for c in range(4):
            for hlf in range(2):
                bd[hlf * 64:(hlf + 1) * 64, a, c, hlf * 64:(hlf + 1) * 64] = w[2 * c + hlf]
    sh["bdiag"] = bd.reshape(128, -1)
    sel = np.zeros((97, 2, 8, 70), f)
    for h in range(8):
        sel[h, 0, h, 64] = 8.0
        sel[32 + h, 0, h, 65] = 8.0
        sel[64 + h, 0, h, 66] = 8.0
        sel[96, 0, h, 67:70] = 8.0
        sel[96, 1, h, 64:67] = 1.0
        sel[h, 1, h, 67] = -1.0
        sel[32 + h, 1, h, 68] = -1.0
        sel[64 + h, 1, h, 69] = -1.0
    sh["sel"] = sel.reshape(97, -1)
    cols = np.zeros((128, NCOL), f)
    colT = lambda v: np.asarray(v, f).reshape(-1, 128).T
    cols[:, 8:56] = colT(b_ada)
    cols[:, 56:64] = colT(inp["norm_mix_pre"][0])
    cols[:, 64:72] = colT(inp["norm_ffn_pre"][0])
    cw = inp["conv_w"][0]
    for c in range(4):
        for j in range(4):
            cols[:, 72 + c * 4 + j] = cw[j, c * 128:(c + 1) * 128]
    cols[:, 88:92] = colT(inp["conv_b"][0])
    cols[:, 92:96] = colT(inp["lru_ba"][0].reshape(-1))
    cols[:, 96:100] = colT(inp["lru_bx"][0].reshape(-1))
    cols[:, 100:104] = colT(inp["lru_lambda"][0])
    cols[:, 104:108] = colT(inp["gn_lru"][0])
    cols[:, 108:112] = colT(inp["gn_attn"][0])
    fb = inp["attn_fb"][0]
    for r0 in (0, 32, 64):
        cols[r0:r0 + 8, 112] = fb
    cols[0:8, 113] = 1.0
    cols[32:40, 114] = 1.0
    cols[64:72, 115] = 1.0
    cols[96, 116] = 1.0
    bu = inp["b_up"][0]
    cols[:, 117:629] = bu.reshape(NE, 16, 128).transpose(2, 0, 1).reshape(128, NE * 16)
    return sh, cols


_CACHE = {}


def kernel(**inputs):
    inp = {k_: np.asarray(v) for k_, v in inputs.items()}
    n = 8
    sh, cols0 = _prep_shared(inp)
    if "prog" not in _CACHE:
        _CACHE["prog"] = build_program()
    nc, stack, _ = _CACHE["prog"]
    in_maps = []
    for b in range(n):
        m = dict(sh)
        m["x"] = np.ascontiguousarray(inp["x"][b], np.float32)
        cols = cols0.copy()
        cols[:, 0:8] = np.asarray(inp["c"][b], np.float32).reshape(8, 128).T
        m["cols"] = cols
        in_maps.append(m)
    res = run_bass_kernel_spmd(nc, in_maps, core_ids=list(range(n)))
    out = np.stack([np.asarray(r["out"], np.float32) for r in res.results], axis=0)
    return out
```

```python
import os
import numpy as np
import concourse.bass as bass
import concourse.mybir as mybir
from concourse.bass_utils import run_bass_kernel_spmd

F32 = mybir.dt.float32
BF16 = mybir.dt.bfloat16
AF = mybir.ActivationFunctionType
ALU = mybir.AluOpType

D = 1024
S = 2048
NT = 16
NE = 32
EPS = 1e-6
SB_BASE = 16512
SB_TOP = 229344
SAME_SYNC = True
KSKIP = os.environ.get('KSKIP', '').split(',')
NCOL = 629
DT_SIZE = {F32: 4, BF16: 2}


class Buf:
    __slots__ = ("w", "r")

    def __init__(self):
        self.w = None
        self.r = {}


class Tile:
    def __init__(self, h):
        self.h = h
        self.bufs = {}

    def ap(self):
        return self.h.ap()

    def __getitem__(self, key):
        return self.h.ap()[key]

    def b(self, key=None):
        if key not in self.bufs:
            self.bufs[key] = Buf()
        return self.bufs[key]


class K:
    def __init__(self, nc, stack):
        self.nc = nc
        self.eng = {"pe": nc.tensor, "act": nc.scalar, "dve": nc.vector, "pool": nc.gpsimd, "sp": nc.sync}
        self.sems = {}
        self.cnt = {}
        self.waited = {e: {} for e in self.eng}
        for e in self.eng:
            self.sems[e] = stack.enter_context(nc.semaphore("s_" + e))
            self.cnt[e] = 0
        self.R = 8
        self.dma_n = {}
        for q in ("sp", "pool", "act"):
            self.dma_n[q] = 0
            for i in range(self.R):
                self.sems[("ring", q, i)] = stack.enter_context(nc.semaphore("r_%s_%d" % (q, i)))
        self.off = SB_BASE
        self.names = 0

    def alloc(self, shape, dtype, off=None, name=None):
        size = int(np.prod(shape[1:])) * DT_SIZE[dtype]
        size = (size + 63) // 64 * 64
        if off is None:
            off = self.off
            self.off += size
        assert off + size <= SB_TOP, ("SBUF overflow", name, off, size)
        self.names += 1
        h = self.nc.alloc_sbuf_tensor_at("%s_%d" % (name or "t", self.names), list(shape), dtype, offset=off)
        return Tile(h)

    def _deps(self, reads, writes):
        deps = {}

        def add(tok):
            if tok is None:
                return
            k, v = tok
            if deps.get(k, 0) < v:
                deps[k] = v

        for b in reads:
            add(b.w)
        for b in writes:
            add(b.w)
            for k, v in b.r.items():
                add((k, v))
        return deps

    def _wait(self, e, deps):
        w = self.waited[e]
        for k, v in deps.items():
            if k == e and (not SAME_SYNC or e == "pe" or e == "sp"):
                continue
            if w.get(k, 0) < v:
                self.eng[e].wait_ge(self.sems[k], v)
                w[k] = v

    def _commit(self, tok, reads, writes):
        k, v = tok
        for b in writes:
            b.w = tok
            b.r = {}
        for b in reads:
            if b.r.get(k, 0) < v:
                b.r[k] = v

    def op(self, e, fn, reads=(), writes=()):
        self._wait(e, self._deps(reads, writes))
        ins = fn(self.eng[e])
        self.cnt[e] += 1
        ins.then_inc(self.sems[e], 1)
        self._commit((e, self.cnt[e]), reads, writes)

    def dma(self, q, out, in_, reads=(), writes=(), **kw):
        n = self.dma_n[q]
        slot = n % self.R
        val = 16 * (n // self.R + 1)
        key = ("ring", q, slot)
        deps = self._deps(reads, writes)
        if val > 16:
            deps[key] = max(deps.get(key, 0), val - 16)
        self._wait(q, deps)
        ins = self.eng[q].dma_start(out=out, in_=in_, **kw)
        ins.then_inc(self.sems[key], 16)
        self.dma_n[q] = n + 1
        self._commit((key, val), reads, writes)

    def all_tokens(self):
        toks = {}
        for e in self.eng:
            if self.cnt[e] > 0:
                toks[e] = self.cnt[e]
        for q, n in self.dma_n.items():
            for i in range(min(n, self.R)):
                last = n - 1 - ((n - 1 - i) % self.R)
                toks[("ring", q, i)] = 16 * (last // self.R + 1)
        return toks

    def barrier(self, engines=("pe", "act", "dve", "pool", "sp")):
        toks = self.all_tokens()
        for e in engines:
            w = self.waited[e]
            for k, v in toks.items():
                if k == e:
                    continue
                if w.get(k, 0) < v:
                    self.eng[e].wait_ge(self.sems[k], v)
                    w[k] = v


def build_program(debug=(), n_experts_dbg=None, stop_after=None):
    from contextlib import ExitStack
    nc = bass.Bass("TRN2", target_bir_lowering=False)
    stack = ExitStack()

    def din(name, shape, dt=F32):
        return nc.dram_tensor(name, list(shape), dt, kind="ExternalInput").ap()

    x_d = din("x", [S, D])
    cols_d = din("cols", [128, NCOL])
    rows_d = din("rows", [4, D])
    brout_d = din("b_router", [1, NE])
    wada_d = din("w_ada", [D, 6 * D])
    win_d = din("w_in", [D, 2568])
    bd_d = din("bdiag", [128, 2 * 4 * 128])
    sel_d = din("sel", [97, 2 * 8 * 70])
    wout_d = din("w_out", [D, D])
    wr_d = din("w_router", [D, NE])
    wup_d = din("w_up", [NE, D, 2 * D])
    wdn_d = din("w_down", [NE, D, D])
    bdn_d = din("b_down", [NE, D])
    out_d = nc.dram_tensor("out", [S, D], F32, kind="ExternalOutput").ap()
    x2_d = nc.dram_tensor("x2_scratch", [S, D], F32, kind="ExternalOutput").ap()
    x2sc = Tile(None)
    dbg_out = {}

    k = K(nc, stack)
    KB = 1024

    PS = [Tile(stack.enter_context(nc.psum_tensor("ps%d" % i, [128, 1024], F32))) for i in range(4)]

    def bank(i):
        return PS[i // 2], (i % 2) * 512

    identf = k.alloc([128, 128], F32, name="identf")
    identb = k.alloc([128, 128], BF16, name="identb")
    onesf = k.alloc([128, 128], F32, name="onesf")
    maskT = k.alloc([128, 128], BF16, name="maskT")
    cols = k.alloc([128, NCOL], F32, name="cols")
    modT = k.alloc([128, 48], F32, name="modT")
    AS = k.alloc([128, 32], F32, name="AS")
    lam = k.alloc([128, 16], F32, name="lam")
    b1c = k.alloc([128, 512], F32, name="b1c")
    Grow_m = k.alloc([128, D], F32, name="Grow_m")
    Grow_f = k.alloc([128, D], F32, name="Grow_f")
    brout = k.alloc([128, NE], F32, name="brout")
    P_END = k.off
    B0 = P_END

    def dbg(name, tile_ap, shape, reads):
        if name not in debug:
            return
        d = nc.dram_tensor("dbg_" + name, list(shape), tile_ap.dtype, kind="ExternalOutput").ap()
        dbg_out[name] = d
        k.dma("sp", d, tile_ap, reads=reads)

    k.op("pool", lambda e: e.memset(identf[:], 1.0), writes=[identf.b()])
    k.op("pool", lambda e: e.affine_select(out=identf[:], in_=identf[:], pattern=[[1, 128]],
                                           compare_op=ALU.is_equal, fill=0.0, base=0, channel_multiplier=-1),
         reads=[identf.b()], writes=[identf.b()])
    k.op("pool", lambda e: e.tensor_copy(out=identb[:], in_=identf[:]), reads=[identf.b()], writes=[identb.b()])
    k.op("pool", lambda e: e.memset(onesf[:], 1.0), writes=[onesf.b()])
    k.op("pool", lambda e: e.memset(maskT[:], 0.0), writes=[maskT.b()])
    k.op("pool", lambda e: e.affine_select(out=maskT[:], in_=maskT[:], pattern=[[1, 128]],
                                           compare_op=ALU.is_ge, fill=-30000.0, base=0, channel_multiplier=-1),
         reads=[maskT.b()], writes=[maskT.b()])
    k.dma("sp", cols[:], cols_d[:, :], writes=[cols.b()])
    k.dma("sp", brout[:], brout_d[0, :].partition_broadcast(128), writes=[brout.b()])
    k.dma("sp", Grow_m[:], rows_d[2, :].partition_broadcast(128), writes=[Grow_m.b()])
    k.dma("sp", Grow_f[:], rows_d[3, :].partition_broadcast(128), writes=[Grow_f.b()])

    o = B0
    hT = k.alloc([128, 8, S], BF16, off=o, name="hT"); o += 32 * KB
    W_A = k.alloc([128, 8, 1024], BF16, off=o, name="W_A"); o += 16 * KB
    merged = k.alloc([128, 8, S], BF16, off=o, name="merged"); o += 32 * KB
    O_ATT = o
    W_QK = k.alloc([128, 8, 16, 70], BF16, off=o, name="W_QK"); o += 17920
    W_V = k.alloc([128, 8, 512], BF16, off=o, name="W_V"); o += 8 * KB
    W_F = k.alloc([128, 8, 97], BF16, off=o, name="W_F"); o += 1600
    vp = k.alloc([128, NT, 8, 65], BF16, off=o, name="vp"); o += 16640
    CP_OFF = o
    CP = k.alloc([97, S], BF16, off=o, name="CP"); o += 4 * KB
    SEL = k.alloc([97, 2, 8, 70], BF16, off=o, name="SEL"); o += 2240
    R0 = o

    win_v = win_d.rearrange("(k p) n -> p k n", p=128)
    k.dma("pool", SEL[:], sel_d.rearrange("p (a h c) -> p a h c", a=2, h=8), writes=[SEL.b()])

    MRG = B0 + 48 * KB
    xin4 = [k.alloc([128, D], F32, off=MRG + i * 4 * KB, name="xin4_%d" % i) for i in range(4)]
    xn = [k.alloc([128, D], F32, off=MRG + 16 * KB + i * 4 * KB, name="xn%d" % i) for i in range(4)]
    junk = k.alloc([128, D], BF16, off=CP_OFF, name="junk")
    st = k.alloc([128, 64], F32, off=CP_OFF + 2 * KB, name="st")

    def pre_B(g):
        for tt in range(4):
            i = g * 4 + tt
            xt = xin4[tt]
            k.dma("sp", xt[:], x_d[i * 128:(i + 1) * 128, :], writes=[xt.b()])
        for tt in range(4):
            xt = xin4[tt]
            sc = st[:, tt * 4:tt * 4 + 4]
            k.op("act", lambda e, xt=xt, sc=sc: e.activation(out=junk[:], in_=xt[:], func=AF.Square, accum_out=sc[:, 0:1]),
                 reads=[xt.b()], writes=[junk.b(), st.b(tt)])
            k.op("act", lambda e, sc=sc: e.activation(out=sc[:, 1:2], in_=sc[:, 0:1], func=AF.Sqrt, scale=1.0 / D, bias=EPS),
                 reads=[st.b(tt)], writes=[st.b(tt)])
            k.op("dve", lambda e, sc=sc: e.reciprocal(out=sc[:, 2:3], in_=sc[:, 1:2]), reads=[st.b(tt)], writes=[st.b(tt)])
            k.op("dve", lambda e, xt=xt, sc=sc, tt=tt: e.tensor_scalar(out=xn[tt][:], in0=xt[:], scalar1=sc[:, 2:3],
                                                                       scalar2=None, op0=ALU.mult),
                 reads=[xt.b(), st.b(tt)], writes=[xn[tt].b()])

    def post_B(g):
        a0 = 0
        for kk in range(8):
            bt, c0 = bank(kk % 2)
            for tt in range(4):
                k.op("pe", lambda e, kk=kk, tt=tt, bt=bt, c0=c0: e.transpose(
                    out=bt[:, c0 + tt * 128:c0 + (tt + 1) * 128], in_=xn[tt][:, kk * 128:(kk + 1) * 128], identity=identf[:]),
                    reads=[xn[tt].b(), identf.b()], writes=[bt.b(kk % 2)])
            k.op("dve", lambda e, kk=kk, bt=bt, c0=c0: e.tensor_scalar(
                out=hT[:, kk, g * 512:(g + 1) * 512], in0=bt[:, c0:c0 + 512],
                scalar1=AS[:, a0 + kk:a0 + kk + 1], scalar2=AS[:, a0 + 8 + kk:a0 + 9 + kk], op0=ALU.mult, op1=ALU.add),
                reads=[bt.b(kk % 2), AS.b()], writes=[hT.b(g)])

    pre_B(0)
    o = R0
    wada = [k.alloc([128, 8, 1024], BF16, off=o + i * 16 * KB, name="wada%d" % i) for i in range(3)]
    o += 48 * KB
    csil = k.alloc([128, 8], F32, off=o, name="csil"); o += 64
    cb = k.alloc([128, 8, 128], BF16, off=o, name="cb"); o += 2 * KB
    brow = k.alloc([128, 2, D], F32, off=o, name="brow"); o += 8 * KB

    k.op("act", lambda e: e.activation(out=csil[:], in_=cols[:, 0:8], func=AF.Silu), reads=[cols.b()], writes=[csil.b()])
    for kk in range(8):
        k.op("dve", lambda e, kk=kk: e.tensor_copy(out=cb[:, kk, :], in_=csil[:, kk:kk + 1].to_broadcast([128, 128])),
             reads=[csil.b()], writes=[cb.b()])
    wada_v = wada_d.rearrange("(k p) n -> p k n", p=128)
    pst, _ = bank(0)

    def adaln_cols(g, wb, cbt, pt, pc0, pkey):
        for j in range(8):
            for kk in range(8):
                k.op("pe", lambda e, j=j, kk=kk: e.matmul(
                    pt[:, pc0 + j:pc0 + j + 1], lhsT=wb[:, kk, j * 128:(j + 1) * 128], rhs=cbt[:, kk, 0:1],
                    start=(kk == 0), stop=(kk == 7)), reads=[wb.b(), cbt.b()], writes=[pkey])

    def adaln_gate(g, wb, cbt, browt):
        gt = PS[1]
        for hh in range(2):
            for kk in range(8):
                k.op("pe", lambda e, hh=hh, kk=kk: e.matmul(
                    gt[:, hh * 512:(hh + 1) * 512], lhsT=cbt[:, kk, :], rhs=wb[:, kk, hh * 512:(hh + 1) * 512],
                    start=(kk == 0), stop=(kk == 7)), reads=[wb.b(), cbt.b()], writes=[gt.b(hh * 512)])
        G = Grow_m if g == 2 else Grow_f
        bi = 0 if g == 2 else 1
        k.op("dve", lambda e: e.tensor_tensor(out=browt[:, bi, :], in0=gt[:, :], in1=browt[:, bi, :], op=ALU.add),
             reads=[gt.b(0), gt.b(512), browt.b()], writes=[browt.b()])
        k.op("dve", lambda e: e.tensor_tensor(out=G[:], in0=G[:], in1=browt[:, bi, :], op=ALU.mult),
             reads=[browt.b(), G.b()], writes=[G.b()])

    def adaln_AS(a0, g0, sc0, sh0):
        k.op("dve", lambda e: e.scalar_tensor_tensor(
            out=AS[:, a0:a0 + 8], in0=modT[:, sc0:sc0 + 8], scalar=1.0, in1=cols[:, g0:g0 + 8],
            op0=ALU.add, op1=ALU.mult), reads=[modT.b(), cols.b()], writes=[AS.b()])
        k.op("dve", lambda e: e.tensor_copy(out=AS[:, a0 + 8:a0 + 16], in_=modT[:, sh0:sh0 + 8]),
             reads=[modT.b()], writes=[AS.b()])

    for g in range(2):
        wb = wada[g]
        k.dma("pool", wb[:], wada_v[:, :, g * 1024:(g + 1) * 1024], writes=[wb.b()])
    k.dma("pool", W_A[:], win_v[:, :, 0:1024], writes=[W_A.b()])
    k.dma("pool", W_V[:], win_v[:, :, 2048:2560], writes=[W_V.b()])
    for g in range(2):
        adaln_cols(g, wada[g], cb, pst, 8 * g, pst.b(0))
    k.op("dve", lambda e: e.tensor_tensor(out=modT[:, 0:16], in0=pst[:, 0:16], in1=cols[:, 8:24], op=ALU.add),
         reads=[pst.b(0), cols.b()], writes=[modT.b()])
    adaln_AS(0, 56, 8, 0)
    L = cols[:, 100:104]
    k.op("act", lambda e: e.activation(out=lam[:, 8:12], in_=L, func=AF.Abs),
         reads=[cols.b()], writes=[lam.b()])
    k.op("act", lambda e: e.activation(out=lam[:, 8:12], in_=lam[:, 8:12], func=AF.Exp, scale=-1.0),
         reads=[lam.b()], writes=[lam.b()])
    k.op("act", lambda e: e.activation(out=lam[:, 8:12], in_=lam[:, 8:12], func=AF.Ln, bias=1.0),
         reads=[lam.b()], writes=[lam.b()])
    k.op("dve", lambda e: e.tensor_scalar(out=lam[:, 12:16], in0=L, scalar1=-1.0, scalar2=0.0, op0=ALU.mult, op1=ALU.max),
         reads=[cols.b()], writes=[lam.b()])
    k.op("dve", lambda e: e.tensor_tensor(out=lam[:, 8:12], in0=lam[:, 8:12], in1=lam[:, 12:16], op=ALU.add),
         reads=[lam.b()], writes=[lam.b()])
    k.op("dve", lambda e: e.tensor_scalar(out=lam[:, 0:4], in0=lam[:, 8:12], scalar1=-8.0, scalar2=None, op0=ALU.mult),
         reads=[lam.b()], writes=[lam.b()])
    k.op("dve", lambda e: e.tensor_scalar(out=lam[:, 4:8], in0=lam[:, 8:12], scalar1=-16.0, scalar2=None, op0=ALU.mult),
         reads=[lam.b()], writes=[lam.b()])
    k.op("dve", lambda e: e.tensor_scalar(out=b1c[:], in0=cols[:, 117:629], scalar1=1.0, scalar2=None, op0=ALU.add),
         reads=[cols.b()], writes=[b1c.b()])
    k.barrier()

    if stop_after == 'A':
        k.barrier()
        return nc, stack, dbg_out
    o = R0
    qkst = k.alloc([128, 8, 1024], BF16, off=o, name="qkst"); o += 16 * KB
    fst = k.alloc([128, 8, 8], BF16, off=o, name="fst"); o += 128
    O_F = o

    k.dma("pool", fst[:], win_v[:, :, 2560:2568], writes=[fst.b()])
    k.dma("pool", qkst[:], win_v[:, :, 1024:2048], writes=[qkst.b()])
    k.op("pool", lambda e: e.memset(W_F[:], 0.0), writes=[W_F.b()])
    for r0 in (0, 32, 64):
        k.op("pool", lambda e, r0=r0: e.tensor_copy(out=W_F[:, :, r0:r0 + 8], in_=fst[:]),
             reads=[fst.b()], writes=[W_F.b()])
    k.op("pool", lambda e: e.memset(vp[:, :, :, 64:65], 1.0), writes=[vp.b("ones")])

    def prenorm_group(g, src_tiles, a0, dstT, dstF32=None, xn=None, junk=None, st=None):
        for tt in range(4):
            xt, xb_ = src_tiles[tt]
            i = g * 4 + tt
            sc = st[:, tt * 4:tt * 4 + 4]
            k.op("act", lambda e, xt=xt, sc=sc: e.activation(out=junk[:], in_=xt[:], func=AF.Square, accum_out=sc[:, 0:1]),
                 reads=[xb_], writes=[junk.b(), st.b(tt)])
            k.op("act", lambda e, sc=sc: e.activation(out=sc[:, 1:2], in_=sc[:, 0:1], func=AF.Sqrt, scale=1.0 / D, bias=EPS),
                 reads=[st.b(tt)], writes=[st.b(tt)])
            k.op("dve", lambda e, sc=sc: e.reciprocal(out=sc[:, 2:3], in_=sc[:, 1:2]), reads=[st.b(tt)], writes=[st.b(tt)])
            k.op("dve", lambda e, xt=xt, sc=sc, tt=tt: e.tensor_scalar(out=xn[tt][:], in0=xt[:], scalar1=sc[:, 2:3],
                                                                       scalar2=None, op0=ALU.mult),
                 reads=[xb_, st.b(tt)], writes=[xn[tt].b()])
        for kk in range(8):
            bt, c0 = bank(kk % 2)
            for tt in range(4):
                k.op("pe", lambda e, kk=kk, tt=tt, bt=bt, c0=c0: e.transpose(
                    out=bt[:, c0 + tt * 128:c0 + (tt + 1) * 128], in_=xn[tt][:, kk * 128:(kk + 1) * 128], identity=identf[:]),
                    reads=[xn[tt].b(), identf.b()], writes=[bt.b(kk % 2)])
            k.op("dve", lambda e, kk=kk, bt=bt, c0=c0: e.tensor_scalar(
                out=dstT[:, kk, g * 512:(g + 1) * 512], in0=bt[:, c0:c0 + 512],
                scalar1=AS[:, a0 + kk:a0 + kk + 1], scalar2=AS[:, a0 + 8 + kk:a0 + 9 + kk], op0=ALU.mult, op1=ALU.add),
                reads=[bt.b(kk % 2), AS.b()], writes=[dstT.b(g)])
            if dstF32 is not None:
                k.op("dve", lambda e, kk=kk, bt=bt, c0=c0: e.tensor_scalar(
                    out=dstF32[:, kk, :], in0=bt[:, c0:c0 + 512],
                    scalar1=AS[:, a0 + kk:a0 + kk + 1], scalar2=AS[:, a0 + 8 + kk:a0 + 9 + kk], op0=ALU.mult, op1=ALU.add),
                    reads=[bt.b(kk % 2), AS.b()], writes=[dstF32.b()])

    o = O_F
    F1 = k.alloc([97, S], F32, off=o, name="F1"); o += 8 * KB
    F2 = k.alloc([97, S], F32, off=o, name="F2"); o += 8 * KB
    F3 = k.alloc([97, S], F32, off=o, name="F3"); o += 8 * KB
    CPh = k.alloc([97, S], BF16, off=o, name="CPh"); o += 4 * KB
    CPm = k.alloc([97, S], BF16, off=o, name="CPm"); o += 4 * KB
    CPl = k.alloc([97, S], BF16, off=o, name="CPl"); o += 4 * KB
    assert o <= SB_TOP
    fbcol = cols[0:97, 112:113]

    def vproj(i):
        bt, c0 = bank(2 + i % 2)
        for kk in range(8):
            k.op("pe", lambda e, i=i, kk=kk, bt=bt, c0=c0: e.matmul(
                bt[:, c0:c0 + 512], lhsT=hT[:, kk, i * 128:(i + 1) * 128], rhs=W_V[:, kk, :],
                start=(kk == 0), stop=(kk == 7)), reads=[hT.b(i // 4), W_V.b()], writes=[bt.b(i % 2)])
        k.op("act", lambda e, i=i, bt=bt, c0=c0: e.activation(
            out=vp[:, i, :, 0:64], in_=bt[:, c0:c0 + 512].rearrange("p (h c) -> p h c", c=64), func=AF.Copy),
            reads=[bt.b(i % 2)], writes=[vp.b(i)])

    def fproj(g):
        bt, c0 = bank(4 + g % 2)
        for kk in range(8):
            k.op("pe", lambda e, g=g, kk=kk, bt=bt, c0=c0: e.matmul(
                bt[0:97, c0:c0 + 512], lhsT=W_F[:, kk, :], rhs=hT[:, kk, g * 512:(g + 1) * 512],
                start=(kk == 0), stop=(kk == 7)), reads=[hT.b(g), W_F.b()], writes=[bt.b(g % 2)])
        k.op("dve", lambda e, g=g, bt=bt, c0=c0: e.tensor_scalar(
            out=F1[:, g * 512:(g + 1) * 512], in0=bt[0:97, c0:c0 + 512], scalar1=fbcol, scalar2=None, op0=ALU.add),
            reads=[bt.b(g % 2), cols.b()], writes=[F1.b()])

    for g in range(4):
        if g >= 1:
            pre_B(g)
        post_B(g)
        if g >= 1:
            for tt in range(4):
                vproj((g - 1) * 4 + tt)
            fproj(g - 1)
    for tt in range(4):
        vproj(12 + tt)
    fproj(3)
    dbg("hT", hT[:], [128, 8, S], [hT.b(g) for g in range(4)])
    k.op("act", lambda e: e.activation(out=F2[:], in_=F1[:], func=AF.Abs),
         reads=[F1.b()], writes=[F2.b()])
    k.op("act", lambda e: e.activation(out=F2[:], in_=F2[:], func=AF.Exp, scale=-1.0), reads=[F2.b()], writes=[F2.b()])
    k.op("act", lambda e: e.activation(out=F2[:], in_=F2[:], func=AF.Ln, bias=1.0), reads=[F2.b()], writes=[F2.b()])
    k.op("dve", lambda e: e.tensor_scalar(out=F1[:], in0=F1[:], scalar1=0.0, scalar2=None, op0=ALU.min),
         reads=[F1.b()], writes=[F1.b()])
    k.op("dve", lambda e: e.tensor_tensor(out=F1[:], in0=F1[:], in1=F2[:], op=ALU.subtract),
         reads=[F1.b(), F2.b()], writes=[F1.b()])
    k.op("pool", lambda e: e.memset(F3[:], 1.0), writes=[F3.b()])
    k.op("pool", lambda e: e.memset(W_QK[:], 0.0), writes=[W_QK.b()])
    k.op("pool", lambda e: e.tensor_copy(out=W_QK[:, :, :, 0:64],
                                         in_=qkst[:].rearrange("p k (h c) -> p k h c", c=64)),
         reads=[qkst.b()], writes=[W_QK.b()])
    k.op("dve", lambda e: e.tensor_tensor_scan(out=F2[:], data0=F3[:], data1=F1[:], initial=0.0, op0=ALU.mult, op1=ALU.add),
         reads=[F1.b(), F3.b()], writes=[F2.b()])
    dbg("C", F2[:], [97, S], [F2.b()])
    k.op("dve", lambda e: e.tensor_copy(out=CPh[:], in_=F2[:]), reads=[F2.b()], writes=[CPh.b()])
    k.op("dve", lambda e: e.tensor_tensor(out=F1[:], in0=F2[:], in1=CPh[:], op=ALU.subtract),
         reads=[F2.b(), CPh.b()], writes=[F1.b()])
    k.op("dve", lambda e: e.tensor_copy(out=CPm[:], in_=F1[:]), reads=[F1.b()], writes=[CPm.b()])
    k.op("dve", lambda e: e.tensor_tensor(out=F3[:], in0=F1[:], in1=CPm[:], op=ALU.subtract),
         reads=[F1.b(), CPm.b()], writes=[F3.b()])
    k.op("dve", lambda e: e.tensor_copy(out=CPl[:], in_=F3[:]), reads=[F3.b()], writes=[CPl.b()])
    mc = lambda j: cols[0:97, 113 + j:114 + j]
    k.op("dve", lambda e: e.tensor_scalar(out=CP[:], in0=CPh[:], scalar1=mc(0), scalar2=None, op0=ALU.mult),
         reads=[CPh.b(), cols.b()], writes=[CP.b()])
    k.op("dve", lambda e: e.scalar_tensor_tensor(out=CP[:], in0=CPm[:], scalar=mc(1), in1=CP[:], op0=ALU.mult, op1=ALU.add),
         reads=[CPm.b(), CP.b()], writes=[CP.b()])
    k.op("dve", lambda e: e.scalar_tensor_tensor(out=CP[:], in0=CPl[:], scalar=mc(2), in1=CP[:], op0=ALU.mult, op1=ALU.add),
         reads=[CPl.b(), CP.b()], writes=[CP.b()])
    k.op("dve", lambda e: e.tensor_scalar(out=CP[:], in0=CP[:], scalar1=mc(3), scalar2=None, op0=ALU.add),
         reads=[CP.b()], writes=[CP.b()])
    k.barrier()

    if stop_after == 'B':
        k.barrier()
        return nc, stack, dbg_out
    o = R0
    qk_t = [[k.alloc([70, S], BF16, off=o + (2 * s + j) * 4 * KB, name="qk%d%d" % (s, j)) for j in range(2)] for s in range(2)]
    o += 16 * KB
    PT = [k.alloc([128, 512], BF16, off=o + i * KB, name="PT%d" % i) for i in range(4)]; o += 4 * KB
    attn = k.alloc([128, NT, 512], F32, off=o, name="attn"); o += 32 * KB
    rden = k.alloc([128, 8], F32, off=o, name="rden"); o += 64
    ant = [k.alloc([128, 512], F32, off=o + i * 2 * KB, name="ant%d" % i) for i in range(4)]; o += 8 * KB
    st2 = k.alloc([128, 64], F32, off=o, name="st2"); o += 256
    junk2 = k.alloc([128, 512], BF16, off=o, name="junk2"); o += KB
    assert o <= SB_TOP

    def qk_proj(h):
        s = h % 2
        for j in range(2):
            dst = qk_t[s][j]
            for g in range(4):
                bt, c0 = bank(6 + g % 2)
                for kk in range(8):
                    k.op("pe", lambda e, j=j, g=g, kk=kk, bt=bt, c0=c0: e.matmul(
                        bt[0:70, c0:c0 + 512], lhsT=W_QK[:, kk, j * 8 + h, :], rhs=hT[:, kk, g * 512:(g + 1) * 512],
                        start=(kk == 0), stop=False), reads=[hT.b(g), W_QK.b()], writes=[bt.b(g % 2)])
                k.op("pe", lambda e, j=j, g=g, bt=bt, c0=c0: e.matmul(
                    bt[0:70, c0:c0 + 512], lhsT=SEL[:, j, h, :], rhs=CP[:, g * 512:(g + 1) * 512],
                    start=False, stop=True), reads=[CP.b(), SEL.b()], writes=[bt.b(g % 2)])
                if j == 0:
                    k.op("dve", lambda e, g=g, bt=bt, c0=c0, dst=dst: e.tensor_scalar(
                        out=dst[:, g * 512:(g + 1) * 512], in0=bt[0:70, c0:c0 + 512], scalar1=0.125, scalar2=None,
                        op0=ALU.mult), reads=[bt.b(g % 2)], writes=[dst.b()])
                else:
                    k.op("dve", lambda e, g=g, bt=bt, c0=c0, dst=dst: e.tensor_copy(
                        out=dst[:, g * 512:(g + 1) * 512], in_=bt[0:70, c0:c0 + 512]),
                        reads=[bt.b(g % 2)], writes=[dst.b()])

    def attn_head(h):
        s = h % 2
        qp, kp = qk_t[s]
        groups = []
        for i in range(NT):
            nb = i + 1
            for g0 in range(0, nb, 4):
                groups.append((i, list(range(g0, min(nb, g0 + 4)))))
        n = len(groups)

        def qk_mm(gi):
            i, blks = groups[gi]
            bt, c0 = bank(gi % 3)
            for jj, j in enumerate(blks):
                diag = (j == i)
                k.op("pe", lambda e, jj=jj, j=j, i=i, bt=bt, c0=c0, diag=diag: e.matmul(
                    bt[:, c0 + jj * 128:c0 + (jj + 1) * 128], lhsT=kp[:, j * 128:(j + 1) * 128],
                    rhs=qp[:, i * 128:(i + 1) * 128], start=True, stop=not diag),
                    reads=[kp.b(), qp.b()], writes=[bt.b(c0)])
                if diag:
                    k.op("pe", lambda e, jj=jj, bt=bt, c0=c0: e.matmul(
                        bt[:, c0 + jj * 128:c0 + (jj + 1) * 128], lhsT=identb[:], rhs=maskT[:],
                        start=False, stop=True), reads=[identb.b(), maskT.b()], writes=[bt.b(c0)])

        def exp_pv(gi):
            i, blks = groups[gi]
            bt, c0 = bank(gi % 3)
            nbk = len(blks)
            pt = PT[gi % 4]
            k.op("act", lambda e, bt=bt, c0=c0, nbk=nbk, pt=pt: e.activation(
                out=pt[:, 0:nbk * 128], in_=bt[:, c0:c0 + nbk * 128], func=AF.Exp),
                reads=[bt.b(c0)], writes=[pt.b()])
            ot, oc0 = bank(3 + (i % 2))
            for jj, j in enumerate(blks):
                k.op("pe", lambda e, jj=jj, j=j, i=i, ot=ot, oc0=oc0, pt=pt: e.matmul(
                    ot[:, oc0:oc0 + 65], lhsT=pt[:, jj * 128:(jj + 1) * 128], rhs=vp[:, j, h, :],
                    start=(j == 0), stop=(j == i)), reads=[pt.b(), vp.b(j), vp.b("ones")], writes=[ot.b(oc0)])
            if blks[-1] == i:
                k.op("dve", lambda e, ot=ot, oc0=oc0: e.reciprocal(out=rden[:, h:h + 1], in_=ot[:, oc0 + 64:oc0 + 65]),
                     reads=[ot.b(oc0)], writes=[rden.b(h)])
                k.op("dve", lambda e, ot=ot, oc0=oc0, i=i: e.tensor_scalar(
                    out=attn[:, i, h * 64:(h + 1) * 64], in0=ot[:, oc0:oc0 + 64], scalar1=rden[:, h:h + 1], scalar2=None,
                    op0=ALU.mult), reads=[ot.b(oc0), rden.b(h)], writes=[attn.b(i)])

        qk_mm(0)
        for gi in range(n):
            if gi + 1 < n:
                qk_mm(gi + 1)
            exp_pv(gi)

    qk_proj(0)
    for h in range(8):
        if h + 1 < 8:
            qk_proj(h + 1)
        attn_head(h)
    dbg("attn", attn[:], [128, NT, 512], [attn.b(i) for i in range(NT)])

    for g in range(4):
        for tt in range(4):
            i = g * 4 + tt
            sc = st2[:, tt * 4:tt * 4 + 4]
            k.op("act", lambda e, i=i, sc=sc: e.activation(out=junk2[:], in_=attn[:, i, :], func=AF.Square, accum_out=sc[:, 0:1]),
                 reads=[attn.b(i)], writes=[junk2.b(), st2.b(tt)])
            k.op("act", lambda e, sc=sc: e.activation(out=sc[:, 1:2], in_=sc[:, 0:1], func=AF.Sqrt, scale=1.0 / 512, bias=EPS),
                 reads=[st2.b(tt)], writes=[st2.b(tt)])
            k.op("dve", lambda e, sc=sc: e.reciprocal(out=sc[:, 2:3], in_=sc[:, 1:2]), reads=[st2.b(tt)], writes=[st2.b(tt)])
            k.op("dve", lambda e, i=i, sc=sc, tt=tt: e.tensor_scalar(out=ant[tt][:], in0=attn[:, i, :], scalar1=sc[:, 2:3],
                                                                     scalar2=None, op0=ALU.mult),
                 reads=[attn.b(i), st2.b(tt)], writes=[ant[tt].b()])
        for cc in range(4):
            bt, c0 = bank(cc % 2)
            for tt in range(4):
                k.op("pe", lambda e, cc=cc, tt=tt, bt=bt, c0=c0: e.transpose(
                    out=bt[:, c0 + tt * 128:c0 + (tt + 1) * 128], in_=ant[tt][:, cc * 128:(cc + 1) * 128], identity=identf[:]),
                    reads=[ant[tt].b(), identf.b()], writes=[bt.b(c0)])
            k.op("dve", lambda e, cc=cc, bt=bt, c0=c0, g=g: e.tensor_scalar(
                out=merged[:, 4 + cc, g * 512:(g + 1) * 512], in0=bt[:, c0:c0 + 512],
                scalar1=cols[:, 108 + cc:109 + cc], scalar2=None, op0=ALU.mult),
                reads=[bt.b(c0), cols.b()], writes=[merged.b(("a", g))])
    k.barrier()

    if stop_after == 'C':
        k.barrier()
        return nc, stack, dbg_out
    o = O_ATT
    xl = k.alloc([128, 4, S + 4], F32, off=o, name="xl"); o += 4 * (S + 4) * 4
    gl = k.alloc([128, 4, S], BF16, off=o, name="gl"); o += 16 * KB
    BD = k.alloc([128, 2, 4, 128], BF16, off=o, name="BD"); o += 2 * KB
    xc2 = [k.alloc([128, S], F32, off=o + i * 8 * KB, name="xc%d" % i) for i in range(2)]; o += 16 * KB
    xcb2 = [k.alloc([128, S], BF16, off=o + i * 4 * KB, name="xcb%d" % i) for i in range(2)]; o += 8 * KB
    T1 = k.alloc([128, S], F32, off=o, name="T1"); o += 8 * KB
    T2 = k.alloc([128, S], F32, off=o, name="T2"); o += 8 * KB
    T3 = k.alloc([128, S], F32, off=o, name="T3"); o += 8 * KB
    sq = [k.alloc([128, 512], F32, off=o + i * 2 * KB, name="sq%d" % i) for i in range(2)]; o += 4 * KB
    rs = [k.alloc([128, 512], F32, off=o + i * 2 * KB, name="rs%d" % i) for i in range(2)]; o += 4 * KB
    assert o <= SB_TOP, o
    k.dma("pool", BD[:], bd_d.rearrange("p (a c j) -> p a c j", a=2, c=4), writes=[BD.b()])
    k.op("pool", lambda e: e.memset(xl[:, :, 0:4], 0.0), writes=[xl.b("pad")])

    def lru_inproj(c):
        for g in range(4):
            bt, c0 = bank(g % 2)
            for kk in range(8):
                k.op("pe", lambda e, c=c, g=g, kk=kk, bt=bt, c0=c0: e.matmul(
                    bt[:, c0:c0 + 512], lhsT=W_A[:, kk, c * 128:(c + 1) * 128], rhs=hT[:, kk, g * 512:(g + 1) * 512],
                    start=(kk == 0), stop=(kk == 7)), reads=[hT.b(g), W_A.b()], writes=[bt.b(c0)])
            k.op("act", lambda e, c=c, g=g, bt=bt, c0=c0: e.activation(
                out=xl[:, c, 4 + g * 512:4 + (g + 1) * 512], in_=bt[:, c0:c0 + 512], func=AF.Copy),
                reads=[bt.b(c0)], writes=[xl.b(c)])
        for g in range(4):
            bt, c0 = bank(2 + g % 2)
            for kk in range(8):
                k.op("pe", lambda e, c=c, g=g, kk=kk, bt=bt, c0=c0: e.matmul(
                    bt[:, c0:c0 + 512], lhsT=W_A[:, kk, 512 + c * 128:512 + (c + 1) * 128], rhs=hT[:, kk, g * 512:(g + 1) * 512],
                    start=(kk == 0), stop=(kk == 7)), reads=[hT.b(g), W_A.b()], writes=[bt.b(c0)])
            k.op("act", lambda e, c=c, g=g, bt=bt, c0=c0: e.activation(
                out=gl[:, c, g * 512:(g + 1) * 512], in_=bt[:, c0:c0 + 512], func=AF.Gelu_apprx_tanh),
                reads=[bt.b(c0)], writes=[gl.b(c)])

    def lru_conv(c):
        xc = xc2[c % 2]
        xcb = xcb2[c % 2]
        cw = lambda j: cols[:, 72 + c * 4 + j:73 + c * 4 + j]
        k.op("dve", lambda e: e.tensor_scalar(out=xc[:], in0=xl[:, c, 4:4 + S], scalar1=cw(3), scalar2=cols[:, 88 + c:89 + c],
                                              op0=ALU.mult, op1=ALU.add),
             reads=[xl.b(c), xl.b("pad"), cols.b()], writes=[xc.b()])
        for j in range(3):
            sh = 3 - j
            k.op("dve", lambda e, j=j, sh=sh: e.scalar_tensor_tensor(
                out=xc[:], in0=xl[:, c, 4 - sh:4 - sh + S], scalar=cw(j), in1=xc[:], op0=ALU.mult, op1=ALU.add),
                reads=[xl.b(c), xl.b("pad"), xc.b()], writes=[xc.b()])
        k.op("pool", lambda e: e.tensor_copy(out=xcb[:], in_=xc[:]), reads=[xc.b()], writes=[xcb.b()])

    def lru_gates(c):
        xcb = xcb2[c % 2]
        for gate, dstT, bcol in ((0, T1, 92), (1, T3, 96)):
            for g in range(4):
                bt, c0 = bank(4 + (gate * 4 + g) % 4)
                k.op("pe", lambda e, gate=gate, g=g, bt=bt, c0=c0: e.matmul(
                    bt[:, c0:c0 + 512], lhsT=BD[:, gate, c, :], rhs=xcb[:, g * 512:(g + 1) * 512], start=True, stop=True),
                    reads=[BD.b(), xcb.b()], writes=[bt.b(c0)])
                k.op("act", lambda e, g=g, bt=bt, c0=c0, dstT=dstT, bcol=bcol: e.activation(
                    out=dstT[:, g * 512:(g + 1) * 512], in_=bt[:, c0:c0 + 512], func=AF.Sigmoid,
                    bias=cols[:, bcol + c:bcol + c + 1]), reads=[bt.b(c0), cols.b()], writes=[dstT.b()])
        k.op("act", lambda e: e.activation(out=T2[:], in_=T1[:], func=AF.Exp, scale=lam[:, c:c + 1]),
             reads=[T1.b(), lam.b()], writes=[T2.b()])
        k.op("act", lambda e: e.activation(out=T1[:], in_=T1[:], func=AF.Exp, scale=lam[:, 4 + c:5 + c]),
             reads=[T1.b(), lam.b()], writes=[T1.b()])
        k.op("act", lambda e: e.activation(out=T1[:], in_=T1[:], func=AF.Sqrt, scale=-1.0, bias=1.0),
             reads=[T1.b()], writes=[T1.b()])

    def lru_rest(c):
        xc = xc2[c % 2]
        k.op("dve", lambda e: e.tensor_tensor(out=T3[:], in0=T3[:], in1=xc[:], op=ALU.mult),
             reads=[T3.b(), xc.b()], writes=[T3.b()])
        k.op("dve", lambda e: e.tensor_tensor(out=T3[:], in0=T3[:], in1=T1[:], op=ALU.mult),
             reads=[T3.b(), T1.b()], writes=[T3.b()])
        k.op("dve", lambda e: e.tensor_tensor_scan(out=T1[:], data0=T2[:], data1=T3[:], initial=0.0, op0=ALU.mult, op1=ALU.add),
             reads=[T2.b(), T3.b(), T1.b()], writes=[T1.b()])
        k.op("dve", lambda e: e.tensor_tensor(out=xl[:, c, 4:4 + S], in0=T1[:], in1=gl[:, c, :], op=ALU.mult),
             reads=[T1.b(), gl.b(c), xl.b(c)], writes=[xl.b(c)])

    wada2 = [k.alloc([128, 8, 1024], BF16, off=B0 + i * 16 * KB, name="wadaL%d" % i) for i in range(2)]
    cb2 = k.alloc([128, 8, 128], BF16, off=B0 + 32 * KB, name="cb2")
    csil2 = k.alloc([128, 8], F32, off=B0 + 34 * KB, name="csil2")
    brow2 = k.alloc([128, 2, D], F32, off=B0 + 36 * KB, name="brow2")

    def late_load(g):
        wb = wada2[g % 2]
        k.dma("pool", wb[:], wada_v[:, :, g * 1024:(g + 1) * 1024], writes=[wb.b()])

    def late_setup():
        for en in ("sp", "act", "dve", "pool"):
            k._wait(en, {"pe": k.cnt["pe"]})
        k.op("act", lambda e: e.activation(out=csil2[:], in_=cols[:, 0:8], func=AF.Silu), reads=[cols.b()], writes=[csil2.b()])
        for kk in range(8):
            k.op("dve", lambda e, kk=kk: e.tensor_copy(out=cb2[:, kk, :], in_=csil2[:, kk:kk + 1].to_broadcast([128, 128])),
                 reads=[csil2.b()], writes=[cb2.b()])
        k.dma("sp", brow2[:, 0, :], rows_d[0, :].partition_broadcast(128), writes=[brow2.b()])
        k.dma("sp", brow2[:, 1, :], rows_d[1, :].partition_broadcast(128), writes=[brow2.b()])
        late_load(2)
        late_load(3)

    def late_pe(g):
        wb = wada2[g % 2]
        pt = PS[1]
        adaln_cols(g, wb, cb2, pt, 0, pt.b(0))
        k.op("dve", lambda e: e.tensor_tensor(out=modT[:, 8 * g:8 * g + 8], in0=pt[:, 0:8], in1=cols[:, 8 + 8 * g:16 + 8 * g], op=ALU.add),
             reads=[pt.b(0), cols.b()], writes=[modT.b()])
        if g in (2, 5):
            adaln_gate(g, wb, cb2, brow2)

    lru_inproj(0)
    lru_conv(0)
    lru_inproj(1)
    for c in range(4):
        lru_gates(c)
        if c >= 2:
            late_pe(c)
            late_load(c + 2)
        if c + 1 < 4:
            lru_conv(c + 1)
        if c + 2 < 4:
            lru_inproj(c + 2)
            if c + 2 == 3:
                late_setup()
        lru_rest(c)
    late_pe(4)
    late_pe(5)
    adaln_AS(16, 64, 32, 24)
    dbg("modT", modT[:], [128, 48], [modT.b()])
    dbg("Grow_m", Grow_m[:], [128, D], [Grow_m.b()])
    dbg("lru", xl[:], [128, 4, S + 4], [xl.b(c) for c in range(4)])
    for g in range(4):
        bt, c0 = bank(g % 2)
        for c in range(4):
            sqt = sq[c % 2]
            k.op("act", lambda e, c=c, g=g, sqt=sqt: e.activation(out=sqt[:], in_=xl[:, c, 4 + g * 512:4 + (g + 1) * 512], func=AF.Square),
                 reads=[xl.b(c)], writes=[sqt.b()])
            k.op("pe", lambda e, c=c, bt=bt, c0=c0, sqt=sqt: e.matmul(bt[:, c0:c0 + 512], lhsT=onesf[:], rhs=sqt[:],
                                                                      start=(c == 0), stop=(c == 3)),
                 reads=[sqt.b(), onesf.b()], writes=[bt.b(c0)])
        rt = rs[g % 2]
        k.op("act", lambda e, bt=bt, c0=c0, rt=rt: e.activation(out=rt[:], in_=bt[:, c0:c0 + 512], func=AF.Sqrt, scale=1.0 / 512, bias=EPS),
             reads=[bt.b(c0)], writes=[rt.b()])
        k.op("dve", lambda e, rt=rt: e.reciprocal(out=rt[:], in_=rt[:]), reads=[rt.b()], writes=[rt.b()])
        for c in range(4):
            k.op("dve", lambda e, c=c, g=g, rt=rt: e.scalar_tensor_tensor(
                out=merged[:, c, g * 512:(g + 1) * 512], in0=xl[:, c, 4 + g * 512:4 + (g + 1) * 512],
                scalar=cols[:, 104 + c:105 + c], in1=rt[:], op0=ALU.mult, op1=ALU.mult),
                reads=[xl.b(c), rt.b(), cols.b()], writes=[merged.b(("l", g))])
    dbg("merged", merged[:], [128, 8, S], [merged.b(("l", g)) for g in range(4)] + [merged.b(("a", g)) for g in range(4)])
    k.barrier()

    if stop_after == 'D':
        k.barrier()
        return nc, stack, dbg_out
    h2T = k.alloc([128, 8, S], BF16, off=B0, name="h2T")
    wts = k.alloc([128, NT, NE], F32, off=B0 + 32 * KB, name="wts")
    yacc = k.alloc([128, NT, D], F32, off=B0 + 34 * KB, name="yacc")
    M0 = B0 + 98 * KB
    o = M0
    w_out = k.alloc([128, 8, D], BF16, off=o, name="w_out"); o += 16 * KB
    xr = [k.alloc([128, D], F32, off=o + i * 4 * KB, name="xr%d" % i) for i in range(3)]; o += 12 * KB
    tt_ = [k.alloc([128, D], F32, off=o + i * 4 * KB, name="tt%d" % i) for i in range(2)]; o += 8 * KB
    x2t = [k.alloc([128, D], F32, off=o + i * 4 * KB, name="x2t%d" % i) for i in range(4)]; o += 16 * KB
    hf = k.alloc([128, 8, 512], F32, off=o, name="hf"); o += 16 * KB
    wrt = k.alloc([128, 8, NE], F32, off=o, name="wrt"); o += KB
    lg = k.alloc([128, 4, 128], F32, off=o, name="lg"); o += 2 * KB
    xn = [k.alloc([128, D], F32, off=o + i * 4 * KB, name="xnb%d" % i) for i in range(4)]; o += 16 * KB
    junk2b = [k.alloc([128, D], BF16, off=o + i * 2 * KB, name="junkb%d" % i) for i in range(2)]; o += 4 * KB
    st3 = k.alloc([128, NT * 8], F32, off=o, name="st3"); o += 512
    assert o <= SB_TOP, o

    k.dma("pool", w_out[:], wout_d.rearrange("(k p) n -> p k n", p=128), writes=[w_out.b()])
    k.dma("sp", wrt[:], wr_d.rearrange("(k p) n -> p k n", p=128), writes=[wrt.b()])

    def ef_load(i):
        xt = xr[i % 3]
        k.dma("sp", xt[:], x_d[i * 128:(i + 1) * 128, :], writes=[xt.b()])

    def ef_s0(i):
        g = i // 4
        yt = PS[1 + i % 2]
        for hh in range(2):
            for kk in range(8):
                k.op("pe", lambda e, i=i, hh=hh, kk=kk, yt=yt: e.matmul(
                    yt[:, hh * 512:(hh + 1) * 512], lhsT=merged[:, kk, i * 128:(i + 1) * 128],
                    rhs=w_out[:, kk, hh * 512:(hh + 1) * 512], start=(kk == 0), stop=(kk == 7)),
                    reads=[merged.b(("l", g)), merged.b(("a", g)), w_out.b()], writes=[yt.b("y")])

    def ef_s1(i):
        xt = xr[i % 3]
        yt = PS[1 + i % 2]
        sc = st3[:, i * 8:i * 8 + 4]
        sb = st3.b(("y", i))
        jk = junk2b[i % 2]
        k.op("act", lambda e, yt=yt, sc=sc, jk=jk: e.activation(out=jk[:], in_=yt[:, :], func=AF.Square, accum_out=sc[:, 0:1]),
             reads=[yt.b("y")], writes=[jk.b(), sb])
        k.op("act", lambda e, sc=sc: e.activation(out=sc[:, 1:2], in_=sc[:, 0:1], func=AF.Sqrt, scale=1.0 / D, bias=EPS),
             reads=[sb], writes=[sb])
        k.op("dve", lambda e, sc=sc: e.reciprocal(out=sc[:, 2:3], in_=sc[:, 1:2]), reads=[sb], writes=[sb])
        tq = tt_[i % 2]
        k.op("dve", lambda e, yt=yt, sc=sc, tq=tq: e.scalar_tensor_tensor(
            out=tq[:], in0=yt[:, :], scalar=sc[:, 2:3], in1=Grow_m[:], op0=ALU.mult, op1=ALU.mult),
            reads=[yt.b("y"), sb, Grow_m.b()], writes=[tq.b()])
        x2 = x2t[i % 4]
        k.op("pool", lambda e, tq=tq, xt=xt, x2=x2: e.tensor_tensor(out=x2[:], in0=tq[:], in1=xt[:], op=ALU.add),
             reads=[tq.b(), xt.b()], writes=[x2.b()])
        k.dma("sp", x2_d[i * 128:(i + 1) * 128, :], x2[:], reads=[x2.b()], writes=[x2sc.b(i)])

    def ef_s2(i):
        x2 = x2t[i % 4]
        sc = st3[:, i * 8 + 4:i * 8 + 8]
        sb = st3.b(("x", i))
        jk = junk2b[i % 2]
        k.op("act", lambda e, x2=x2, sc=sc, jk=jk: e.activation(out=jk[:], in_=x2[:], func=AF.Square, accum_out=sc[:, 0:1]),
             reads=[x2.b()], writes=[jk.b(), sb])
        k.op("act", lambda e, sc=sc: e.activation(out=sc[:, 1:2], in_=sc[:, 0:1], func=AF.Sqrt, scale=1.0 / D, bias=EPS),
             reads=[sb], writes=[sb])
        k.op("dve", lambda e, sc=sc: e.reciprocal(out=sc[:, 2:3], in_=sc[:, 1:2]), reads=[sb], writes=[sb])
        k.op("dve", lambda e, x2=x2, sc=sc, i=i: e.tensor_scalar(out=xn[i % 4][:], in0=x2[:], scalar1=sc[:, 2:3],
                                                                  scalar2=None, op0=ALU.mult),
             reads=[x2.b(), sb], writes=[xn[i % 4].b()])

    def ef_s3(g):
        a0 = 16
        for kk in range(8):
            bt, c0 = bank(kk % 2)
            for tt in range(4):
                k.op("pe", lambda e, kk=kk, tt=tt, bt=bt, c0=c0: e.transpose(
                    out=bt[:, c0 + tt * 128:c0 + (tt + 1) * 128], in_=xn[tt][:, kk * 128:(kk + 1) * 128], identity=identf[:]),
                    reads=[xn[tt].b(), identf.b()], writes=[bt.b(kk % 2)])
            k.op("dve", lambda e, kk=kk, bt=bt, c0=c0: e.tensor_scalar(
                out=h2T[:, kk, g * 512:(g + 1) * 512], in0=bt[:, c0:c0 + 512],
                scalar1=AS[:, a0 + kk:a0 + kk + 1], scalar2=AS[:, a0 + 8 + kk:a0 + 9 + kk], op0=ALU.mult, op1=ALU.add),
                reads=[bt.b(kk % 2), AS.b()], writes=[h2T.b(g)])
            k.op("dve", lambda e, kk=kk, bt=bt, c0=c0: e.tensor_scalar(
                out=hf[:, kk, :], in0=bt[:, c0:c0 + 512],
                scalar1=AS[:, a0 + kk:a0 + kk + 1], scalar2=AS[:, a0 + 8 + kk:a0 + 9 + kk], op0=ALU.mult, op1=ALU.add),
                reads=[bt.b(kk % 2), AS.b()], writes=[hf.b()])

    def ef_s4(g):
        for tt in range(4):
            i = g * 4 + tt
            lt, lc0 = bank(6 + tt % 2)
            for kk in range(8):
                k.op("pe", lambda e, tt=tt, kk=kk, lt=lt, lc0=lc0: e.matmul(
                    lt[:, lc0:lc0 + NE], lhsT=hf[:, kk, tt * 128:(tt + 1) * 128], rhs=wrt[:, kk, :],
                    start=(kk == 0), stop=(kk == 7)), reads=[hf.b(), wrt.b()], writes=[lt.b(lc0)])
            L_ = lg[:, tt, 0:32]
            X_ = lg[:, tt, 32:48]
            M_ = lg[:, tt, 48:80]
            E_ = lg[:, tt, 80:112]
            lb = lg.b(tt)
            k.op("dve", lambda e, lt=lt, lc0=lc0, L_=L_: e.tensor_tensor(out=L_, in0=lt[:, lc0:lc0 + NE], in1=brout[:], op=ALU.add),
                 reads=[lt.b(lc0), brout.b()], writes=[lb])
            k.op("dve", lambda e, L_=L_, X_=X_: e.max(out=X_[:, 0:8], in_=L_), reads=[lb], writes=[lb])
            k.op("dve", lambda e, X_=X_: e.tensor_scalar(out=X_[:, 8:9], in0=X_[:, 0:1], scalar1=-1.0, scalar2=None, op0=ALU.mult),
                 reads=[lb], writes=[lb])
            k.op("dve", lambda e, L_=L_, X_=X_, M_=M_: e.tensor_scalar(out=M_, in0=L_, scalar1=X_[:, 3:4], scalar2=None, op0=ALU.is_ge),
                 reads=[lb], writes=[lb])
            k.op("act", lambda e, L_=L_, X_=X_, E_=E_: e.activation(out=E_, in_=L_, func=AF.Exp, bias=X_[:, 8:9]),
                 reads=[lb], writes=[lb])
            k.op("dve", lambda e, E_=E_, M_=M_, X_=X_: e.scalar_tensor_tensor(
                out=E_, in0=E_, scalar=1.0, in1=M_, op0=ALU.mult, op1=ALU.mult, accum_out=X_[:, 9:10]),
                reads=[lb], writes=[lb])
            k.op("dve", lambda e, X_=X_: e.reciprocal(out=X_[:, 10:11], in_=X_[:, 9:10]), reads=[lb], writes=[lb])
            k.op("dve", lambda e, E_=E_, X_=X_, i=i: e.tensor_scalar(out=wts[:, i, :], in0=E_, scalar1=X_[:, 10:11], scalar2=None, op0=ALU.mult),
                 reads=[lb], writes=[wts.b(i)])

    wup = [k.alloc([128, 8, 2, 512], BF16, off=M0 + i * 16 * KB, name="wup%d" % i) for i in range(2)]

    def load_up(n):
        e_, p_ = n // 2, n % 2
        src = wup_d[e_].rearrange("(k p) (t f) -> p k t f", p=128, t=2)
        for t in range(2):
            k.dma("pool", wup[n % 2][:, :, t, :], src[:, :, t, p_ * 512:(p_ + 1) * 512], writes=[wup[n % 2].b()])

    ef_load(0)
    ef_load(1)
    for step in range(NT + 5):
        if step < NT:
            ef_s0(step)
            if step == NT - 1:
                k._wait("pool", {"pe": k.cnt["pe"]})
                load_up(0)
        if 0 <= step - 1 < NT:
            ef_s1(step - 1)
        if step + 2 < NT:
            ef_load(step + 2)
        t3 = step - 3
        if 0 <= t3 < NT and t3 % 4 == 3:
            ef_s3(t3 // 4)
        t4 = step - 4
        if 0 <= t4 < NT and t4 % 4 == 3:
            ef_s4(t4 // 4)
        if 0 <= step - 2 < NT:
            ef_s2(step - 2)
    dbg("h2T", h2T[:], [128, 8, S], [h2T.b(g) for g in range(4)])
    dbg("wts", wts[:], [128, NT, NE], [wts.b(i) for i in range(NT)])
    k.barrier()

    if stop_after == 'EF':
        k.barrier()
        return nc, stack, dbg_out
    o = M0
    o += 32 * KB
    actb = [k.alloc([128, 4, S], BF16, off=o + i * 16 * KB, name="act%d" % i) for i in range(2)]; o += 32 * KB
    wdn = [k.alloc([128, 4, D], BF16, off=o + i * 8 * KB, name="wdn%d" % i) for i in range(2)]; o += 16 * KB
    TG_OFF = o
    tG = [k.alloc([128, 512], F32, off=o + i * 2 * KB, name="tG%d" % i) for i in range(2)]; o += 4 * KB
    tS = [k.alloc([128, 512], BF16, off=o + i * KB, name="tS%d" % i) for i in range(2)]; o += 2 * KB
    TL_OFF = o
    tL = [k.alloc([128, 512], F32, off=o + i * 2 * KB, name="tL%d" % i) for i in range(2)]; o += 4 * KB
    wT = k.alloc([32, 128], F32, off=o, name="wT"); o += 512
    BDN_OFF = o
    bdn = k.alloc([32, D], BF16, off=o, name="bdn"); o += 4 * KB
    assert o <= SB_TOP, (o, SB_TOP)
    MOE_END = o

    NP = NE * 2 if n_experts_dbg is None else n_experts_dbg * 2

    def load_dn(n):
        e_, p_ = n // 2, n % 2
        k.dma("pool", wdn[n % 2][:], wdn_d[e_, p_ * 512:(p_ + 1) * 512, :].rearrange("(kk p) d -> p kk d", p=128),
              writes=[wdn[n % 2].b()])

    load_dn(0)
    k.dma("pool", bdn[:], bdn_d[:, :], writes=[bdn.b()])
    wTh = k.alloc([32, 1024], BF16, off=BDN_OFF + 2 * KB, name="wTh")

    def yinit_T(half):
        pt = PS[2]
        for t8 in range(8):
            tt = half * 8 + t8
            k.op("pe", lambda e, tt=tt, t8=t8: e.transpose(
                out=pt[0:32, t8 * 128:(t8 + 1) * 128], in_=wts[:, tt, :], identity=identf[:]),
                reads=[wts.b(tt), identf.b()], writes=[pt.b("d")])
        k.op("act", lambda e: e.activation(out=wTh[:], in_=pt[0:32, :], func=AF.Copy),
             reads=[pt.b("d")], writes=[wTh.b()])

    def yinit_M(half):
        for t8 in range(8):
            tt = half * 8 + t8
            yt = PS[3 - t8 % 2]
            for hh in range(2):
                k.op("pe", lambda e, hh=hh, yt=yt, t8=t8: e.matmul(
                    yt[:, hh * 512:(hh + 1) * 512], lhsT=wTh[:, t8 * 128:(t8 + 1) * 128], rhs=bdn[:, hh * 512:(hh + 1) * 512],
                    start=True, stop=True),
                    reads=[wTh.b(), bdn.b()], writes=[yt.b("d")])
            k.op("act", lambda e, tt=tt, yt=yt: e.activation(out=yacc[:, tt, :], in_=yt[:, :], func=AF.Copy),
                 reads=[yt.b("d")], writes=[yacc.b(tt)])

    yinit_T(0)

    unit = [0]
    vcnt = [0]

    def up(n):
        e_, p_ = n // 2, n % 2
        wb = wup[n % 2]
        ab = actb[n % 2]
        for j in range(4):
            cg = 117 + e_ * 16 + 4 * p_ + j
            cl = e_ * 16 + 8 + 4 * p_ + j
            for tg in range(4):
                u = unit[0]; unit[0] += 1
                gt, gc0 = bank((u % 2) * 2)
                lt, lc0 = bank((u % 2) * 2 + 1)
                for kk in range(8):
                    k.op("pe", lambda e, kk=kk, j=j, tg=tg, gt=gt, gc0=gc0, wb=wb: e.matmul(
                        gt[:, gc0:gc0 + 512], lhsT=wb[:, kk, 0, j * 128:(j + 1) * 128], rhs=h2T[:, kk, tg * 512:(tg + 1) * 512],
                        start=(kk == 0), stop=(kk == 7)), reads=[wb.b(), h2T.b(tg)], writes=[gt.b(("u", gc0))])
                for kk in range(8):
                    k.op("pe", lambda e, kk=kk, j=j, tg=tg, lt=lt, lc0=lc0, wb=wb: e.matmul(
                        lt[:, lc0:lc0 + 512], lhsT=wb[:, kk, 1, j * 128:(j + 1) * 128], rhs=h2T[:, kk, tg * 512:(tg + 1) * 512],
                        start=(kk == 0), stop=(kk == 7)), reads=[wb.b(), h2T.b(tg)], writes=[lt.b(("u", lc0))])
                g_, s_, l_ = tG[u % 2], tS[u % 2], tL[u % 2]
                k.op("dve", lambda e, gt=gt, gc0=gc0, g_=g_, cg=cg: e.tensor_scalar(
                    out=g_[:], in0=gt[:, gc0:gc0 + 512], scalar1=cols[:, cg:cg + 1], scalar2=7.0, op0=ALU.add, op1=ALU.min),
                    reads=[gt.b(("u", gc0)), cols.b()], writes=[g_.b()])
                k.op("act", lambda e, g_=g_, s_=s_: e.activation(out=s_[:], in_=g_[:], func=AF.Gelu_apprx_sigmoid),
                     reads=[g_.b()], writes=[s_.b()])
                k.op("dve", lambda e, lt=lt, lc0=lc0, l_=l_, cl=cl: e.tensor_scalar(
                    out=l_[:], in0=lt[:, lc0:lc0 + 512], scalar1=b1c[:, cl:cl + 1], scalar2=8.0, op0=ALU.add, op1=ALU.min),
                    reads=[lt.b(("u", lc0)), b1c.b()], writes=[l_.b()])
                k.op("dve", lambda e, l_=l_, s_=s_, ab=ab, j=j, tg=tg: e.scalar_tensor_tensor(
                    out=ab[:, j, tg * 512:(tg + 1) * 512], in0=l_[:], scalar=-6.0, in1=s_[:], op0=ALU.max, op1=ALU.mult),
                    reads=[l_.b(), s_.b()], writes=[ab.b(tg)])

    FO = M0
    xf = [k.alloc([128, D], F32, off=FO + i * 4 * KB, name="xf%d" % i) for i in range(3)]
    tqf = [k.alloc([128, D], F32, off=FO + 12 * KB + i * 4 * KB, name="tf%d" % i) for i in range(2)]
    otf = [k.alloc([128, D], F32, off=FO + 20 * KB + i * 4 * KB, name="of%d" % i) for i in range(2)]
    junkf = [k.alloc([128, D], BF16, off=FO + 28 * KB + i * 2 * KB, name="junkf%d" % i) for i in range(2)]
    st3f = k.alloc([128, NT * 4], F32, off=MOE_END, name="st3f")
    assert MOE_END + 256 <= SB_TOP

    def final_load(i):
        k.dma("sp", xf[i % 3][:], x2_d[i * 128:(i + 1) * 128, :], reads=[x2sc.b(i)], writes=[xf[i % 3].b()])

    def final_tile(i):
        xt = xf[i % 3]
        sc = st3f[:, i * 4:i * 4 + 4]
        sb = st3f.b(i)
        jk = junkf[i % 2]
        k.op("act", lambda e, i=i, sc=sc, jk=jk: e.activation(out=jk[:], in_=yacc[:, i, :], func=AF.Square, accum_out=sc[:, 0:1]),
             reads=[yacc.b(i)], writes=[jk.b(), sb])
        k.op("act", lambda e, sc=sc: e.activation(out=sc[:, 1:2], in_=sc[:, 0:1], func=AF.Sqrt, scale=1.0 / D, bias=EPS),
             reads=[sb], writes=[sb])
        k.op("dve", lambda e, sc=sc: e.reciprocal(out=sc[:, 2:3], in_=sc[:, 1:2]), reads=[sb], writes=[sb])
        tq = tqf[i % 2]
        k.op("dve", lambda e, i=i, sc=sc, tq=tq: e.scalar_tensor_tensor(
            out=tq[:], in0=yacc[:, i, :], scalar=sc[:, 2:3], in1=Grow_f[:], op0=ALU.mult, op1=ALU.mult),
            reads=[yacc.b(i), sb, Grow_f.b()], writes=[tq.b()])
        ot = otf[i % 2]
        k.op("pool", lambda e, tq=tq, xt=xt, ot=ot: e.tensor_tensor(out=ot[:], in0=tq[:], in1=xt[:], op=ALU.add),
             reads=[tq.b(), xt.b()], writes=[ot.b()])
        k.dma("sp", out_d[i * 128:(i + 1) * 128, :], ot[:], reads=[ot.b()])
        if i + 2 < NT:
            final_load(i + 2)

    def down(n):
        e_, p_ = n // 2, n % 2
        wb = wdn[n % 2]
        ab = actb[n % 2]
        last = (n == NP - 1)
        if last:
            for en in ("sp", "act", "dve", "pool"):
                k._wait(en, {"pe": k.cnt["pe"]})
            final_load(0)
            final_load(1)
        for tt in range(NT):
            v = vcnt[0]; vcnt[0] += 1
            yt = PS[2 + v % 2]
            for hh in range(2):
                for kk in range(4):
                    k.op("pe", lambda e, tt=tt, hh=hh, kk=kk, yt=yt, wb=wb, ab=ab: e.matmul(
                        yt[:, hh * 512:(hh + 1) * 512], lhsT=ab[:, kk, tt * 128:(tt + 1) * 128], rhs=wb[:, kk, hh * 512:(hh + 1) * 512],
                        start=(kk == 0), stop=(kk == 3)), reads=[ab.b(tt // 4), wb.b()], writes=[yt.b("d")])
            k.op("dve", lambda e, tt=tt, yt=yt, e_=e_: e.scalar_tensor_tensor(
                out=yacc[:, tt, :], in0=yt[:, :], scalar=wts[:, tt, e_:e_ + 1], in1=yacc[:, tt, :], op0=ALU.mult, op1=ALU.add),
                reads=[yt.b("d"), wts.b(tt), yacc.b(tt)], writes=[yacc.b(tt)])
            if last:
                final_tile(tt)

    for n in range(NP + 1):
        if n < NP:
            if n + 1 < NP:
                load_up(n + 1)
            up(n)
            if n == 0:
                yinit_M(0)
                yinit_T(1)
                yinit_M(1)
        if n >= 1:
            down(n - 1)
        if n + 1 < NP:
            load_dn(n + 1)
    dbg("yacc", yacc[:], [128, NT, D], [yacc.b(i) for i in range(NT)])
    k.barrier()
    return nc, stack, dbg_out


def _prep_shared(inp):
    f = np.float32
    sh = {}
    sh["w_ada"] = np.ascontiguousarray(inp["w_ada"][0], f)
    sh["w_in"] = np.ascontiguousarray(inp["w_in"][0], f)
    sh["w_out"] = np.ascontiguousarray(inp["w_out"][0], f)
    sh["w_router"] = np.ascontiguousarray(inp["w_router"][0], f)
    sh["w_up"] = np.ascontiguousarray(inp["w_up"][0], f)
    sh["w_down"] = np.ascontiguousarray(inp["w_down"][0], f)
    sh["b_down"] = np.ascontiguousarray(inp["b_down"][0], f)
    sh["b_router"] = np.ascontiguousarray(inp["b_router"][0].reshape(1, NE), f)
    b_ada = inp["b_ada"][0]
    rows = np.zeros((4, D), f)
    rows[0] = b_ada[2 * D:3 * D]
    rows[1] = b_ada[5 * D:6 * D]
    rows[2] = inp["norm_mix_post"][0]
    rows[3] = inp["norm_ffn_post"][0]
    sh["rows"] = rows
    bd = np.zeros((128, 2, 4, 128), f)
    for a, w in enumerate((inp["lru_wa"][0], inp["lru_wx"][0])):
        for c in range(4):
            for hlf in range(2):
                bd[hlf * 64:(hlf + 1) * 64, a, c, hlf * 64:(hlf + 1) * 64] = w[2 * c + hlf]
    sh["bdiag"] = bd.reshape(128, -1)
    sel = np.zeros((97, 2, 8, 70), f)
    for h in range(8):
        sel[h, 0, h, 64] = 8.0
        sel[32 + h, 0, h, 65] = 8.0
        sel[64 + h, 0, h, 66] = 8.0
        sel[96, 0, h, 67:70] = 8.0
        sel[96, 1, h, 64:67] = 1.0
        sel[h, 1, h, 67] = -1.0
        sel[32 + h, 1, h, 68] = -1.0
        sel[64 + h, 1, h, 69] = -1.0
    sh["sel"] = sel.reshape(97, -1)
    cols = np.zeros((128, NCOL), f)
    colT = lambda v: np.asarray(v, f).reshape(-1, 128).T
    cols[:, 8:56] = colT(b_ada)
    cols[:, 56:64] = colT(inp["norm_mix_pre"][0])
    cols[:, 64:72] = colT(inp["norm_ffn_pre"][0])
    cw = inp["conv_w"][0]
    for c in range(4):
        for j in range(4):
            cols[:, 72 + c * 4 + j] = cw[j, c * 128:(c + 1) * 128]
    cols[:, 88:92] = colT(inp["conv_b"][0])
    cols[:, 92:96] = colT(inp["lru_ba"][0].reshape(-1))
    cols[:, 96:100] = colT(inp["lru_bx"][0].reshape(-1))
    cols[:, 100:104] = colT(inp["lru_lambda"][0])
    cols[:, 104:108] = colT(inp["gn_lru"][0])
    cols[:, 108:112] = colT(inp["gn_attn"][0])
    fb = inp["attn_fb"][0]
    for r0 in (0, 32, 64):
        cols[r0:r0 + 8, 112] = fb
    cols[0:8, 113] = 1.0
    cols[32:40, 114] = 1.0
    cols[64:72, 115] = 1.0
    cols[96, 116] = 1.0
    bu = inp["b_up"][0]
    cols[:, 117:629] = bu.reshape(NE, 16, 128).transpose(2, 0, 1).reshape(128, NE * 16)
    return sh, cols


_CACHE = {}


def kernel(**inputs):
    inp = {k_: np.asarray(v) for k_, v in inputs.items()}
    n = 8
    sh, cols0 = _prep_shared(inp)
    if "prog" not in _CACHE:
        _CACHE["prog"] = build_program()
    nc, stack, _ = _CACHE["prog"]
    in_maps = []
    for b in range(n):
        m = dict(sh)
        m["x"] = np.ascontiguousarray(inp["x"][b], np.float32)
        cols = cols0.copy()
        cols[:, 0:8] = np.asarray(inp["c"][b], np.float32).reshape(8, 128).T
        m["cols"] = cols
        in_maps.append(m)
    res = run_bass_kernel_spmd(nc, in_maps, core_ids=list(range(n)))
    out = np.stack([np.asarray(r["out"], np.float32) for r in res.results], axis=0)
    return out
```
